# Optimizing a Trainium2 kernel written in Bass

```python
import math
import jax, jax.numpy as jnp
from jax import lax
import numpy as np

D_MODEL = 2048
BATCH = 4
SEQ = 4096
DEPTH = 2

CHUNK = 64
N_MIXERS = 2
EPS = 1e-6
S5_GROUP_CH = 16
S5_GROUPS = D_MODEL // S5_GROUP_CH
S5_STATE = 64
S5_DT_MIN = 1e-3
S5_DT_MAX = 1e-1
ML_HEADS = 8
ML_QK = D_MODEL // 2
ML_V = D_MODEL
ML_DQK = ML_QK // ML_HEADS
ML_DV = ML_V // ML_HEADS
ML_IN = 2 * ML_QK + 2 * ML_V + 2 * ML_HEADS
GATE_SOFTCAP = 15.0
MOE_GROUPS = 8
MOE_PER_GROUP = 8
MOE_EXPERTS = MOE_GROUPS * MOE_PER_GROUP
MOE_TOPK = 2
MOE_HIDDEN = 768
MOE_BLOCK = 128

kernel_name = "hybrid_s5_mlstm_hmoe_trunk"


def rmsnorm(x, g):
    x32 = x.astype(jnp.float32)
    r = x32 * lax.rsqrt(jnp.mean(x32 * x32, axis=-1, keepdims=True) + EPS)
    return (r * g.astype(jnp.float32)).astype(x.dtype)


def s5_mixer(h, lam_re, lam_im, log_dt, b_re, b_im, c_re, c_im, d_skip, w_glu):
    bsz, s, d = h.shape
    h32 = h.astype(jnp.float32)
    u = h32.reshape(bsz, s, S5_GROUPS, S5_GROUP_CH)
    lam = lax.complex(lam_re.astype(jnp.float32), lam_im.astype(jnp.float32))
    dt = jnp.exp(log_dt.astype(jnp.float32))[:, None]
    lam_bar = jnp.exp(lam * dt)
    b_c = lax.complex(b_re.astype(jnp.float32), b_im.astype(jnp.float32))
    b_bar = ((lam_bar - 1.0) / lam)[..., None] * b_c
    bu = lax.complex(jnp.einsum('bsgc,gpc->bsgp', u, jnp.real(b_bar)),
                     jnp.einsum('bsgc,gpc->bsgp', u, jnp.imag(b_bar)))
    a = jnp.broadcast_to(lam_bar[None, None], (1, s, S5_GROUPS, S5_STATE))

    def combine(e1, e2):
        a1, b1 = e1
        a2, b2 = e2
        return a1 * a2, a2 * b1 + b2

    _, states = lax.associative_scan(combine, (a, bu), axis=1)
    y = (jnp.einsum('bsgp,gcp->bsgc', jnp.real(states), c_re.astype(jnp.float32))
         - jnp.einsum('bsgp,gcp->bsgc', jnp.imag(states), c_im.astype(jnp.float32)))
    y = y.reshape(bsz, s, d) + d_skip.astype(jnp.float32) * h32
    g = jax.nn.gelu(y).astype(h.dtype)
    vg = g @ w_glu
    val, gate = vg[..., :d], vg[..., d:]
    return (val * jax.nn.sigmoid(gate)).astype(h.dtype)


def mlstm_chunkwise(q, k, v, ig, lf):
    bsz, nh, s, dk = q.shape
    dv = v.shape[-1]
    nc = s // CHUNK

    def to_chunks(t):
        return jnp.moveaxis(t.reshape(bsz, nh, nc, CHUNK, *t.shape[3:]), 2, 0)

    qc, kc, vc, ic, fc = (to_chunks(t) for t in (q, k, v, ig, lf))
    causal = jnp.tril(jnp.ones((CHUNK, CHUNK), dtype=bool))

    def step(carry, xs):
        c_mat, n_vec, m = carry
        q_, k_, v_, i_, f_ = xs
        bcum = jnp.cumsum(f_, axis=-1)
        a_log = bcum + m[..., None]
        d_log = bcum[..., :, None] - bcum[..., None, :] + i_[..., None, :]
        d_log = jnp.where(causal, d_log, -jnp.inf)
        m_t = jnp.maximum(a_log, jnp.max(d_log, axis=-1))
        w_intra = jnp.exp(d_log - m_t[..., None])
        w_inter = jnp.exp(a_log - m_t)
        sc = jnp.einsum('bhtd,bhsd->bhts', q_, k_) * w_intra
        num = (w_inter[..., None] * jnp.einsum('bhtd,bhde->bhte', q_, c_mat)
               + jnp.einsum('bhts,bhse->bhte', sc, v_))
        den = w_inter * jnp.einsum('bhtd,bhd->bht', q_, n_vec) + jnp.sum(sc, axis=-1)
        h_out = num / jnp.maximum(jnp.abs(den), jnp.exp(-m_t))[..., None]
        b_last = bcum[..., -1]
        g_log = b_last[..., None] - bcum + i_
        m_new = jnp.maximum(b_last + m, jnp.max(g_log, axis=-1))
        decay = jnp.exp(b_last + m - m_new)
        wk = jnp.exp(g_log - m_new[..., None])
        c_new = decay[..., None, None] * c_mat + jnp.einsum('bhs,bhsd,bhse->bhde', wk, k_, v_)
        n_new = decay[..., None] * n_vec + jnp.einsum('bhs,bhsd->bhd', wk, k_)
        return (c_new, n_new, m_new), h_out

    init = (jnp.zeros((bsz, nh, dk, dv), jnp.float32),
            jnp.zeros((bsz, nh, dk), jnp.float32),
            jnp.zeros((bsz, nh), jnp.float32))
    _, hs = lax.scan(step, init, (qc, kc, vc, ic, fc))
    return jnp.moveaxis(hs, 0, 2).reshape(bsz, nh, s, dv)


def mlstm_mixer(h, w_in, b_gate, g_head, w_out):
    bsz, s, _ = h.shape
    z = h @ w_in
    o1, o2, o3, o4, o5 = ML_QK, 2 * ML_QK, 2 * ML_QK + ML_V, 2 * ML_QK + 2 * ML_V, 2 * ML_QK + 2 * ML_V + ML_HEADS
    q, k, v, o = z[..., :o1], z[..., o1:o2], z[..., o2:o3], z[..., o3:o4]
    b32 = b_gate.astype(jnp.float32)
    ig_pre = z[..., o4:o5].astype(jnp.float32) + b32[:ML_HEADS]
    fg_pre = z[..., o5:].astype(jnp.float32) + b32[ML_HEADS:]
    ig = GATE_SOFTCAP * jnp.tanh(ig_pre / GATE_SOFTCAP)
    lf = jax.nn.log_sigmoid(GATE_SOFTCAP * jnp.tanh(fg_pre / GATE_SOFTCAP))

    def heads(t, dh):
        return t.astype(jnp.float32).reshape(bsz, s, ML_HEADS, dh).transpose(0, 2, 1, 3)

    qh = heads(q, ML_DQK)
    kh = heads(k, ML_DQK) * (ML_DQK ** -0.5)
    vh = heads(v, ML_DV)
    hh = mlstm_chunkwise(qh, kh, vh, ig.transpose(0, 2, 1), lf.transpose(0, 2, 1))
    hh = hh * lax.rsqrt(jnp.mean(hh * hh, axis=-1, keepdims=True) + EPS)
    hh = hh * g_head.astype(jnp.float32).reshape(ML_HEADS, 1, ML_DV)
    hh = hh.transpose(0, 2, 1, 3).reshape(bsz, s, ML_V)
    out = (jax.nn.sigmoid(o.astype(jnp.float32)) * hh).astype(h.dtype)
    return out @ w_out


def hier_moe(h, layer, w_group, b_group, w_expert, b_expert, w_gu, w_down):
    bsz, s, d = h.shape
    n_tok = bsz * s
    ht = h.reshape(n_tok, d)
    h32 = ht.astype(jnp.float32)
    lg = h32 @ w_group.astype(jnp.float32) + b_group.astype(jnp.float32)
    pg = jax.nn.softmax(lg, axis=-1)
    g_sel = jnp.argmax(lg, axis=-1).astype(jnp.int32)
    p_sel = jnp.take_along_axis(pg, g_sel[:, None], axis=1)
    le = (h32 @ w_expert.astype(jnp.float32) + b_expert.astype(jnp.float32)).reshape(n_tok, MOE_GROUPS, MOE_PER_GROUP)
    le_sel = jnp.take_along_axis(le, g_sel[:, None, None], axis=1)[:, 0]
    top_v, top_j = lax.top_k(le_sel, MOE_TOPK)
    gates = p_sel * jax.nn.softmax(top_v, axis=-1)
    expert_idx = g_sel[:, None] * MOE_PER_GROUP + top_j.astype(jnp.int32)

    n_asg = n_tok * MOE_TOPK
    n_blocks = n_asg // MOE_BLOCK + MOE_EXPERTS
    flat_e = expert_idx.reshape(-1)
    flat_g = gates.reshape(-1)
    flat_tok = jnp.arange(n_asg, dtype=jnp.int32) // MOE_TOPK
    order = jnp.argsort(flat_e, stable=True)
    sorted_e = flat_e[order]
    counts = jnp.bincount(flat_e, length=MOE_EXPERTS).astype(jnp.int32)
    padded = ((counts + MOE_BLOCK - 1) // MOE_BLOCK) * MOE_BLOCK
    pad_end = jnp.cumsum(padded)
    pad_start = pad_end - padded
    seg_start = jnp.cumsum(counts) - counts
    dest = pad_start[sorted_e] + (jnp.arange(n_asg, dtype=jnp.int32) - seg_start[sorted_e])
    buf_tok = jnp.zeros((n_blocks * MOE_BLOCK,), jnp.int32).at[dest].set(flat_tok[order])
    buf_w = jnp.zeros((n_blocks * MOE_BLOCK,), jnp.float32).at[dest].set(flat_g[order])
    block_start = jnp.arange(n_blocks, dtype=jnp.int32) * MOE_BLOCK
    block_expert = jnp.minimum(jnp.searchsorted(pad_end, block_start, side='right'),
                               MOE_EXPERTS - 1).astype(jnp.int32)

    def expert_block(args):
        tok, e = args
        xb = ht[tok]
        gu = xb @ w_gu[layer, e]
        act = jax.nn.silu(gu[:, :MOE_HIDDEN]) * gu[:, MOE_HIDDEN:]
        return act @ w_down[layer, e]

    yb = lax.map(expert_block, (buf_tok.reshape(n_blocks, MOE_BLOCK), block_expert))
    out = jnp.zeros((n_tok, d), jnp.float32).at[buf_tok].add(
        yb.reshape(-1, d).astype(jnp.float32) * buf_w[:, None])
    return out.reshape(bsz, s, d).astype(h.dtype)


def setup_inputs(seed: int = 0) -> dict:
    key = jax.random.key(seed)
    ks = jax.random.split(key, 32)
    f32 = jnp.float32
    nrm = jax.random.normal
    n_a = (DEPTH + N_MIXERS - 1) // N_MIXERS
    n_b = DEPTH // N_MIXERS
    D, G, P, CG = D_MODEL, S5_GROUPS, S5_STATE, S5_GROUP_CH
    x = nrm(ks[0], (BATCH, SEQ, D), f32)
    n_idx = jnp.arange(P, dtype=f32)
    s5_lam_re = -0.5 + 0.01 * nrm(ks[1], (n_a, G, P), f32)
    s5_lam_im = math.pi * n_idx + 0.01 * nrm(ks[2], (n_a, G, P), f32)
    s5_log_dt = jax.random.uniform(ks[3], (n_a, G), f32, minval=math.log(S5_DT_MIN), maxval=math.log(S5_DT_MAX))
    s5_b_re = nrm(ks[4], (n_a, G, P, CG), f32) * (2 * CG) ** -0.5
    s5_b_im = nrm(ks[5], (n_a, G, P, CG), f32) * (2 * CG) ** -0.5
    s5_c_re = nrm(ks[6], (n_a, G, CG, P), f32) * (2 * P) ** -0.5
    s5_c_im = nrm(ks[7], (n_a, G, CG, P), f32) * (2 * P) ** -0.5
    s5_d = nrm(ks[8], (n_a, D), f32)
    s5_w_glu = nrm(ks[9], (n_a, D, 2 * D), f32) * D ** -0.5
    ml_w_in = nrm(ks[10], (n_b, D, ML_IN), f32) * D ** -0.5
    ig_b = 0.01 * nrm(ks[11], (n_b, ML_HEADS), f32)
    fg_b = jnp.linspace(3.0, 6.0, ML_HEADS, dtype=f32) + 0.01 * nrm(ks[12], (n_b, ML_HEADS), f32)
    ml_b_gate = jnp.concatenate([ig_b, fg_b], axis=-1)
    ml_g_head = 1.0 + 0.01 * nrm(ks[13], (n_b, ML_V), f32)
    ml_w_out = nrm(ks[14], (n_b, ML_V, D), f32) * ML_V ** -0.5
    norm_mix = 1.0 + 0.01 * nrm(ks[15], (DEPTH, D), f32)
    norm_ffn = 1.0 + 0.01 * nrm(ks[16], (DEPTH, D), f32)
    moe_w_group = nrm(ks[17], (DEPTH, D, MOE_GROUPS), f32) * D ** -0.5
    moe_b_group = 0.01 * nrm(ks[18], (DEPTH, MOE_GROUPS), f32)
    moe_w_expert = nrm(ks[19], (DEPTH, D, MOE_EXPERTS), f32) * D ** -0.5
    moe_b_expert = 0.01 * nrm(ks[20], (DEPTH, MOE_EXPERTS), f32)
    moe_w_gu = nrm(ks[21], (DEPTH, MOE_EXPERTS, D, 2 * MOE_HIDDEN), f32) * D ** -0.5
    moe_w_down = nrm(ks[22], (DEPTH, MOE_EXPERTS, MOE_HIDDEN, D), f32) * MOE_HIDDEN ** -0.5
    norm_final = 1.0 + 0.01 * nrm(ks[23], (D,), f32)
    return {"x": x, "s5_lam_re": s5_lam_re, "s5_lam_im": s5_lam_im, "s5_log_dt": s5_log_dt,
            "s5_b_re": s5_b_re, "s5_b_im": s5_b_im, "s5_c_re": s5_c_re, "s5_c_im": s5_c_im,
            "s5_d": s5_d, "s5_w_glu": s5_w_glu, "ml_w_in": ml_w_in, "ml_b_gate": ml_b_gate,
            "ml_g_head": ml_g_head, "ml_w_out": ml_w_out, "norm_mix": norm_mix, "norm_ffn": norm_ffn,
            "moe_w_group": moe_w_group, "moe_b_group": moe_b_group, "moe_w_expert": moe_w_expert,
            "moe_b_expert": moe_b_expert, "moe_w_gu": moe_w_gu, "moe_w_down": moe_w_down,
            "norm_final": norm_final}


def reference(x, s5_lam_re, s5_lam_im, s5_log_dt, s5_b_re, s5_b_im, s5_c_re, s5_c_im,
              s5_d, s5_w_glu, ml_w_in, ml_b_gate, ml_g_head, ml_w_out, norm_mix, norm_ffn,
              moe_w_group, moe_b_group, moe_w_expert, moe_b_expert, moe_w_gu, moe_w_down,
              norm_final):
    for layer in range(DEPTH):
        j = layer // N_MIXERS
        h = rmsnorm(x, norm_mix[layer])
        if layer % N_MIXERS == 0:
            mix = s5_mixer(h, s5_lam_re[j], s5_lam_im[j], s5_log_dt[j], s5_b_re[j], s5_b_im[j],
                           s5_c_re[j], s5_c_im[j], s5_d[j], s5_w_glu[j])
        else:
            mix = mlstm_mixer(h, ml_w_in[j], ml_b_gate[j], ml_g_head[j], ml_w_out[j])
        x = x + mix
        h = rmsnorm(x, norm_ffn[layer])
        x = x + hier_moe(h, layer, moe_w_group[layer], moe_b_group[layer], moe_w_expert[layer],
                         moe_b_expert[layer], moe_w_gu, moe_w_down)
    return rmsnorm(x, norm_final)
```

```python
from contextlib import ExitStack
import numpy as np
import concourse.bass as bass
import concourse.mybir as mybir
from concourse.bass_utils import run_bass_kernel_spmd

F32 = mybir.dt.float32
BF16 = mybir.dt.bfloat16
AF = mybir.ActivationFunctionType
ALU = mybir.AluOpType
AX = mybir.AxisListType

D = 2048
EPS = 1e-6
NE = 64
import os
DBG_CUT = int(os.environ.get('DBG_CUT', '0'))
DBG_OUT = int(os.environ.get('DBG_OUT', '0'))
DBG_NE = int(os.environ.get('DBG_NE', '0'))
FH = 768


class Buf:
    def __init__(self, t, name):
        self.t = t
        self.name = name
        self.w = None
        self.r = []

    def __getitem__(self, k):
        return V(self.t[k], self)


class V:
    def __init__(self, ap, buf):
        self.ap = ap
        self.buf = buf

    def re(self, s, **kw):
        return V(self.ap.rearrange(s, **kw), self.buf)

    def __getitem__(self, k):
        return V(self.ap[k], self.buf)


class Prog:
    def __init__(self, nc, es):
        self.nc = nc
        self.es = es
        self.eng = {"pe": nc.tensor, "act": nc.scalar, "dve": nc.vector, "pool": nc.gpsimd, "sp": nc.sync}
        self.sem = {}
        self.cnt = {}
        self.waited = {}
        for k in self.eng:
            self.sem[k] = es.enter_context(nc.semaphore("s_" + k))
            self.cnt[k] = 0
        self.ndma = 0

    def dma_sem(self, name):
        key = "d_" + name
        self.sem[key] = self.es.enter_context(self.nc.semaphore(key))
        self.cnt[key] = 0
        return key

    def sb(self, name, shape, dt):
        return Buf(self.es.enter_context(self.nc.sbuf_tensor(name, list(shape), dt)), name)

    def ps(self, name, shape, dt=F32):
        return Buf(self.es.enter_context(self.nc.psum_tensor(name, list(shape), dt)), name)

    def dram(self, name, shape, dt, kind="Internal"):
        if DBG_OUT and kind == "Internal" and name != "Xs":
            kind = "ExternalOutput"
        t = self.nc.dram_tensor(name, list(shape), dt, kind=kind)
        return Buf(t.ap(), name)

    def _deps(self, reads, writes):
        deps = {}
        def add(d):
            if d is None:
                return
            k, s = d
            if deps.get(k, 0) < s:
                deps[k] = s
        for v in reads:
            add(v.buf.w)
        for v in writes:
            add(v.buf.w)
            for d in v.buf.r:
                add(d)
        return deps

    def _wait(self, ek, deps):
        e = self.eng[ek]
        for k, s in deps.items():
            if k == ek and ek in ("pe", "sp"):
                continue
            if self.waited.get((ek, k), 0) >= s:
                continue
            mult = 1
            if k.startswith("d_"):
                mult = 16
                s = self.cnt[k]
            e.wait_ge(self.sem[k], s * mult)
            self.waited[(ek, k)] = s

    def _record(self, key, seq, reads, writes):
        for v in reads:
            v.buf.r.append((key, seq))
            if len(v.buf.r) > 64:
                m = {}
                for k, s in v.buf.r:
                    if m.get(k, 0) < s:
                        m[k] = s
                v.buf.r = list(m.items())
        for v in writes:
            v.buf.w = (key, seq)
            v.buf.r = []

    def op(self, ek, fn, reads, writes, sig=True):
        self._wait(ek, self._deps(reads, writes))
        ins = fn(self.eng[ek])
        seq = self.cnt[ek] + 1
        if sig:
            ins.then_inc(self.sem[ek], 1)
            self.cnt[ek] = seq
        self._record(ek, seq, reads, writes)
        return ins

    def dma(self, qk, dkey, out, in_, **kw):
        self._wait(qk, self._deps([in_], [out]))
        ins = self.eng[qk].dma_start(out=out.ap, in_=in_.ap, **kw)
        ins.then_inc(self.sem[dkey], 16)
        self.cnt[dkey] += 1
        self._record(dkey, self.cnt[dkey], [in_], [out])
        self.ndma += 1
        return ins

    def mm(self, out, lhsT, rhs, start, stop, sig=None):
        if sig is None:
            sig = stop
        return self.op("pe", lambda e: e.matmul(out.ap, lhsT.ap, rhs.ap, start=start, stop=stop),
                       [lhsT, rhs], [out], sig=sig)

    def tr(self, out, in_, ident, sig=True):
        return self.op("pe", lambda e: e.transpose(out.ap, in_.ap, ident.ap), [in_, ident], [out], sig=sig)

    def act(self, out, in_, func, bias=None, scale=None, accum=None, eng="act"):
        kw = {}
        reads = [in_]
        writes = [out]
        if bias is not None:
            if isinstance(bias, V):
                kw["bias"] = bias.ap
                reads.append(bias)
            else:
                kw["bias"] = bias
        if scale is not None:
            if isinstance(scale, V):
                kw["scale"] = scale.ap
                reads.append(scale)
            else:
                kw["scale"] = scale
        if accum is not None:
            kw["accum_out"] = accum.ap
            writes.append(accum)
        return self.op(eng, lambda e: e.activation(out=out.ap, in_=in_.ap, func=func, **kw), reads, writes)

    def ts(self, out, in0, s1, s2, op0, op1=None, eng="dve", accum=None):
        reads = [in0]
        writes = [out]
        a1 = s1
        a2 = s2
        if isinstance(s1, V):
            reads.append(s1)
            a1 = s1.ap
        if isinstance(s2, V):
            reads.append(s2)
            a2 = s2.ap
        kw = {}
        if op1 is not None:
            kw["op1"] = op1
        if accum is not None:
            kw["accum_out"] = accum.ap
            writes.append(accum)
        return self.op(eng, lambda e: e.tensor_scalar(out=out.ap, in0=in0.ap, scalar1=a1, scalar2=a2, op0=op0, **kw),
                       reads, writes)

    def tt(self, out, in0, in1, op, eng="dve"):
        return self.op(eng, lambda e: e.tensor_tensor(out=out.ap, in0=in0.ap, in1=in1.ap, op=op), [in0, in1], [out])

    def stt(self, out, in0, scalar, in1, op0, op1, eng="dve"):
        reads = [in0, in1]
        a = scalar
        if isinstance(scalar, V):
            reads.append(scalar)
            a = scalar.ap
        return self.op(eng, lambda e: e.scalar_tensor_tensor(out=out.ap, in0=in0.ap, scalar=a, in1=in1.ap,
                                                              op0=op0, op1=op1), reads, [out])

    def copy(self, out, in_, eng="dve"):
        if eng == "act":
            return self.op("act", lambda e: e.copy(out=out.ap, in_=in_.ap), [in_], [out])
        return self.op(eng, lambda e: e.tensor_copy(out=out.ap, in_=in_.ap), [in_], [out])

    def memset(self, out, val, eng="dve"):
        return self.op(eng, lambda e: e.memset(out.ap, val), [], [out])

    def red(self, out, in_, op, eng="dve"):
        return self.op(eng, lambda e: e.tensor_reduce(out=out.ap, in_=in_.ap, axis=AX.X, op=op), [in_], [out])

    def dbg(self, name, v, shape, dt):
        if not DBG_OUT:
            return
        if not hasattr(self, "_dq"):
            self._dq = self.dma_sem("dbg")
        t = self.nc.dram_tensor("dbg_" + name, list(shape), dt, kind="ExternalOutput")
        self.dma("sp", self._dq, V(t.ap(), Buf(t.ap(), name)), v)

    def barrier(self):
        for ek, e in self.eng.items():
            for k, c in self.cnt.items():
                if c > 0 and k != ek and self.waited.get((ek, k), 0) < c:
                    e.wait_ge(self.sem[k], c * (16 if k.startswith("d_") else 1))
                    self.waited[(ek, k)] = c

    def finish(self):
        e = self.eng["sp"]
        for k, c in self.cnt.items():
            if c > 0 and k != "sp":
                e.wait_ge(self.sem[k], c * (16 if k.startswith("d_") else 1))


class Ctx:
    pass


def setup_common(P, ntok):
    c = Ctx()
    c.ntok = ntok
    c.ident = P.sb("ident", [128, 128], F32)
    c.identb = P.sb("identb", [128, 128], BF16)
    nc = P.nc
    P.memset(c.ident[:], 1.0, eng="pool")
    P.op("pool", lambda e: e.affine_select(out=c.ident.t[:], in_=c.ident.t[:], pattern=[[-1, 128]],
                                           compare_op=ALU.is_equal, fill=0.0, base=0, channel_multiplier=1),
         [c.ident[:]], [c.ident[:]])
    P.copy(c.identb[:], c.ident[:], eng="pool")
    c.psb = [P.ps("psb%d" % i, [128, 512], F32) for i in range(8)]
    c.gvec = P.sb("gvec", [128, D], F32)
    c.dq = {k: P.dma_sem(k) for k in ["misc", "xin", "xout", "w0", "w1", "w2", "w3"]}
    return c


def rms_h(P, c, xt, ht, tmp, stat):
    P.act(tmp, xt, AF.Square, accum=stat[:, 0:1])
    P.ts(stat[:, 1:2], stat[:, 0:1], 1.0 / D, EPS, ALU.mult, ALU.add)
    P.act(stat[:, 3:4], stat[:, 1:2], AF.Sqrt)
    P.op("dve", lambda e: e.reciprocal(out=stat.buf.t[:, 2:3], in_=stat.buf.t[:, 3:4]), [stat], [stat])
    P.stt(ht, xt, stat[:, 2:3], c.gvec[:], ALU.mult, ALU.mult)


def stage_moe(P, c, X, layer, io, TT=512):
    ntok = c.ntok
    nsub = TT // 128
    KC = D // 128
    es = ExitStack()
    P_es = P.es
    P.es = es
    _sb0 = P.sb
    P.sb = lambda name, shape, dt: _sb0("%s_l%d" % (name, layer), shape, dt)
    acc = P.sb("m_acc", [128, nsub, D], F32)
    h32 = P.sb("m_h32", [128, D], F32)
    sq = P.sb("m_sq", [128, D], F32)
    stat = P.sb("m_stat", [128, 8], F32)
    hT32 = P.sb("m_hT32", [128, KC, 128], F32)
    hTb = P.sb("m_hTb", [128, KC, TT], BF16)
    actT = P.sb("m_actT", [128, 6, TT], BF16)
    sil = [P.sb("m_sil%d" % i, [128, TT], F32) for i in range(2)]
    wr = P.sb("m_wr", [128, KC, 72], F32)
    br = P.sb("m_br", [128, 72], F32)
    G = P.sb("m_G", [128, nsub, NE], F32)
    lg = P.sb("m_lg", [128, 72], F32)
    lem = P.sb("m_lem", [128, NE], F32)
    lem2 = P.sb("m_lem2", [128, NE], F32)
    msk = P.sb("m_msk", [128, NE], F32)
    sm = P.sb("m_sm", [128, 16], F32)
    wslot = [P.sb("m_w%d" % i, [128, 6 * D], BF16) for i in range(4)]
    wq = ["w0", "w1", "w2", "w3"]
    psb = c.psb

    P.dma("sp", c.dq["misc"], c.gvec[:], V(io["norm_ffn"].t[layer:layer + 1, :].partition_broadcast(128), io["norm_ffn"]))
    P.dma("sp", c.dq["misc"], wr[:, :, 0:8], V(io["moe_w_group"].t[layer].rearrange("(k p) n -> p k n", p=128), io["moe_w_group"]))
    P.dma("sp", c.dq["misc"], wr[:, :, 8:72], V(io["moe_w_expert"].t[layer].rearrange("(k p) n -> p k n", p=128), io["moe_w_expert"]))
    P.dma("sp", c.dq["misc"], br[:, 0:8], V(io["moe_b_group"].t[layer:layer + 1, :].partition_broadcast(128), io["moe_b_group"]))
    P.dma("sp", c.dq["misc"], br[:, 8:72], V(io["moe_b_expert"].t[layer:layer + 1, :].partition_broadcast(128), io["moe_b_expert"]))

    wgu = io.get("moe_w_gu")
    wdn = io.get("moe_w_down")
    slot_i = [0]

    def load_piece(e, piece):
        s = slot_i[0] % 4
        slot_i[0] += 1
        dst = wslot[s]
        if piece < 2:
            src = wgu.t[layer, e, :, piece * FH:(piece + 1) * FH].rearrange("(k p) f -> p k f", p=128)
            dv = dst[:, :].re("p (k f) -> p k f", k=KC)
            for hk in range(2):
                P.dma("pool", c.dq[wq[s]], dv[:, hk * 8:(hk + 1) * 8, :], V(src[:, hk * 8:(hk + 1) * 8, :], wgu))
        else:
            src = wdn.t[layer, e].rearrange("(k p) d -> p k d", p=128)
            dv = dst[:, :].re("p (k d) -> p k d", k=6)
            for hk in range(2):
                P.dma("pool", c.dq[wq[s]], dv[:, hk * 3:(hk + 1) * 3, :], V(src[:, hk * 3:(hk + 1) * 3, :], wdn))
        return dst

    for t0 in range(0, ntok, TT):
        for s in range(nsub):
            r0 = t0 + s * 128
            P.dma("sp", c.dq["xin"], acc[:, s, :], X[r0:r0 + 128, :])
            rms_h(P, c, acc[:, s, :], h32[:], sq[:], stat[:])
            if DBG_CUT == 2:
                continue
            for q in range(4):
                for j in range(4):
                    k = q * 4 + j
                    P.tr(psb[q][:, j * 128:(j + 1) * 128], h32[:, k * 128:(k + 1) * 128], c.ident[:], sig=(j == 3))
                if DBG_CUT == 6:
                    continue
                P.copy(hT32[:, q * 4:(q + 1) * 4, :], psb[q][:, :].re("p (j t) -> p j t", j=4), eng="act")
                if DBG_CUT == 7:
                    continue
                P.copy(hTb[:, q * 4:(q + 1) * 4, s * 128:(s + 1) * 128], hT32[:, q * 4:(q + 1) * 4, :], eng="dve")
            if DBG_CUT in (3, 6, 7):
                continue
            for k in range(KC):
                P.mm(psb[4][:, 0:72], hT32[:, k, :], wr[:, k, :], start=(k == 0), stop=(k == KC - 1))
            if DBG_CUT == 4:
                continue
            P.tt(lg[:], psb[4][:, 0:72], br[:], ALU.add)
            P.red(sm[:, 0:1], lg[:, 0:8], ALU.max)
            P.ts(sm[:, 1:2], sm[:, 0:1], -1.0, None, ALU.mult)
            P.act(msk[:, 0:8], lg[:, 0:8], AF.Exp, bias=sm[:, 1:2], accum=sm[:, 2:3])
            P.op("dve", lambda e: e.reciprocal(out=sm.t[:, 3:4], in_=sm.t[:, 2:3]), [sm[:]], [sm[:]])
            P.ts(msk[:, 8:16], lg[:, 0:8], sm[:, 0:1], None, ALU.is_ge)
            P.ts(msk[:, 8:16], msk[:, 8:16], 1e30, -1e30, ALU.mult, ALU.add)
            P.tt(lem[:, :].re("p (g e) -> p g e", g=8), lg[:, 8:72].re("p (g e) -> p g e", g=8),
                 V(msk.t[:, 8:16].unsqueeze(2).broadcast_to([128, 8, 8]), msk), ALU.add)
            if DBG_CUT == 5:
                continue
            P.red(sm[:, 4:5], lem[:], ALU.max)
            P.ts(msk[:], lem[:], sm[:, 4:5], None, ALU.is_ge)
            P.stt(lem2[:], msk[:], -1e30, lem[:], ALU.mult, ALU.add)
            P.red(sm[:, 5:6], lem2[:], ALU.max)
            P.ts(lem2[:], lem2[:], sm[:, 5:6], None, ALU.is_ge)
            P.tt(sm[:, 6:7], sm[:, 5:6], sm[:, 4:5], ALU.subtract)
            P.act(sm[:, 7:8], sm[:, 6:7], AF.Exp)
            P.ts(sm[:, 8:9], sm[:, 7:8], 1.0, None, ALU.add)
            P.op("dve", lambda e: e.reciprocal(out=sm.t[:, 9:10], in_=sm.t[:, 8:9]), [sm[:]], [sm[:]])
            P.tt(sm[:, 10:11], sm[:, 9:10], sm[:, 3:4], ALU.mult)
            P.tt(sm[:, 11:12], sm[:, 10:11], sm[:, 7:8], ALU.mult)
            P.ts(msk[:], msk[:], sm[:, 10:11], None, ALU.mult)
            P.stt(G[:, s, :], lem2[:], sm[:, 11:12], msk[:], ALU.mult, ALU.add)
        for e in range((DBG_NE or NE) if not DBG_CUT else 0):
            wg = load_piece(e, 0)
            wu = load_piece(e, 1)
            wd = load_piece(e, 2)
            wgv = wg[:, :].re("p (k f) -> p k f", k=KC)
            wuv = wu[:, :].re("p (k f) -> p k f", k=KC)
            wdv = wd[:, :].re("p (k d) -> p k d", k=6)
            for fc in range(6):
                pg = psb[(fc % 2) * 2]
                pu = psb[(fc % 2) * 2 + 1]
                for k in range(KC):
                    P.mm(pg[:, 0:TT], wgv[:, k, fc * 128:(fc + 1) * 128], hTb[:, k, :], start=(k == 0), stop=(k == KC - 1))
                for k in range(KC):
                    P.mm(pu[:, 0:TT], wuv[:, k, fc * 128:(fc + 1) * 128], hTb[:, k, :], start=(k == 0), stop=(k == KC - 1))
                P.act(sil[fc % 2][:, :], pg[:, 0:TT], AF.Silu)
                P.tt(actT[:, fc, :], sil[fc % 2][:, :], pu[:, 0:TT], ALU.mult)
            i = 0
            for s in range(nsub):
                for dg in range(4):
                    pd = psb[4 + (i % 4)]
                    i += 1
                    for fc in range(6):
                        P.mm(pd[:, :], actT[:, fc, s * 128:(s + 1) * 128], wdv[:, fc, dg * 512:(dg + 1) * 512],
                             start=(fc == 0), stop=(fc == 5))
                    P.stt(acc[:, s, dg * 512:(dg + 1) * 512], pd[:, :], G[:, s, e:e + 1],
                          acc[:, s, dg * 512:(dg + 1) * 512], ALU.mult, ALU.add)
        for s in range(nsub):
            r0 = t0 + s * 128
            P.dma("sp", c.dq["xout"], X[r0:r0 + 128, :], acc[:, s, :])
    P.barrier()
    P.es = P_es
    P.sb = _sb0
    return es


I32 = mybir.dt.int32


def floor_pos(P, out, x, itmp, ftmp):
    P.copy(itmp, x)
    P.copy(out, itmp)
    P.tt(ftmp, out, x, ALU.is_gt)
    P.tt(out, out, ftmp, ALU.subtract)


def stage_moe_g(P, c, X, layer, io, TS=512):
    ntok = c.ntok
    nsub = TS // 128
    KC = D // 128
    NTI = ntok // 128
    NT = ntok // TS + 8
    NSLOT = NT * TS
    psb = c.psb
    L_ = "_g%d" % layer
    HS = P.dram("mg_HS" + L_, [NSLOT, D], BF16)
    GS = P.dram("mg_GS" + L_, [NSLOT, 8], F32)
    YS = P.dram("mg_YS" + L_, [NSLOT, D], F32)
    wsrc = [io["moe_wg_r"], io["moe_wu_r"], io["moe_wd_r"]]
    eso = ExitStack(); P_eso = P.es; P.es = eso
    slot_i = P.sb("mg_slot" + L_, [128, NTI], I32)
    idxW = P.sb("mg_idxW" + L_, [128, NT * 8], I32)
    P.es = P_eso
    es = ExitStack(); P_es = P.es; P.es = es
    _sb0 = P.sb
    P.sb = lambda name, shape, dt: _sb0(name + L_, shape, dt)
    hall = P.sb("g_hall", [128, NTI, D], BF16)
    xt = P.sb("g_xt", [128, D], F32)
    h32 = P.sb("g_h32", [128, D], F32)
    sq = P.sb("g_sq", [128, D], F32)
    stat = P.sb("g_stat", [128, 8], F32)
    hT32 = P.sb("g_hT32", [128, KC, 128], F32)
    wr = P.sb("g_wr", [128, KC, 72], F32)
    br = P.sb("g_br", [128, 72], F32)
    G = P.sb("g_G", [128, NE], F32)
    lg = P.sb("g_lg", [128, 72], F32)
    lem = P.sb("g_lem", [128, NE], F32)
    lem2 = P.sb("g_lem2", [128, NE], F32)
    msk = P.sb("g_msk", [128, NE], F32)
    sm = P.sb("g_sm", [128, 16], F32)
    OH = P.sb("g_OH", [128, NTI, 8], F32)
    G8 = P.sb("g_G8", [128, NTI, 8], F32)
    R = P.sb("g_R", [128, NTI, 8], F32)
    OHs = P.sb("g_OHs", [128, 8], F32)
    Ls = P.sb("g_Ls", [128, 128], F32)
    ones = P.sb("g_ones", [128, 128], F32)
    zb = P.sb("g_zb", [128, D], BF16)
    sc = P.sb("g_sc", [128, 64], F32)
    sci = P.sb("g_sci", [128, 64], I32)
    jv = P.sb("g_jv", [128, NT], F32)
    gidf = P.sb("g_gidf", [128, NT], F32)
    idf = P.sb("g_idf", [128, NT, 8], F32)
    Tt = P.sb("g_Tt", [128, NTI, 8], F32)
    slf = P.sb("g_slf", [128, NTI], F32)
    P.memset(Ls[:], 1.0, eng="pool")
    P.op("pool", lambda e: e.affine_select(out=Ls.t[:], in_=Ls.t[:], pattern=[[1, 128]], compare_op=ALU.is_gt,
                                           fill=0.0, base=0, channel_multiplier=-1), [Ls[:]], [Ls[:]])
    P.memset(ones[:], 1.0)
    P.memset(OHs[:], 0.0)
    P.memset(zb[:], 0.0)
    P.op("pool", lambda e: e.iota(jv.t[:], pattern=[[1, NT]], base=0, channel_multiplier=0,
                                  allow_small_or_imprecise_dtypes=True), [], [jv[:]])
    P.op("pool", lambda e: e.iota(idf.t[:], pattern=[[0, NT], [128, 8]], base=layer * NE * 128, channel_multiplier=1,
                                  allow_small_or_imprecise_dtypes=True), [], [idf[:]])
    for r0 in range(0, NSLOT, 128):
        P.dma("sp", c.dq["xout"], HS[r0:r0 + 128, :], zb[:])
    P.dma("sp", c.dq["xout"], V(GS.t.rearrange("(a p) e -> p a e", p=128), GS),
          V(zb.t[:, 0:NSLOT // 128 * 8 * 2].bitcast(F32).rearrange("p (a e) -> p a e", e=8), zb))
    P.dma("sp", c.dq["misc"], c.gvec[:], V(io["norm_ffn"].t[layer:layer + 1, :].partition_broadcast(128), io["norm_ffn"]))
    P.dma("sp", c.dq["misc"], wr[:, :, 0:8], V(io["moe_w_group"].t[layer].rearrange("(k p) n -> p k n", p=128), io["moe_w_group"]))
    P.dma("sp", c.dq["misc"], wr[:, :, 8:72], V(io["moe_w_expert"].t[layer].rearrange("(k p) n -> p k n", p=128), io["moe_w_expert"]))
    P.dma("sp", c.dq["misc"], br[:, 0:8], V(io["moe_b_group"].t[layer:layer + 1, :].partition_broadcast(128), io["moe_b_group"]))
    P.dma("sp", c.dq["misc"], br[:, 8:72], V(io["moe_b_expert"].t[layer:layer + 1, :].partition_broadcast(128), io["moe_b_expert"]))
    for i in range(NTI):
        r0 = i * 128
        P.dma("sp", c.dq["xin"], xt[:], X[r0:r0 + 128, :])
        rms_h(P, c, xt[:], h32[:], sq[:], stat[:])
        P.copy(hall[:, i, :], h32[:], eng="pool")
        for q in range(4):
            for j in range(4):
                k = q * 4 + j
                P.tr(psb[q][:, j * 128:(j + 1) * 128], h32[:, k * 128:(k + 1) * 128], c.ident[:], sig=(j == 3))
            P.copy(hT32[:, q * 4:(q + 1) * 4, :], psb[q][:, :].re("p (j t) -> p j t", j=4), eng="act")
        for k in range(KC):
            P.mm(psb[4][:, 0:72], hT32[:, k, :], wr[:, k, :], start=(k == 0), stop=(k == KC - 1))
        P.tt(lg[:], psb[4][:, 0:72], br[:], ALU.add)
        P.red(sm[:, 0:1], lg[:, 0:8], ALU.max)
        P.ts(sm[:, 1:2], sm[:, 0:1], -1.0, None, ALU.mult)
        P.act(msk[:, 0:8], lg[:, 0:8], AF.Exp, bias=sm[:, 1:2], accum=sm[:, 2:3])
        P.op("dve", lambda e: e.reciprocal(out=sm.t[:, 3:4], in_=sm.t[:, 2:3]), [sm[:]], [sm[:]])
        P.ts(OH[:, i, :], lg[:, 0:8], sm[:, 0:1], None, ALU.is_ge)
        P.ts(msk[:, 8:16], OH[:, i, :], 1e30, -1e30, ALU.mult, ALU.add)
        P.tt(lem[:, :].re("p (g e) -> p g e", g=8), lg[:, 8:72].re("p (g e) -> p g e", g=8),
             V(msk.t[:, 8:16].unsqueeze(2).broadcast_to([128, 8, 8]), msk), ALU.add)
        P.red(sm[:, 4:5], lem[:], ALU.max)
        P.ts(msk[:], lem[:], sm[:, 4:5], None, ALU.is_ge)
        P.stt(lem2[:], msk[:], -1e30, lem[:], ALU.mult, ALU.add)
        P.red(sm[:, 5:6], lem2[:], ALU.max)
        P.ts(lem2[:], lem2[:], sm[:, 5:6], None, ALU.is_ge)
        P.tt(sm[:, 6:7], sm[:, 5:6], sm[:, 4:5], ALU.subtract)
        P.act(sm[:, 7:8], sm[:, 6:7], AF.Exp)
        P.ts(sm[:, 8:9], sm[:, 7:8], 1.0, None, ALU.add)
        P.op("dve", lambda e: e.reciprocal(out=sm.t[:, 9:10], in_=sm.t[:, 8:9]), [sm[:]], [sm[:]])
        P.tt(sm[:, 10:11], sm[:, 9:10], sm[:, 3:4], ALU.mult)
        P.tt(sm[:, 11:12], sm[:, 10:11], sm[:, 7:8], ALU.mult)
        P.ts(msk[:], msk[:], sm[:, 10:11], None, ALU.mult)
        P.stt(G[:], lem2[:], sm[:, 11:12], msk[:], ALU.mult, ALU.add)
        P.red(G8[:, i, :], G[:, :].re("p (g e) -> p e g", g=8), ALU.add)
        P.mm(psb[5][:, 0:8], Ls[:], OH[:, i, :], True, False, sig=False)
        P.mm(psb[5][:, 0:8], ones[:], OHs[:], False, True)
        P.copy(R[:, i, :], psb[5][:, 0:8])
        P.tt(OHs[:], OHs[:], OH[:, i, :], ALU.add)
    P.mm(psb[5][:, 8:16], ones[:], OHs[:], True, True)
    P.ts(sc[:, 0:8], psb[5][:, 8:16], float(TS - 1), 1.0 / TS, ALU.add, ALU.mult)
    floor_pos(P, sc[:, 8:16], sc[:, 0:8], sci[:, 0:8], sc[:, 16:24])
    P.copy(sc[:, 24:25], sc[:, 8:9])
    for g in range(1, 8):
        P.tt(sc[:, 24 + g:25 + g], sc[:, 23 + g:24 + g], sc[:, 8 + g:9 + g], ALU.add)
    P.tt(sc[:, 32:40], sc[:, 24:32], sc[:, 8:16], ALU.subtract)
    P.ts(sc[:, 32:40], sc[:, 32:40], float(TS), None, ALU.mult)
    P.tt(Tt[:], R[:], V(sc.t[:, 32:40].unsqueeze(1).broadcast_to([128, NTI, 8]), sc), ALU.add)
    P.tt(Tt[:], Tt[:], OH[:], ALU.mult)
    P.red(slf[:], Tt[:], ALU.add)
    P.copy(slot_i[:], slf[:])
    P.ts(gidf[:], jv[:], sc[:, 24:25], None, ALU.is_ge)
    for g in range(1, 8):
        P.stt(gidf[:], jv[:], sc[:, 24 + g:25 + g], gidf[:], ALU.is_ge, ALU.add)
    P.ts(gidf[:], gidf[:], 7.0, 1024.0, ALU.min, ALU.mult)
    P.tt(idf[:], idf[:], V(gidf.t[:, :].unsqueeze(2).broadcast_to([128, NT, 8]), gidf), ALU.add)
    P.copy(idxW[:, :].re("p (j e) -> p j e", e=8), idf[:])
    for i in range(NTI):
        for (dst, src) in ((HS, hall[:, i, :]), (GS, G8[:, i, :])):
            P._wait("pool", P._deps([src, slot_i[:]], [dst[:, :]]))
            ins = P.nc.gpsimd.indirect_dma_start(out=dst.t[:, :], out_offset=bass.IndirectOffsetOnAxis(ap=slot_i.t[:, i:i + 1], axis=0),
                                                 in_=src.ap, in_offset=None)
            ins.then_inc(P.sem[c.dq["w3"]], 16); P.cnt[c.dq["w3"]] += 1
            P._record(c.dq["w3"], P.cnt[c.dq["w3"]], [src, slot_i[:]], [dst[:, :]])
    P.barrier(); P.es = P_es; P.sb = _sb0; es.close()

    es = ExitStack(); P_es = P.es; P.es = es
    _sb0 = P.sb
    P.sb = lambda name, shape, dt: _sb0(name + L_, shape, dt)
    acc = P.sb("h_acc", [128, nsub, D], F32)
    hs = P.sb("h_hs", [128, nsub, D], BF16)
    hTb = P.sb("h_hTb", [128, KC, TS], BF16)
    actT = P.sb("h_actT", [128, 6, TS], BF16)
    sil = [P.sb("h_sil%d" % i, [128, TS], F32) for i in range(2)]
    gst = P.sb("h_gst", [128, nsub, 8], F32)
    wslot = [P.sb("h_w%d" % i, [128, 6 * D], BF16) for i in range(4)]
    wq = ["w0", "w1", "w2", "w3"]
    slot_n = [0]
    pbf = [V(psb[i].t[:, :].bitcast(BF16), psb[i]) for i in range(2)]

    def gather_piece(j, e, piece):
        s_ = slot_n[0] % 4
        slot_n[0] += 1
        dst = wslot[s_]
        ixa = idxW.t[:, j * 8 + e:j * 8 + e + 1]
        src = wsrc[piece]
        P._wait("pool", P._deps([idxW[:]], [dst[:]]))
        ins = P.nc.gpsimd.indirect_dma_start(out=dst.t[:, :], out_offset=None, in_=src.t[:, :],
                                             in_offset=bass.IndirectOffsetOnAxis(ap=ixa, axis=0))
        ins.then_inc(P.sem[c.dq[wq[s_]]], 16); P.cnt[c.dq[wq[s_]]] += 1
        P._record(c.dq[wq[s_]], P.cnt[c.dq[wq[s_]]], [src[:, :], idxW[:]], [dst[:]])
        return dst

    for j in range(NT):
        t0 = j * TS
        P.dma("sp", c.dq["xin"], hs[:], V(HS.t[t0:t0 + TS, :].rearrange("(s p) d -> p s d", p=128), HS))
        P.dma("sp", c.dq["xin"], gst[:], V(GS.t[t0:t0 + TS, :].rearrange("(s p) e -> p s e", p=128), GS))
        for s in range(nsub):
            for q in range(2):
                for jj in range(8):
                    k = q * 8 + jj
                    P.tr(pbf[q][:, jj * 128:(jj + 1) * 128], hs[:, s, k * 128:(k + 1) * 128], c.identb[:], sig=(jj == 7))
                P.copy(hTb[:, q * 8:(q + 1) * 8, s * 128:(s + 1) * 128], pbf[q][:, :].re("p (j t) -> p j t", j=8), eng="act")
        for e in range(8):
            wg = gather_piece(j, e, 0)
            wu = gather_piece(j, e, 1)
            wd = gather_piece(j, e, 2)
            wgv = wg[:, :].re("p (k f) -> p k f", k=KC)
            wuv = wu[:, :].re("p (k f) -> p k f", k=KC)
            wdv = wd[:, :].re("p (k d) -> p k d", k=6)
            for fc in range(6):
                pg = psb[(fc % 2) * 2]
                pu = psb[(fc % 2) * 2 + 1]
                for k in range(KC):
                    P.mm(pg[:, 0:TS], wgv[:, k, fc * 128:(fc + 1) * 128], hTb[:, k, :], start=(k == 0), stop=(k == KC - 1))
                for k in range(KC):
                    P.mm(pu[:, 0:TS], wuv[:, k, fc * 128:(fc + 1) * 128], hTb[:, k, :], start=(k == 0), stop=(k == KC - 1))
                P.act(sil[fc % 2][:, :], pg[:, 0:TS], AF.Silu)
                P.tt(actT[:, fc, :], sil[fc % 2][:, :], pu[:, 0:TS], ALU.mult)
            i = 0
            for s in range(nsub):
                for dg in range(4):
                    pd = psb[4 + (i % 4)]
                    i += 1
                    for fc in range(6):
                        P.mm(pd[:, :], actT[:, fc, s * 128:(s + 1) * 128], wdv[:, fc, dg * 512:(dg + 1) * 512],
                             start=(fc == 0), stop=(fc == 5))
                    a_ = acc[:, s, dg * 512:(dg + 1) * 512]
                    if e == 0:
                        P.ts(a_, pd[:, :], gst[:, s, 0:1], None, ALU.mult)
                    else:
                        P.stt(a_, pd[:, :], gst[:, s, e:e + 1], a_, ALU.mult, ALU.add)
        P.dma("sp", c.dq["xout"], V(YS.t[t0:t0 + TS, :].rearrange("(s p) d -> p s d", p=128), YS), acc[:])
        if j == 1 and layer == 0:
            P.dbg("g_hs", hs[:], [128, nsub, D], BF16)
            P.dbg("g_gst", gst[:], [128, nsub, 8], F32)
            P.dbg("g_hTb", hTb[:], [128, KC, TS], BF16)
            P.dbg("g_acc", acc[:], [128, nsub, D], F32)
    P.barrier(); P.es = P_es; P.sb = _sb0; es.close()

    es = ExitStack(); P_es = P.es; P.es = es
    _sb0 = P.sb
    P.sb = lambda name, shape, dt: _sb0(name + L_, shape, dt)
    xr = [P.sb("r_x%d" % i, [128, D], F32) for i in range(2)]
    yr = [P.sb("r_y%d" % i, [128, D], F32) for i in range(2)]
    ysem = [c.dq["w0"], c.dq["w1"]]
    for i in range(NTI):
        b = i % 2
        r0 = i * 128
        P.dma("sp", c.dq["xin"], xr[b][:], X[r0:r0 + 128, :])
        P._wait("pool", P._deps([YS[:, :], slot_i[:]], [yr[b][:]]))
        ins = P.nc.gpsimd.indirect_dma_start(out=yr[b].t[:, :], out_offset=None, in_=YS.t[:, :],
                                             in_offset=bass.IndirectOffsetOnAxis(ap=slot_i.t[:, i:i + 1], axis=0))
        ins.then_inc(P.sem[ysem[b]], 16); P.cnt[ysem[b]] += 1
        P._record(ysem[b], P.cnt[ysem[b]], [YS[:, :], slot_i[:]], [yr[b][:]])
        P.tt(xr[b][:], xr[b][:], yr[b][:], ALU.add)
        P.dma("sp", c.dq["xout"], X[r0:r0 + 128, :], xr[b][:])
    P.barrier(); P.es = P_es; P.sb = _sb0; es.close()
    eso.close()


def front_end(P, c, X, t0, nsub, xt, h32, sq, stat, hTb, psb, identb_unused=None):
    for s in range(nsub):
        r0 = t0 + s * 128
        P.dma("sp", c.dq["xin"], xt[:], X[r0:r0 + 128, :])
        rms_h(P, c, xt[:], h32[:], sq[:], stat[:])
        for q in range(4):
            for j in range(4):
                k = q * 4 + j
                P.tr(psb[q][:, j * 128:(j + 1) * 128], h32[:, k * 128:(k + 1) * 128], c.ident[:], sig=(j == 3))
            P.copy(hTb[:, q * 4:(q + 1) * 4, s * 128:(s + 1) * 128], psb[q][:, :].re("p (j t) -> p j t", j=4), eng="act")


def stage_mlstm(P, c, X, io):
    ntok = c.ntok
    NH, DK, DV, L = 8, 128, 256, 64
    KC = D // 128
    TT = 512
    nsub = TT // 128
    psb = c.psb
    win = io["ml_w_in"]
    QT = P.dram("ml_QT", [NH, DK, ntok], BF16)
    KT = P.dram("ml_KT", [NH, DK, ntok], BF16)
    Ktm = P.dram("ml_Ktm", [ntok, NH * DK], F32)
    Vs = P.dram("ml_V", [ntok, NH * DV], BF16)
    Os = P.dram("ml_O", [ntok, NH * DV], F32)
    Gs = P.dram("ml_G", [ntok, 16], F32)
    OUTs = P.dram("ml_OUT", [ntok, NH * DV], BF16)

    es = ExitStack(); P_es = P.es; P.es = es
    xt = P.sb("a_xt", [128, D], F32)
    h32 = P.sb("a_h32", [128, D], F32)
    sq = P.sb("a_sq", [128, D], F32)
    stat = P.sb("a_stat", [128, 8], F32)
    hTb = P.sb("a_hTb", [128, KC, TT], BF16)
    wsl = [P.sb("a_w%d" % i, [128, KC, 512], BF16) for i in range(3)]
    wg = P.sb("a_wg", [128, KC, 16], BF16)
    bg = P.sb("a_bg", [128, 16], F32)
    stb = [P.sb("a_stb%d" % i, [128, 512], BF16) for i in range(3)]
    stf = [P.sb("a_stf%d" % i, [128, 512], F32) for i in range(3)]
    gt = P.sb("a_gt", [128, 48], F32)
    P.dma("sp", c.dq["misc"], c.gvec[:], V(io["norm_mix"].t[1:2, :].partition_broadcast(128), io["norm_mix"]))
    P.dma("pool", c.dq["misc"], wg[:], V(win.t[0, :, 6144:6160].rearrange("(k p) n -> p k n", p=128), win))
    P.dma("sp", c.dq["misc"], bg[:], V(io["ml_b_gate"].t[0:1, :].partition_broadcast(128), io["ml_b_gate"]))
    wq = ["w0", "w1", "w2"]
    cnt = [0, 0, 0]
    for t0 in range(0, ntok, TT):
        front_end(P, c, X, t0, nsub, xt, h32, sq, stat, hTb, psb)
        for s in range(nsub):
            r0 = t0 + s * 128
            for k in range(KC):
                P.mm(psb[4][:, 0:16], hTb[:, k, s * 128:(s + 1) * 128], wg[:, k, :], start=(k == 0), stop=(k == KC - 1))
            P.tt(gt[:, 0:16], psb[4][:, 0:16], bg[:], ALU.add)
            P.act(gt[:, 16:32], gt[:, 0:16], AF.Tanh, scale=1.0 / 15.0)
            P.ts(gt[:, 32:40], gt[:, 16:24], 15.0, None, ALU.mult)
            P.act(gt[:, 0:8], gt[:, 24:32], AF.Exp, scale=-15.0)
            P.ts(gt[:, 0:8], gt[:, 0:8], 1.0, None, ALU.add)
            P.act(gt[:, 8:16], gt[:, 0:8], AF.Ln)
            P.ts(gt[:, 40:48], gt[:, 8:16], -1.0, None, ALU.mult)
            P.dma("sp", c.dq["xout"], Gs[r0:r0 + 128, :], gt[:, 32:48])
        for blk in range(12):
            i = cnt[0] % 3
            cnt[0] += 1
            w = wsl[i]
            for hk in range(2):
                P.dma("pool", c.dq[wq[i]], w[:, hk * 8:(hk + 1) * 8, :],
                      V(win.t[0, :, blk * 512:(blk + 1) * 512].rearrange("(k p) n -> p k n", p=128)[:, hk * 8:(hk + 1) * 8, :], win))
            if blk < 4:
                dst = QT if blk < 2 else KT
                sc = 1.0 if blk < 2 else DK ** -0.5
                for hh in range(4):
                    head = (blk % 2) * 4 + hh
                    pp = psb[4 + (cnt[1] % 2)]
                    for k in range(KC):
                        P.mm(pp[:, :], w[:, k, hh * 128:(hh + 1) * 128], hTb[:, k, :], start=(k == 0), stop=(k == KC - 1))
                    sb_ = stb[cnt[1] % 3]
                    cnt[1] += 1
                    P.act(sb_[:], pp[:, :], AF.Copy, scale=sc)
                    P.dma("sp", c.dq["xout"], dst[head, :, t0:t0 + TT], sb_[:])
            if blk >= 2:
                for s in range(nsub):
                    r0 = t0 + s * 128
                    pp = psb[6 + (cnt[2] % 2)]
                    for k in range(KC):
                        P.mm(pp[:, :], hTb[:, k, s * 128:(s + 1) * 128], w[:, k, :], start=(k == 0), stop=(k == KC - 1))
                    j = cnt[2] % 3
                    cnt[2] += 1
                    if blk < 4:
                        P.act(stf[j][:], pp[:, :], AF.Copy, scale=DK ** -0.5)
                        P.dma("sp", c.dq["xout"], Ktm[r0:r0 + 128, (blk - 2) * 512:(blk - 1) * 512], stf[j][:])
                    elif blk < 8:
                        P.copy(stb[j][:], pp[:, :], eng="act")
                        P.dma("sp", c.dq["xout"], Vs[r0:r0 + 128, (blk - 4) * 512:(blk - 3) * 512], stb[j][:])
                    else:
                        P.act(stf[j][:], pp[:, :], AF.Sigmoid)
                        P.dma("sp", c.dq["xout"], Os[r0:r0 + 128, (blk - 8) * 512:(blk - 7) * 512], stf[j][:])
    P.barrier(); P.es = P_es; es.close()

    es = ExitStack(); P_es = P.es; P.es = es
    triU = P.sb("b_triU", [64, 64], F32)
    negm = P.sb("b_negm", [64, 64], F32)
    ones = P.sb("b_ones", [64, 128], F32)
    onesb = P.sb("b_onesb", [64, 1], BF16)
    Cst = P.sb("b_C", [128, NH, DV], F32)
    Cb = P.sb("b_Cb", [128, NH, DV], BF16)
    nst = P.sb("b_n", [128, NH], F32)
    nb = P.sb("b_nb", [128, NH], BF16)
    ghead = P.sb("b_gh", [64, NH * DV], F32)
    P.memset(triU[:], 1.0, eng="pool")
    P.op("pool", lambda e: e.affine_select(out=triU.t[:], in_=triU.t[:], pattern=[[1, 64]], compare_op=ALU.is_ge,
                                           fill=0.0, base=0, channel_multiplier=-1), [triU[:]], [triU[:]])
    P.ts(negm[:], triU[:], 30000.0, -30000.0, ALU.mult, ALU.add)
    P.memset(ones[:], 1.0)
    P.memset(onesb[:], 1.0)
    P.memset(Cst[:], 0.0); P.memset(Cb[:], 0.0); P.memset(nst[:], 0.0); P.memset(nb[:], 0.0)
    P.dma("sp", c.dq["misc"], ghead[:], V(io["ml_g_head"].t[0:1, :].partition_broadcast(64), io["ml_g_head"]))
    NB = 2
    qT = [P.sb("b_qT%d" % i, [128, NH, L], BF16) for i in range(NB)]
    kT = [P.sb("b_kT%d" % i, [128, NH, L], BF16) for i in range(NB)]
    ktm = [P.sb("b_ktm%d" % i, [64, NH * DK], F32) for i in range(NB)]
    vv = [P.sb("b_v%d" % i, [64, NH, DV], BF16) for i in range(NB)]
    oo = [P.sb("b_o%d" % i, [64, NH * DV], F32) for i in range(NB)]
    gg = [P.sb("b_g%d" % i, [64, 16], F32) for i in range(NB)]
    dsem = [P.dma_sem("bl%d" % i) for i in range(NB)]
    sml = P.sb("b_sml", [128, 96], F32)
    bts = P.sb("b_bts", [128, NH * L], F32)
    lfb = P.sb("b_lfb", [64, NH, 128], F32)
    Eb = P.sb("b_E", [64, NH, L], F32)
    Dm = P.sb("b_Dm", [64, NH, L], F32)
    SD = P.sb("b_SD", [64, NH, L], BF16)
    aT = P.sb("b_aT", [128, NH, L], F32)
    qs = P.sb("b_qs", [128, NH, L], BF16)
    kw = P.sb("b_kw", [64, NH, DK], BF16)
    hh = P.sb("b_hh", [64, NH, DV], F32)
    hsq = P.sb("b_hsq", [64, DV], F32)
    gho = P.sb("b_gho", [64, NH * DV], F32)
    outb = [P.sb("b_out%d" % i, [64, NH * DV], BF16) for i in range(2)]
    nchunk = ntok // L

    def load_chunk(ci):
        b = ci % NB
        t0 = ci * L
        P.dma("sp", dsem[b], qT[b][:], V(QT.t[:, :, t0:t0 + L].rearrange("h d t -> d h t"), QT))
        P.dma("sp", dsem[b], kT[b][:], V(KT.t[:, :, t0:t0 + L].rearrange("h d t -> d h t"), KT))
        P.dma("sp", dsem[b], ktm[b][:], Ktm[t0:t0 + L, :])
        P.dma("sp", dsem[b], vv[b][:, :, :].re("t h d -> t (h d)"), Vs[t0:t0 + L, :])
        P.dma("sp", dsem[b], oo[b][:], Os[t0:t0 + L, :])
        P.dma("sp", dsem[b], gg[b][:], Gs[t0:t0 + L, :])

    load_chunk(0)
    for ci in range(nchunk):
        b = ci % NB
        if ci + 1 < nchunk:
            load_chunk(ci + 1)
        ig = gg[b][:, 0:8]
        lf = gg[b][:, 8:16]
        p0 = psb[0]
        P.mm(p0[0:64, 0:8], triU[:], lf, True, True)
        P.mm(p0[0:64, 8:16], ones[:, 0:64], lf, True, True)
        P.mm(p0[:, 16:24], ones[:, :], lf, True, True)
        P.copy(sml[:, 0:24], p0[:, 0:24])
        P.act(sml[:, 16:24], sml[:, 16:24], AF.Exp)
        P.tt(sml[0:64, 24:32], ig, sml[0:64, 0:8], ALU.subtract)
        P.tt(sml[0:64, 32:40], sml[0:64, 24:32], sml[0:64, 8:16], ALU.add)
        P.act(sml[0:64, 40:48], sml[0:64, 32:40], AF.Exp)
        P.copy(lfb[:], V(gg[b].t[:, 8:16].unsqueeze(2).broadcast_to([64, NH, 128]), gg[b]))
        for h in range(NH):
            P.mm(psb[1][:, h * L:(h + 1) * L], lfb[:, h, :], triU[:], True, True, sig=(h == NH - 1))
        for h in range(NH):
            P.mm(psb[2][0:64, h * L:(h + 1) * L], kT[b][:, h, :], qT[b][:, h, :], True, True, sig=(h == NH - 1))
        P.copy(bts[:], psb[1][:, :])
        P.act(aT[:, :, :].re("p h t -> p (h t)"), bts[:], AF.Exp)
        P.tt(Eb[:], bts[0:64, :].re("p (h t) -> p h t", h=NH), V(negm.t[:, :].unsqueeze(1).broadcast_to([64, NH, L]), negm), ALU.add)
        for h in range(NH):
            P.act(Dm[:, h, :], Eb[:, h, :], AF.Exp, bias=sml[0:64, 24 + h:25 + h])
        P.tt(SD[:], psb[2][0:64, :].re("p (h t) -> p h t", h=NH), Dm[:], ALU.mult)
        P.tt(qs[:], qT[b][:], aT[:], ALU.mult, eng="pool")
        for h in range(NH):
            P.mm(p0[0:64, 24 + h:25 + h], SD[:, h, :], onesb[:], True, False, sig=False)
            P.mm(p0[0:64, 24 + h:25 + h], qs[:, h, :], nb[:, h:h + 1], False, True, sig=(h == NH - 1))
        P.ts(sml[0:64, 48:56], p0[0:64, 24:32], -1.0, None, ALU.mult)
        P.tt(sml[0:64, 48:56], sml[0:64, 48:56], p0[0:64, 24:32], ALU.max)
        P.ts(sml[0:64, 48:56], sml[0:64, 48:56], 1.0, None, ALU.max)
        P.op("dve", lambda e: e.reciprocal(out=sml.t[0:64, 56:64], in_=sml.t[0:64, 48:56]), [sml[:]], [sml[:]])
        for h in range(NH):
            pn = psb[3 + h // 2]
            o_ = (h % 2) * DV
            P.mm(pn[0:64, o_:o_ + DV], SD[:, h, :], vv[b][:, h, :], True, False, sig=False)
            P.mm(pn[0:64, o_:o_ + DV], qs[:, h, :], Cb[:, h, :], False, True)
            P.act(hh[:, h, :], pn[0:64, o_:o_ + DV], AF.Copy, scale=sml[0:64, 56 + h:57 + h])
            P.act(hsq[:], hh[:, h, :], AF.Square, accum=sml[0:64, 64 + h:65 + h])
        P.ts(sml[0:64, 72:80], sml[0:64, 64:72], 1.0 / DV, EPS, ALU.mult, ALU.add)
        P.act(sml[0:64, 80:88], sml[0:64, 72:80], AF.Sqrt)
        P.op("dve", lambda e: e.reciprocal(out=sml.t[0:64, 88:96], in_=sml.t[0:64, 80:88]), [sml[:]], [sml[:]])
        P.tt(gho[:], oo[b][:], ghead[:], ALU.mult, eng="pool")
        ob = outb[ci % 2]
        for h in range(NH):
            P.stt(ob[:, h * DV:(h + 1) * DV], hh[:, h, :], sml[0:64, 88 + h:89 + h], gho[:, h * DV:(h + 1) * DV], ALU.mult, ALU.mult)
        P.dma("sp", c.dq["xout"], OUTs[ci * L:(ci + 1) * L, :], ob[:])
        if ci == 0:
            P.dbg("sml", sml[:], [128, 96], F32)
            P.dbg("lfb", lfb[:], [64, NH, 128], F32)
            P.dbg("bts", bts[:], [128, NH * L], F32)
            P.dbg("Eb", Eb[:], [64, NH, L], F32)
            P.dbg("Dm", Dm[:], [64, NH, L], F32)
            P.dbg("SD", SD[:], [64, NH, L], BF16)
            P.dbg("aT", aT[:], [128, NH, L], F32)
            P.dbg("qs", qs[:], [128, NH, L], BF16)
            P.dbg("qT", qT[b][:], [128, NH, L], BF16)
            P.dbg("kT", kT[b][:], [128, NH, L], BF16)
            P.dbg("vv", vv[b][:], [64, NH, DV], BF16)
            P.dbg("hh", hh[:], [64, NH, DV], F32)
            P.dbg("negm", negm[:], [64, 64], F32)
            P.dbg("triU", triU[:], [64, 64], F32)
        for h in range(NH):
            P.ts(kw[:, h, :], ktm[b][:, h * DK:(h + 1) * DK], sml[0:64, 40 + h:41 + h], None, ALU.mult)
        for h in range(NH):
            P.mm(p0[:, 32 + h:33 + h], kw[:, h, :], onesb[:], True, True, sig=(h == NH - 1))
        for h in range(NH):
            pn = psb[3 + h // 2]
            o_ = (h % 2) * DV
            P.mm(pn[:, o_:o_ + DV], kw[:, h, :], vv[b][:, h, :], True, True)
            P.stt(Cst[:, h, :], Cst[:, h, :], sml[:, 16 + h:17 + h], pn[:, o_:o_ + DV], ALU.mult, ALU.add)
        P.tt(nst[:], nst[:], sml[:, 16:24], ALU.mult)
        P.tt(nst[:], nst[:], p0[:, 32:40], ALU.add)
        P.copy(Cb[:], Cst[:], eng="pool")
        P.copy(nb[:], nst[:])
    P.barrier(); P.es = P_es; es.close()

    es = ExitStack(); P_es = P.es; P.es = es
    wo = P.sb("c_wo", [128, KC, D], BF16)
    ot = P.sb("c_ot", [128, D], BF16)
    oT = P.sb("c_oT", [128, KC, 128], BF16)
    xr = [P.sb("c_x%d" % i, [128, D], F32) for i in range(2)]
    pst = [P.ps("c_pst%d" % i, [128, 1024], BF16) for i in range(0)]
    wov = io["ml_w_out"]
    for hk in range(4):
        P.dma("pool", c.dq["w0"], wo[:, hk * 4:(hk + 1) * 4, :],
              V(wov.t[0].rearrange("(k p) n -> p k n", p=128)[:, hk * 4:(hk + 1) * 4, :], wov))
    pbf = [V(psb[i].t[:, :].bitcast(BF16), psb[i]) for i in range(2)]
    for ti, r0 in enumerate(range(0, ntok, 128)):
        x_ = xr[ti % 2]
        P.dma("sp", c.dq["xin"], ot[:], OUTs[r0:r0 + 128, :])
        P.dma("sp", c.dq["xin"], x_[:], X[r0:r0 + 128, :])
        for q in range(2):
            for j in range(8):
                k = q * 8 + j
                P.tr(pbf[q][:, j * 128:(j + 1) * 128], ot[:, k * 128:(k + 1) * 128], c.identb[:], sig=(j == 7))
            P.copy(oT[:, q * 8:(q + 1) * 8, :], pbf[q][:, :].re("p (j t) -> p j t", j=8), eng="act")
        for dg in range(4):
            pp = psb[4 + dg]
            for k in range(KC):
                P.mm(pp[:, :], oT[:, k, :], wo[:, k, dg * 512:(dg + 1) * 512], start=(k == 0), stop=(k == KC - 1))
            P.tt(x_[:, dg * 512:(dg + 1) * 512], x_[:, dg * 512:(dg + 1) * 512], pp[:, :], ALU.add)
        P.dma("sp", c.dq["xout"], X[r0:r0 + 128, :], x_[:])
    P.barrier(); P.es = P_es; es.close()


import math


def frac_pm_half(P, out, a, itmp, ftmp):
    P.copy(itmp, a)
    P.copy(ftmp, itmp)
    P.tt(out, a, ftmp, ALU.subtract)
    P.ts(ftmp, out, 0.5, None, ALU.is_gt)
    P.tt(out, out, ftmp, ALU.subtract)
    P.ts(ftmp, out, -0.5, None, ALU.is_lt)
    P.tt(out, out, ftmp, ALU.add)


def stage_s5(P, c, X, io):
    ntok = c.ntok
    KC = D // 128
    psb = c.psb
    NT = ntok // 512
    HT = P.dram("s5_HT", [D, ntok], F32)
    GT = P.dram("s5_GT", [D, ntok], BF16)
    PRM = P.dram("s5_PRM", [6, 128, 64], F32)
    es = ExitStack(); P_es = P.es; P.es = es
    xt = P.sb("sa_xt", [128, D], F32)
    h32 = P.sb("sa_h32", [128, D], F32)
    sq = P.sb("sa_sq", [128, D], F32)
    stat = P.sb("sa_stat", [128, 8], F32)
    hT = [P.sb("sa_hT%d" % i, [128, KC, 128], F32) for i in range(2)]
    P.dma("sp", c.dq["misc"], c.gvec[:], V(io["norm_mix"].t[0:1, :].partition_broadcast(128), io["norm_mix"]))
    for ti, r0 in enumerate(range(0, ntok, 128)):
        P.dma("sp", c.dq["xin"], xt[:], X[r0:r0 + 128, :])
        rms_h(P, c, xt[:], h32[:], sq[:], stat[:])
        hb = hT[ti % 2]
        for q in range(4):
            for j in range(4):
                k = q * 4 + j
                P.tr(psb[q][:, j * 128:(j + 1) * 128], h32[:, k * 128:(k + 1) * 128], c.ident[:], sig=(j == 3))
            P.copy(hb[:, q * 4:(q + 1) * 4, :], psb[q][:, :].re("p (j t) -> p j t", j=4), eng="act")
        P.dma("sp", c.dq["xout"], V(HT.t[:, r0:r0 + 128].rearrange("(k p) t -> p k t", p=128), HT), hb[:])
    lr = P.sb("sa_lr", [128, 64], F32); li = P.sb("sa_li", [128, 64], F32); ldt = P.sb("sa_ldt", [128, 1], F32)
    w = [P.sb("sa_w%d" % i, [128, 64], F32) for i in range(12)]
    cpi = P.sb("sa_cpi", [128, 1], F32)
    P.memset(cpi[:], -math.pi)
    P.dma("sp", c.dq["misc"], lr[:], io["s5_lam_re"][:, :])
    P.dma("sp", c.dq["misc"], li[:], io["s5_lam_im"][:, :])
    P.dma("sp", c.dq["misc"], ldt[:], io["s5_log_dt"][:, :])
    P.act(ldt[:], ldt[:], AF.Exp)
    P.ts(w[0][:], lr[:], ldt[:, 0:1], None, ALU.mult)
    P.ts(w[1][:], li[:], ldt[:, 0:1], None, ALU.mult)
    P.act(w[2][:], w[0][:], AF.Exp)
    wi_ = P.sb("sa_wi", [128, 64], mybir.dt.int32)
    P.ts(w[3][:], w[1][:], 1.0 / (2 * math.pi), None, ALU.mult)
    frac_pm_half(P, w[3][:], w[3][:], wi_[:], w[4][:])
    P.ts(w[4][:], w[3][:], 0.25, None, ALU.add)
    frac_pm_half(P, w[4][:], w[4][:], wi_[:], w[5][:])
    P.act(w[6][:], w[4][:], AF.Sin, scale=2 * math.pi)
    P.act(w[5][:], w[3][:], AF.Sin, scale=2 * math.pi)
    P.tt(w[7][:], w[2][:], w[6][:], ALU.mult)
    P.tt(w[8][:], w[2][:], w[5][:], ALU.mult)
    P.ts(w[7][:], w[7][:], -1.0, None, ALU.add)
    P.tt(w[9][:], lr[:], lr[:], ALU.mult)
    P.tt(w[10][:], li[:], li[:], ALU.mult)
    P.tt(w[9][:], w[9][:], w[10][:], ALU.add)
    P.op("dve", lambda e: e.reciprocal(out=w[9].t[:], in_=w[9].t[:]), [w[9][:]], [w[9][:]])
    P.tt(w[10][:], w[7][:], lr[:], ALU.mult)
    P.tt(w[11][:], w[8][:], li[:], ALU.mult)
    P.tt(w[10][:], w[10][:], w[11][:], ALU.add)
    P.tt(w[10][:], w[10][:], w[9][:], ALU.mult)
    P.tt(w[11][:], w[8][:], lr[:], ALU.mult)
    P.tt(w[0][:], w[7][:], li[:], ALU.mult)
    P.tt(w[11][:], w[11][:], w[0][:], ALU.subtract)
    P.tt(w[11][:], w[11][:], w[9][:], ALU.mult)
    P.dma("sp", c.dq["xout"], PRM[0], w[10][:])
    P.dma("sp", c.dq["xout"], PRM[1], w[11][:])
    P.dma("sp", c.dq["xout"], PRM[2], w[2][:])
    P.dma("sp", c.dq["xout"], PRM[3], w[3][:])
    P.barrier(); P.es = P_es; es.close()

    es = ExitStack(); P_es = P.es; P.es = es
    iot = P.sb("sb_iota", [64, ntok], F32)
    P.op("pool", lambda e: e.iota(iot.t[:], pattern=[[1, ntok]], base=0, channel_multiplier=0,
                                  allow_small_or_imprecise_dtypes=True), [], [iot[:]])
    cpi = P.sb("sb_cpi", [64, 1], F32)
    P.memset(cpi[:], -math.pi)
    magT = P.sb("sb_magT", [64, 128], F32)
    phiT = P.sb("sb_phiT", [64, 128], F32)
    P.dma("sp", c.dq["misc"], magT[:], V(PRM.t[2].rearrange("g p -> p g"), PRM), allow_slow_non_contiguous=True)
    P.dma("sp", c.dq["misc"], phiT[:], V(PRM.t[3].rearrange("g p -> p g"), PRM), allow_slow_non_contiguous=True)
    dT = P.sb("sb_dT", [128, KC], F32)
    P.dma("sp", c.dq["misc"], dT[:], io["s5_dT"][:, :])
    u = P.sb("sb_u", [128, ntok], F32)
    ysb = P.sb("sb_y", [128, ntok], F32)
    gb = P.sb("sb_gb", [128, ntok], BF16)
    NBF = 2
    bTr = [P.sb("sb_bTr%d" % i, [128, 64], F32) for i in range(NBF)]
    bTi = [P.sb("sb_bTi%d" % i, [128, 64], F32) for i in range(NBF)]
    Qr = [P.sb("sb_Qr%d" % i, [128, 64], F32) for i in range(NBF)]
    Qi = [P.sb("sb_Qi%d" % i, [128, 64], F32) for i in range(NBF)]
    cTr = [P.sb("sb_cTr%d" % i, [64, 128], F32) for i in range(NBF)]
    cTi = [P.sb("sb_cTi%d" % i, [64, 128], F32) for i in range(NBF)]
    Br = P.sb("sb_Br", [128, 64], F32); Bi = P.sb("sb_Bi", [128, 64], F32); t1 = P.sb("sb_t1", [128, 64], F32)
    gsem = [P.dma_sem("s5g%d" % i) for i in range(NBF)]
    bur = P.sb("sb_bur", [64, ntok], F32); bui = P.sb("sb_bui", [64, ntok], F32)
    Cn = P.sb("sb_Cn", [64, ntok], F32); Sn = P.sb("sb_Sn", [64, ntok], F32)
    ta = P.sb("sb_ta", [64, ntok], F32); tb = P.sb("sb_tb", [64, ntok], F32)
    mr = P.sb("sb_mr", [64, ntok], F32); mi = P.sb("sb_mi", [64, ntok], F32)
    tint_v = V(mi.t[:, :].bitcast(mybir.dt.int32), mi)

    def load_group(g):
        b = g % NBF
        P.dma("sp", gsem[b], bTr[b][:], io["s5_bT_re"][g])
        P.dma("sp", gsem[b], bTi[b][:], io["s5_bT_im"][g])
        P.dma("sp", gsem[b], cTr[b][:], io["s5_cT_re"][g])
        P.dma("sp", gsem[b], cTi[b][:], io["s5_cT_im"][g])
        P.dma("sp", gsem[b], Qr[b][:], V(PRM.t[0, g:g + 1, :].partition_broadcast(128), PRM))
        P.dma("sp", gsem[b], Qi[b][:], V(PRM.t[1, g:g + 1, :].partition_broadcast(128), PRM))

    load_group(0)
    for fc in range(KC):
        P.dma("sp", c.dq["xin"], u[:], HT[fc * 128:(fc + 1) * 128, :])
        for gi in range(8):
            g = fc * 8 + gi
            b = g % NBF
            if g + 1 < 128:
                load_group(g + 1)
            P.tt(Br[:], bTr[b][:], Qr[b][:], ALU.mult)
            P.tt(t1[:], bTi[b][:], Qi[b][:], ALU.mult)
            P.tt(Br[:], Br[:], t1[:], ALU.subtract)
            P.tt(Bi[:], bTi[b][:], Qr[b][:], ALU.mult)
            P.tt(t1[:], bTr[b][:], Qi[b][:], ALU.mult)
            P.tt(Bi[:], Bi[:], t1[:], ALU.add)
            P.ts(cTi[b][:], cTi[b][:], -1.0, None, ALU.mult)
            for tbk in range(NT):
                sl = slice(tbk * 512, (tbk + 1) * 512)
                P.mm(psb[tbk % 2][0:64, :], Br[:], u[:, sl], True, True)
                P.mm(psb[2 + tbk % 2][0:64, :], Bi[:], u[:, sl], True, True)
                P.copy(bur[:, sl], psb[tbk % 2][0:64, :], eng="act")
                P.copy(bui[:, sl], psb[2 + tbk % 2][0:64, :], eng="act")
            P.ts(ta[:], iot[:], phiT[:, g:g + 1], None, ALU.mult)
            frac_pm_half(P, ta[:], ta[:], tint_v, mr[:])
            P.ts(tb[:], ta[:], 0.25, None, ALU.add)
            frac_pm_half(P, tb[:], tb[:], tint_v, mr[:])
            P.act(Sn[:], ta[:], AF.Sin, scale=2 * math.pi)
            P.act(Cn[:], tb[:], AF.Sin, scale=2 * math.pi)
            P.tt(mr[:], Cn[:], bur[:], ALU.mult)
            P.tt(ta[:], Sn[:], bui[:], ALU.mult, eng="pool")
            P.tt(mr[:], mr[:], ta[:], ALU.add)
            P.tt(mi[:], Cn[:], bui[:], ALU.mult)
            P.tt(tb[:], Sn[:], bur[:], ALU.mult, eng="pool")
            P.tt(mi[:], mi[:], tb[:], ALU.subtract)
            dec = V(magT.t[:, g:g + 1].broadcast_to([64, ntok]), magT)
            P.op("dve", lambda e: e.tensor_tensor_scan(out=bur.t[:], data0=dec.ap, data1=mr.t[:], initial=0.0,
                                                       op0=ALU.mult, op1=ALU.add), [dec, mr[:]], [bur[:]])
            P.op("dve", lambda e: e.tensor_tensor_scan(out=bui.t[:], data0=dec.ap, data1=mi.t[:], initial=0.0,
                                                       op0=ALU.mult, op1=ALU.add), [dec, mi[:]], [bui[:]])
            P.tt(mr[:], Cn[:], bur[:], ALU.mult)
            P.tt(ta[:], Sn[:], bui[:], ALU.mult, eng="pool")
            P.tt(mr[:], mr[:], ta[:], ALU.subtract)
            P.tt(mi[:], Cn[:], bui[:], ALU.mult)
            P.tt(tb[:], Sn[:], bur[:], ALU.mult, eng="pool")
            P.tt(mi[:], mi[:], tb[:], ALU.add)
            for tbk in range(NT):
                sl = slice(tbk * 512, (tbk + 1) * 512)
                pp = psb[4 + tbk % 4]
                P.mm(pp[:, :], cTr[b][:], mr[:, sl], True, False, sig=False)
                P.mm(pp[:, :], cTi[b][:], mi[:, sl], False, True)
                if gi == 0:
                    P.copy(ysb[:, sl], pp[:, :])
                else:
                    P.tt(ysb[:, sl], ysb[:, sl], pp[:, :], ALU.add)
        P.stt(ysb[:], u[:], dT[:, fc:fc + 1], ysb[:], ALU.mult, ALU.add)
        P.act(gb[:], ysb[:], AF.Gelu)
        P.dma("sp", c.dq["xout"], GT[fc * 128:(fc + 1) * 128, :], gb[:])
    P.barrier(); P.es = P_es; es.close()

    es = ExitStack(); P_es = P.es; P.es = es
    gT = P.sb("sc_gT", [128, KC, 512], BF16)
    wv = [P.sb("sc_wv%d" % i, [128, KC, 512], BF16) for i in range(2)]
    wg = [P.sb("sc_wg%d" % i, [128, KC, 512], BF16) for i in range(2)]
    xr = P.sb("sc_x", [128, 4, D], F32)
    sg = [P.sb("sc_sg%d" % i, [128, 512], F32) for i in range(2)]
    wgl = io["s5_w_glu"]
    wsem = [c.dq["w0"], c.dq["w1"]]
    n = 0
    for t0 in range(0, ntok, 512):
        P.dma("sp", c.dq["xin"], gT[:], V(GT.t[:, t0:t0 + 512].rearrange("(k p) t -> p k t", p=128), GT))
        for s in range(4):
            P.dma("sp", c.dq["xin"], xr[:, s, :], X[t0 + s * 128:t0 + (s + 1) * 128, :])
        for j in range(4):
            b = n % 2
            n += 1
            for hk in range(2):
                P.dma("pool", wsem[b], wv[b][:, hk * 8:(hk + 1) * 8, :],
                      V(wgl.t[0, :, j * 512:(j + 1) * 512].rearrange("(k p) n -> p k n", p=128)[:, hk * 8:(hk + 1) * 8, :], wgl))
                P.dma("pool", wsem[b], wg[b][:, hk * 8:(hk + 1) * 8, :],
                      V(wgl.t[0, :, D + j * 512:D + (j + 1) * 512].rearrange("(k p) n -> p k n", p=128)[:, hk * 8:(hk + 1) * 8, :], wgl))
            for s in range(4):
                pv = psb[(s % 2) * 2]
                pg = psb[(s % 2) * 2 + 1]
                for k in range(KC):
                    P.mm(pv[:, :], gT[:, k, s * 128:(s + 1) * 128], wv[b][:, k, :], start=(k == 0), stop=(k == KC - 1))
                for k in range(KC):
                    P.mm(pg[:, :], gT[:, k, s * 128:(s + 1) * 128], wg[b][:, k, :], start=(k == 0), stop=(k == KC - 1))
                P.act(sg[s % 2][:], pg[:, :], AF.Sigmoid)
                P.tt(sg[s % 2][:], sg[s % 2][:], pv[:, :], ALU.mult)
                P.tt(xr[:, s, j * 512:(j + 1) * 512], xr[:, s, j * 512:(j + 1) * 512], sg[s % 2][:], ALU.add)
        for s in range(4):
            P.dma("sp", c.dq["xout"], X[t0 + s * 128:t0 + (s + 1) * 128, :], xr[:, s, :])
    P.barrier(); P.es = P_es; es.close()


def stage_final(P, c, X, io, out):
    es = ExitStack()
    P_es = P.es
    P.es = es
    xt = [P.sb("f_x%d" % i, [128, D], F32) for i in range(2)]
    ht = [P.sb("f_h%d" % i, [128, D], F32) for i in range(2)]
    sq = P.sb("f_sq", [128, D], F32)
    stat = P.sb("f_stat", [128, 8], F32)
    P.dma("sp", c.dq["misc"], c.gvec[:], V(io["norm_final"].t.unsqueeze(0).partition_broadcast(128), io["norm_final"]))
    for i, r0 in enumerate(range(0, c.ntok, 128)):
        P.dma("sp", c.dq["xin"], xt[i % 2][:], X[r0:r0 + 128, :])
        rms_h(P, c, xt[i % 2][:], ht[i % 2][:], sq[:], stat[:])
        P.dma("sp", c.dq["xout"], out[r0:r0 + 128, :], ht[i % 2][:])
    P.es = P_es
    return es


IN_SHAPES = {
    "x": [4096, D],
    "norm_ffn": [2, D], "norm_mix": [2, D], "norm_final": [D],
    "moe_w_group": [2, D, 8], "moe_b_group": [2, 8], "moe_w_expert": [2, D, 64], "moe_b_expert": [2, 64],
    "moe_w_gu": [2, NE, D, 2 * FH], "moe_w_down": [2, NE, FH, D],
    "moe_wg_r": [2 * NE * 128, 16 * FH], "moe_wu_r": [2 * NE * 128, 16 * FH], "moe_wd_r": [2 * NE * 128, 6 * D],
    "s5_lam_re": [128, 64], "s5_lam_im": [128, 64], "s5_log_dt": [128, 1], "s5_dT": [128, 16],
    "s5_bT_re": [128, 128, 64], "s5_bT_im": [128, 128, 64], "s5_cT_re": [128, 64, 128], "s5_cT_im": [128, 64, 128],
    "s5_w_glu": [1, D, 2 * D],
    "ml_w_in": [1, D, 6160], "ml_b_gate": [1, 16], "ml_g_head": [1, D], "ml_w_out": [1, D, D],
}


def build(stages=("moe0",), ntok=4096, in_names=None):
    nc = bass.Bass("TRN2", target_bir_lowering=False)
    with ExitStack() as es:
        P = Prog(nc, es)
        io = {}
        for k in (in_names or IN_SHAPES.keys()):
            shp = list(IN_SHAPES[k])
            if k == "x":
                shp[0] = ntok
            if DBG_NE and k in ("moe_w_gu", "moe_w_down"):
                shp[1] = DBG_NE
            io[k] = P.dram(k, shp, F32, kind="ExternalInput")
        out = P.dram("out", [ntok, D], F32, kind="ExternalOutput")
        X = P.dram("Xs", [ntok, D], F32)
        c = setup_common(P, ntok)
        P.dma("sp", c.dq["misc"], X[:, :], io["x"][:, :])
        for st in stages:
            if st == "moe0":
                stage_moe(P, c, X, 0, io).close()
            elif st == "moe1":
                stage_moe(P, c, X, 1, io).close()
            elif st == "mlstm":
                stage_mlstm(P, c, X, io)
            elif st == "moeg0":
                stage_moe_g(P, c, X, 0, io)
            elif st == "moeg1":
                stage_moe_g(P, c, X, 1, io)
            elif st == "s5":
                stage_s5(P, c, X, io)
        stage_final(P, c, X, io, out).close()
        P.finish()
    return nc


def moe_layout(inp):
    o = {}
    wg = inp["moe_w_gu"].reshape(2, NE, 16, 128, 2, FH)
    o["moe_wg_r"] = np.ascontiguousarray(wg[:, :, :, :, 0, :].transpose(0, 1, 3, 2, 4)).reshape(2 * NE * 128, 16 * FH)
    o["moe_wu_r"] = np.ascontiguousarray(wg[:, :, :, :, 1, :].transpose(0, 1, 3, 2, 4)).reshape(2 * NE * 128, 16 * FH)
    wd = inp["moe_w_down"].reshape(2, NE, 6, 128, D)
    o["moe_wd_r"] = np.ascontiguousarray(wd.transpose(0, 1, 3, 2, 4)).reshape(2 * NE * 128, 6 * D)
    return o


def s5_layout(inp):
    G, PS, CG = 128, 64, 16
    o = {}
    o["s5_lam_re"] = np.ascontiguousarray(inp["s5_lam_re"][0])
    o["s5_lam_im"] = np.ascontiguousarray(inp["s5_lam_im"][0])
    o["s5_log_dt"] = np.ascontiguousarray(inp["s5_log_dt"][0].reshape(G, 1))
    o["s5_dT"] = np.ascontiguousarray(inp["s5_d"][0].reshape(16, 128).T)
    for nm, src in (("s5_bT_re", "s5_b_re"), ("s5_bT_im", "s5_b_im")):
        a = np.zeros((G, 128, PS), np.float32)
        bt = np.transpose(inp[src][0], (0, 2, 1))
        for g in range(G):
            gi = g % 8
            a[g, gi * CG:(gi + 1) * CG, :] = bt[g]
        o[nm] = a
    for nm, src in (("s5_cT_re", "s5_c_re"), ("s5_cT_im", "s5_c_im")):
        a = np.zeros((G, PS, 128), np.float32)
        ct = np.transpose(inp[src][0], (0, 2, 1))
        for g in range(G):
            gi = g % 8
            a[g, :, gi * CG:(gi + 1) * CG] = ct[g]
        o[nm] = a
    o["s5_w_glu"] = inp["s5_w_glu"]
    return o


_NC_CACHE = {}


def kernel(**inputs):
    inp = {k: np.asarray(v) for k, v in inputs.items()}
    B = inp["x"].shape[0]
    stages = ("s5", "moeg0", "mlstm", "moeg1")
    shared = s5_layout(inp)
    shared.update(moe_layout(inp))
    for k in ["norm_ffn", "norm_mix", "norm_final", "moe_w_group", "moe_b_group", "moe_w_expert", "moe_b_expert",
              "ml_w_in", "ml_b_gate", "ml_g_head", "ml_w_out"]:
        shared[k] = np.ascontiguousarray(inp[k])
    names = ["x"] + list(shared.keys())
    if "nc" not in _NC_CACHE:
        _NC_CACHE["nc"] = build(stages=stages, ntok=inp["x"].shape[1], in_names=names)
    nc = _NC_CACHE["nc"]
    in_maps = []
    for b in range(B):
        m = dict(shared)
        m["x"] = np.ascontiguousarray(inp["x"][b])
        in_maps.append(m)
    res = run_bass_kernel_spmd(nc, in_maps, core_ids=list(range(B)))
    return np.stack([np.asarray(r["out"]) for r in res.results], axis=0).astype(np.float32)
```

```python
from contextlib import ExitStack
import numpy as np
import concourse.bass as bass
import concourse.mybir as mybir
from concourse.bass_utils import run_bass_kernel_spmd

F32 = mybir.dt.float32
BF16 = mybir.dt.bfloat16
AF = mybir.ActivationFunctionType
ALU = mybir.AluOpType
AX = mybir.AxisListType

D = 2048
EPS = 1e-6
NE = 64
import os
DBG_CUT = int(os.environ.get('DBG_CUT', '0'))
DBG_OUT = int(os.environ.get('DBG_OUT', '0'))
DBG_NE = int(os.environ.get('DBG_NE', '0'))
FH = 768


class Buf:
    def __init__(self, t, name):
        self.t = t
        self.name = name
        self.w = None
        self.r = []

    def __getitem__(self, k):
        return V(self.t[k], self)


class V:
    def __init__(self, ap, buf):
        self.ap = ap
        self.buf = buf

    def re(self, s, **kw):
        return V(self.ap.rearrange(s, **kw), self.buf)

    def __getitem__(self, k):
        return V(self.ap[k], self.buf)


class Prog:
    def __init__(self, nc, es):
        self.nc = nc
        self.es = es
        self.eng = {"pe": nc.tensor, "act": nc.scalar, "dve": nc.vector, "pool": nc.gpsimd, "sp": nc.sync}
        self.sem = {}
        self.cnt = {}
        self.waited = {}
        for k in self.eng:
            self.sem[k] = es.enter_context(nc.semaphore("s_" + k))
            self.cnt[k] = 0
        self.ndma = 0

    def dma_sem(self, name):
        key = "d_" + name
        self.sem[key] = self.es.enter_context(self.nc.semaphore(key))
        self.cnt[key] = 0
        return key

    def sb(self, name, shape, dt):
        return Buf(self.es.enter_context(self.nc.sbuf_tensor(name, list(shape), dt)), name)

    def ps(self, name, shape, dt=F32):
        return Buf(self.es.enter_context(self.nc.psum_tensor(name, list(shape), dt)), name)

    def dram(self, name, shape, dt, kind="Internal"):
        if DBG_OUT and kind == "Internal" and name != "Xs":
            kind = "ExternalOutput"
        t = self.nc.dram_tensor(name, list(shape), dt, kind=kind)
        return Buf(t.ap(), name)

    def _deps(self, reads, writes):
        deps = {}
        def add(d):
            if d is None:
                return
            k, s = d
            if deps.get(k, 0) < s:
                deps[k] = s
        for v in reads:
            add(v.buf.w)
        for v in writes:
            add(v.buf.w)
            for d in v.buf.r:
                add(d)
        return deps

    def _wait(self, ek, deps):
        e = self.eng[ek]
        for k, s in deps.items():
            if k == ek and ek in ("pe", "sp"):
                continue
            if self.waited.get((ek, k), 0) >= s:
                continue
            mult = 1
            if k.startswith("d_"):
                mult = 16
                s = self.cnt[k]
            e.wait_ge(self.sem[k], s * mult)
            self.waited[(ek, k)] = s

    def _record(self, key, seq, reads, writes):
        for v in reads:
            v.buf.r.append((key, seq))
            if len(v.buf.r) > 64:
                m = {}
                for k, s in v.buf.r:
                    if m.get(k, 0) < s:
                        m[k] = s
                v.buf.r = list(m.items())
        for v in writes:
            v.buf.w = (key, seq)
            v.buf.r = []

    def op(self, ek, fn, reads, writes, sig=True):
        self._wait(ek, self._deps(reads, writes))
        ins = fn(self.eng[ek])
        seq = self.cnt[ek] + 1
        if sig:
            ins.then_inc(self.sem[ek], 1)
            self.cnt[ek] = seq
        self._record(ek, seq, reads, writes)
        return ins

    def dma(self, qk, dkey, out, in_, **kw):
        self._wait(qk, self._deps([in_], [out]))
        ins = self.eng[qk].dma_start(out=out.ap, in_=in_.ap, **kw)
        ins.then_inc(self.sem[dkey], 16)
        self.cnt[dkey] += 1
        self._record(dkey, self.cnt[dkey], [in_], [out])
        self.ndma += 1
        return ins

    def mm(self, out, lhsT, rhs, start, stop, sig=None):
        if sig is None:
            sig = stop
        return self.op("pe", lambda e: e.matmul(out.ap, lhsT.ap, rhs.ap, start=start, stop=stop),
                       [lhsT, rhs], [out], sig=sig)

    def tr(self, out, in_, ident, sig=True):
        return self.op("pe", lambda e: e.transpose(out.ap, in_.ap, ident.ap), [in_, ident], [out], sig=sig)

    def act(self, out, in_, func, bias=None, scale=None, accum=None, eng="act"):
        kw = {}
        reads = [in_]
        writes = [out]
        if bias is not None:
            if isinstance(bias, V):
                kw["bias"] = bias.ap
                reads.append(bias)
            else:
                kw["bias"] = bias
        if scale is not None:
            if isinstance(scale, V):
                kw["scale"] = scale.ap
                reads.append(scale)
            else:
                kw["scale"] = scale
        if accum is not None:
            kw["accum_out"] = accum.ap
            writes.append(accum)
        return self.op(eng, lambda e: e.activation(out=out.ap, in_=in_.ap, func=func, **kw), reads, writes)

    def ts(self, out, in0, s1, s2, op0, op1=None, eng="dve", accum=None):
        reads = [in0]
        writes = [out]
        a1 = s1
        a2 = s2
        if isinstance(s1, V):
            reads.append(s1)
            a1 = s1.ap
        if isinstance(s2, V):
            reads.append(s2)
            a2 = s2.ap
        kw = {}
        if op1 is not None:
            kw["op1"] = op1
        if accum is not None:
            kw["accum_out"] = accum.ap
            writes.append(accum)
        return self.op(eng, lambda e: e.tensor_scalar(out=out.ap, in0=in0.ap, scalar1=a1, scalar2=a2, op0=op0, **kw),
                       reads, writes)

    def tt(self, out, in0, in1, op, eng="dve"):
        return self.op(eng, lambda e: e.tensor_tensor(out=out.ap, in0=in0.ap, in1=in1.ap, op=op), [in0, in1], [out])

    def stt(self, out, in0, scalar, in1, op0, op1, eng="dve"):
        reads = [in0, in1]
        a = scalar
        if isinstance(scalar, V):
            reads.append(scalar)
            a = scalar.ap
        return self.op(eng, lambda e: e.scalar_tensor_tensor(out=out.ap, in0=in0.ap, scalar=a, in1=in1.ap,
                                                              op0=op0, op1=op1), reads, [out])

    def copy(self, out, in_, eng="dve"):
        if eng == "act":
            return self.op("act", lambda e: e.copy(out=out.ap, in_=in_.ap), [in_], [out])
        return self.op(eng, lambda e: e.tensor_copy(out=out.ap, in_=in_.ap), [in_], [out])

    def memset(self, out, val, eng="dve"):
        return self.op(eng, lambda e: e.memset(out.ap, val), [], [out])

    def red(self, out, in_, op, eng="dve"):
        return self.op(eng, lambda e: e.tensor_reduce(out=out.ap, in_=in_.ap, axis=AX.X, op=op), [in_], [out])

    def dbg(self, name, v, shape, dt):
        if not DBG_OUT:
            return
        if not hasattr(self, "_dq"):
            self._dq = self.dma_sem("dbg")
        t = self.nc.dram_tensor("dbg_" + name, list(shape), dt, kind="ExternalOutput")
        self.dma("sp", self._dq, V(t.ap(), Buf(t.ap(), name)), v)

    def barrier(self):
        for ek, e in self.eng.items():
            for k, c in self.cnt.items():
                if c > 0 and k != ek and self.waited.get((ek, k), 0) < c:
                    e.wait_ge(self.sem[k], c * (16 if k.startswith("d_") else 1))
                    self.waited[(ek, k)] = c

    def finish(self):
        e = self.eng["sp"]
        for k, c in self.cnt.items():
            if c > 0 and k != "sp":
                e.wait_ge(self.sem[k], c * (16 if k.startswith("d_") else 1))


class Ctx:
    pass


def setup_common(P, ntok):
    c = Ctx()
    c.ntok = ntok
    c.ident = P.sb("ident", [128, 128], F32)
    c.identb = P.sb("identb", [128, 128], BF16)
    nc = P.nc
    P.memset(c.ident[:], 1.0, eng="pool")
    P.op("pool", lambda e: e.affine_select(out=c.ident.t[:], in_=c.ident.t[:], pattern=[[-1, 128]],
                                           compare_op=ALU.is_equal, fill=0.0, base=0, channel_multiplier=1),
         [c.ident[:]], [c.ident[:]])
    P.copy(c.identb[:], c.ident[:], eng="pool")
    c.psb = [P.ps("psb%d" % i, [128, 512], F32) for i in range(8)]
    c.gvec = P.sb("gvec", [128, D], F32)
    c.dq = {k: P.dma_sem(k) for k in ["misc", "xin", "xout", "w0", "w1", "w2", "w3"]}
    return c


def rms_h(P, c, xt, ht, tmp, stat):
    P.act(tmp, xt, AF.Square, accum=stat[:, 0:1])
    P.ts(stat[:, 1:2], stat[:, 0:1], 1.0 / D, EPS, ALU.mult, ALU.add)
    P.act(stat[:, 3:4], stat[:, 1:2], AF.Sqrt)
    P.op("dve", lambda e: e.reciprocal(out=stat.buf.t[:, 2:3], in_=stat.buf.t[:, 3:4]), [stat], [stat])
    P.stt(ht, xt, stat[:, 2:3], c.gvec[:], ALU.mult, ALU.mult)


def stage_moe(P, c, X, layer, io, TT=512):
    ntok = c.ntok
    nsub = TT // 128
    KC = D // 128
    es = ExitStack()
    P_es = P.es
    P.es = es
    _sb0 = P.sb
    P.sb = lambda name, shape, dt: _sb0("%s_l%d" % (name, layer), shape, dt)
    acc = P.sb("m_acc", [128, nsub, D], F32)
    h32 = P.sb("m_h32", [128, D], F32)
    sq = P.sb("m_sq", [128, D], F32)
    stat = P.sb("m_stat", [128, 8], F32)
    hT32 = P.sb("m_hT32", [128, KC, 128], F32)
    hTb = P.sb("m_hTb", [128, KC, TT], BF16)
    actT = P.sb("m_actT", [128, 6, TT], BF16)
    sil = [P.sb("m_sil%d" % i, [128, TT], F32) for i in range(2)]
    wr = P.sb("m_wr", [128, KC, 72], F32)
    br = P.sb("m_br", [128, 72], F32)
    G = P.sb("m_G", [128, nsub, NE], F32)
    lg = P.sb("m_lg", [128, 72], F32)
    lem = P.sb("m_lem", [128, NE], F32)
    lem2 = P.sb("m_lem2", [128, NE], F32)
    msk = P.sb("m_msk", [128, NE], F32)
    sm = P.sb("m_sm", [128, 16], F32)
    wslot = [P.sb("m_w%d" % i, [128, 6 * D], BF16) for i in range(4)]
    wq = ["w0", "w1", "w2", "w3"]
    psb = c.psb

    P.dma("sp", c.dq["misc"], c.gvec[:], V(io["norm_ffn"].t[layer:layer + 1, :].partition_broadcast(128), io["norm_ffn"]))
    P.dma("sp", c.dq["misc"], wr[:, :, 0:8], V(io["moe_w_group"].t[layer].rearrange("(k p) n -> p k n", p=128), io["moe_w_group"]))
    P.dma("sp", c.dq["misc"], wr[:, :, 8:72], V(io["moe_w_expert"].t[layer].rearrange("(k p) n -> p k n", p=128), io["moe_w_expert"]))
    P.dma("sp", c.dq["misc"], br[:, 0:8], V(io["moe_b_group"].t[layer:layer + 1, :].partition_broadcast(128), io["moe_b_group"]))
    P.dma("sp", c.dq["misc"], br[:, 8:72], V(io["moe_b_expert"].t[layer:layer + 1, :].partition_broadcast(128), io["moe_b_expert"]))

    wgu = io.get("moe_w_gu")
    wdn = io.get("moe_w_down")
    slot_i = [0]

    def load_piece(e, piece):
        s = slot_i[0] % 4
        slot_i[0] += 1
        dst = wslot[s]
        if piece < 2:
            src = wgu.t[layer, e, :, piece * FH:(piece + 1) * FH].rearrange("(k p) f -> p k f", p=128)
            dv = dst[:, :].re("p (k f) -> p k f", k=KC)
            for hk in range(2):
                P.dma("pool", c.dq[wq[s]], dv[:, hk * 8:(hk + 1) * 8, :], V(src[:, hk * 8:(hk + 1) * 8, :], wgu))
        else:
            src = wdn.t[layer, e].rearrange("(k p) d -> p k d", p=128)
            dv = dst[:, :].re("p (k d) -> p k d", k=6)
            for hk in range(2):
                P.dma("pool", c.dq[wq[s]], dv[:, hk * 3:(hk + 1) * 3, :], V(src[:, hk * 3:(hk + 1) * 3, :], wdn))
        return dst

    for t0 in range(0, ntok, TT):
        for s in range(nsub):
            r0 = t0 + s * 128
            P.dma("sp", c.dq["xin"], acc[:, s, :], X[r0:r0 + 128, :])
            rms_h(P, c, acc[:, s, :], h32[:], sq[:], stat[:])
            if DBG_CUT == 2:
                continue
            for q in range(4):
                for j in range(4):
                    k = q * 4 + j
                    P.tr(psb[q][:, j * 128:(j + 1) * 128], h32[:, k * 128:(k + 1) * 128], c.ident[:], sig=(j == 3))
                if DBG_CUT == 6:
                    continue
                P.copy(hT32[:, q * 4:(q + 1) * 4, :], psb[q][:, :].re("p (j t) -> p j t", j=4), eng="act")
                if DBG_CUT == 7:
                    continue
                P.copy(hTb[:, q * 4:(q + 1) * 4, s * 128:(s + 1) * 128], hT32[:, q * 4:(q + 1) * 4, :], eng="dve")
            if DBG_CUT in (3, 6, 7):
                continue
            for k in range(KC):
                P.mm(psb[4][:, 0:72], hT32[:, k, :], wr[:, k, :], start=(k == 0), stop=(k == KC - 1))
            if DBG_CUT == 4:
                continue
            P.tt(lg[:], psb[4][:, 0:72], br[:], ALU.add)
            P.red(sm[:, 0:1], lg[:, 0:8], ALU.max)
            P.ts(sm[:, 1:2], sm[:, 0:1], -1.0, None, ALU.mult)
            P.act(msk[:, 0:8], lg[:, 0:8], AF.Exp, bias=sm[:, 1:2], accum=sm[:, 2:3])
            P.op("dve", lambda e: e.reciprocal(out=sm.t[:, 3:4], in_=sm.t[:, 2:3]), [sm[:]], [sm[:]])
            P.ts(msk[:, 8:16], lg[:, 0:8], sm[:, 0:1], None, ALU.is_ge)
            P.ts(msk[:, 8:16], msk[:, 8:16], 1e30, -1e30, ALU.mult, ALU.add)
            P.tt(lem[:, :].re("p (g e) -> p g e", g=8), lg[:, 8:72].re("p (g e) -> p g e", g=8),
                 V(msk.t[:, 8:16].unsqueeze(2).broadcast_to([128, 8, 8]), msk), ALU.add)
            if DBG_CUT == 5:
                continue
            P.red(sm[:, 4:5], lem[:], ALU.max)
            P.ts(msk[:], lem[:], sm[:, 4:5], None, ALU.is_ge)
            P.stt(lem2[:], msk[:], -1e30, lem[:], ALU.mult, ALU.add)
            P.red(sm[:, 5:6], lem2[:], ALU.max)
            P.ts(lem2[:], lem2[:], sm[:, 5:6], None, ALU.is_ge)
            P.tt(sm[:, 6:7], sm[:, 5:6], sm[:, 4:5], ALU.subtract)
            P.act(sm[:, 7:8], sm[:, 6:7], AF.Exp)
            P.ts(sm[:, 8:9], sm[:, 7:8], 1.0, None, ALU.add)
            P.op("dve", lambda e: e.reciprocal(out=sm.t[:, 9:10], in_=sm.t[:, 8:9]), [sm[:]], [sm[:]])
            P.tt(sm[:, 10:11], sm[:, 9:10], sm[:, 3:4], ALU.mult)
            P.tt(sm[:, 11:12], sm[:, 10:11], sm[:, 7:8], ALU.mult)
            P.ts(msk[:], msk[:], sm[:, 10:11], None, ALU.mult)
            P.stt(G[:, s, :], lem2[:], sm[:, 11:12], msk[:], ALU.mult, ALU.add)
        for e in range((DBG_NE or NE) if not DBG_CUT else 0):
            wg = load_piece(e, 0)
            wu = load_piece(e, 1)
            wd = load_piece(e, 2)
            wgv = wg[:, :].re("p (k f) -> p k f", k=KC)
            wuv = wu[:, :].re("p (k f) -> p k f", k=KC)
            wdv = wd[:, :].re("p (k d) -> p k d", k=6)
            for fc in range(6):
                pg = psb[(fc % 2) * 2]
                pu = psb[(fc % 2) * 2 + 1]
                for k in range(KC):
                    P.mm(pg[:, 0:TT], wgv[:, k, fc * 128:(fc + 1) * 128], hTb[:, k, :], start=(k == 0), stop=(k == KC - 1))
                for k in range(KC):
                    P.mm(pu[:, 0:TT], wuv[:, k, fc * 128:(fc + 1) * 128], hTb[:, k, :], start=(k == 0), stop=(k == KC - 1))
                P.act(sil[fc % 2][:, :], pg[:, 0:TT], AF.Silu)
                P.tt(actT[:, fc, :], sil[fc % 2][:, :], pu[:, 0:TT], ALU.mult)
            i = 0
            for s in range(nsub):
                for dg in range(4):
                    pd = psb[4 + (i % 4)]
                    i += 1
                    for fc in range(6):
                        P.mm(pd[:, :], actT[:, fc, s * 128:(s + 1) * 128], wdv[:, fc, dg * 512:(dg + 1) * 512],
                             start=(fc == 0), stop=(fc == 5))
                    P.stt(acc[:, s, dg * 512:(dg + 1) * 512], pd[:, :], G[:, s, e:e + 1],
                          acc[:, s, dg * 512:(dg + 1) * 512], ALU.mult, ALU.add)
        for s in range(nsub):
            r0 = t0 + s * 128
            P.dma("sp", c.dq["xout"], X[r0:r0 + 128, :], acc[:, s, :])
    P.barrier()
    P.es = P_es
    P.sb = _sb0
    return es


I32 = mybir.dt.int32


def floor_pos(P, out, x, itmp, ftmp):
    P.copy(itmp, x)
    P.copy(out, itmp)
    P.tt(ftmp, out, x, ALU.is_gt)
    P.tt(out, out, ftmp, ALU.subtract)


def stage_moe_g(P, c, X, layer, io, TS=512):
    ntok = c.ntok
    nsub = TS // 128
    KC = D // 128
    NTI = ntok // 128
    NT = ntok // TS + 8
    NSLOT = NT * TS
    psb = c.psb
    L_ = "_g%d" % layer
    HS = P.dram("mg_HS" + L_, [NSLOT, D], BF16)
    GS = P.dram("mg_GS" + L_, [NSLOT, 8], F32)
    YS = P.dram("mg_YS" + L_, [NSLOT, D], F32)
    wsrc = [io["moe_wg_r"], io["moe_wu_r"], io["moe_wd_r"]]
    eso = ExitStack(); P_eso = P.es; P.es = eso
    slot_i = P.sb("mg_slot" + L_, [128, NTI], I32)
    idxW = P.sb("mg_idxW" + L_, [128, NT * 8], I32)
    P.es = P_eso
    es = ExitStack(); P_es = P.es; P.es = es
    _sb0 = P.sb
    P.sb = lambda name, shape, dt: _sb0(name + L_, shape, dt)
    hall = P.sb("g_hall", [128, NTI, D], BF16)
    xt = P.sb("g_xt", [128, D], F32)
    h32 = P.sb("g_h32", [128, D], F32)
    sq = P.sb("g_sq", [128, D], F32)
    stat = P.sb("g_stat", [128, 8], F32)
    hT32 = P.sb("g_hT32", [128, KC, 128], F32)
    wr = P.sb("g_wr", [128, KC, 72], F32)
    br = P.sb("g_br", [128, 72], F32)
    G = P.sb("g_G", [128, NE], F32)
    lg = P.sb("g_lg", [128, 72], F32)
    lem = P.sb("g_lem", [128, NE], F32)
    lem2 = P.sb("g_lem2", [128, NE], F32)
    msk = P.sb("g_msk", [128, NE], F32)
    sm = P.sb("g_sm", [128, 16], F32)
    OH = P.sb("g_OH", [128, NTI, 8], F32)
    G8 = P.sb("g_G8", [128, NTI, 8], F32)
    R = P.sb("g_R", [128, NTI, 8], F32)
    OHs = P.sb("g_OHs", [128, 8], F32)
    Ls = P.sb("g_Ls", [128, 128], F32)
    ones = P.sb("g_ones", [128, 128], F32)
    zb = P.sb("g_zb", [128, D], BF16)
    sc = P.sb("g_sc", [128, 64], F32)
    sci = P.sb("g_sci", [128, 64], I32)
    jv = P.sb("g_jv", [128, NT], F32)
    gidf = P.sb("g_gidf", [128, NT], F32)
    idf = P.sb("g_idf", [128, NT, 8], F32)
    Tt = P.sb("g_Tt", [128, NTI, 8], F32)
    slf = P.sb("g_slf", [128, NTI], F32)
    P.memset(Ls[:], 1.0, eng="pool")
    P.op("pool", lambda e: e.affine_select(out=Ls.t[:], in_=Ls.t[:], pattern=[[1, 128]], compare_op=ALU.is_gt,
                                           fill=0.0, base=0, channel_multiplier=-1), [Ls[:]], [Ls[:]])
    P.memset(ones[:], 1.0)
    P.memset(OHs[:], 0.0)
    P.memset(zb[:], 0.0)
    P.op("pool", lambda e: e.iota(jv.t[:], pattern=[[1, NT]], base=0, channel_multiplier=0,
                                  allow_small_or_imprecise_dtypes=True), [], [jv[:]])
    P.op("pool", lambda e: e.iota(idf.t[:], pattern=[[0, NT], [128, 8]], base=layer * NE * 128, channel_multiplier=1,
                                  allow_small_or_imprecise_dtypes=True), [], [idf[:]])
    for r0 in range(0, NSLOT, 128):
        P.dma("sp", c.dq["xout"], HS[r0:r0 + 128, :], zb[:])
    P.dma("sp", c.dq["xout"], V(GS.t.rearrange("(a p) e -> p a e", p=128), GS),
          V(zb.t[:, 0:NSLOT // 128 * 8 * 2].bitcast(F32).rearrange("p (a e) -> p a e", e=8), zb))
    P.dma("sp", c.dq["misc"], c.gvec[:], V(io["norm_ffn"].t[layer:layer + 1, :].partition_broadcast(128), io["norm_ffn"]))
    P.dma("sp", c.dq["misc"], wr[:, :, 0:8], V(io["moe_w_group"].t[layer].rearrange("(k p) n -> p k n", p=128), io["moe_w_group"]))
    P.dma("sp", c.dq["misc"], wr[:, :, 8:72], V(io["moe_w_expert"].t[layer].rearrange("(k p) n -> p k n", p=128), io["moe_w_expert"]))
    P.dma("sp", c.dq["misc"], br[:, 0:8], V(io["moe_b_group"].t[layer:layer + 1, :].partition_broadcast(128), io["moe_b_group"]))
    P.dma("sp", c.dq["misc"], br[:, 8:72], V(io["moe_b_expert"].t[layer:layer + 1, :].partition_broadcast(128), io["moe_b_expert"]))
    for i in range(NTI):
        r0 = i * 128
        P.dma("sp", c.dq["xin"], xt[:], X[r0:r0 + 128, :])
        rms_h(P, c, xt[:], h32[:], sq[:], stat[:])
        P.copy(hall[:, i, :], h32[:], eng="pool")
        for q in range(4):
            for j in range(4):
                k = q * 4 + j
                P.tr(psb[q][:, j * 128:(j + 1) * 128], h32[:, k * 128:(k + 1) * 128], c.ident[:], sig=(j == 3))
            P.copy(hT32[:, q * 4:(q + 1) * 4, :], psb[q][:, :].re("p (j t) -> p j t", j=4), eng="act")
        for k in range(KC):
            P.mm(psb[4][:, 0:72], hT32[:, k, :], wr[:, k, :], start=(k == 0), stop=(k == KC - 1))
        P.tt(lg[:], psb[4][:, 0:72], br[:], ALU.add)
        P.red(sm[:, 0:1], lg[:, 0:8], ALU.max)
        P.ts(sm[:, 1:2], sm[:, 0:1], -1.0, None, ALU.mult)
        P.act(msk[:, 0:8], lg[:, 0:8], AF.Exp, bias=sm[:, 1:2], accum=sm[:, 2:3])
        P.op("dve", lambda e: e.reciprocal(out=sm.t[:, 3:4], in_=sm.t[:, 2:3]), [sm[:]], [sm[:]])
        P.ts(OH[:, i, :], lg[:, 0:8], sm[:, 0:1], None, ALU.is_ge)
        P.ts(msk[:, 8:16], OH[:, i, :], 1e30, -1e30, ALU.mult, ALU.add)
        P.tt(lem[:, :].re("p (g e) -> p g e", g=8), lg[:, 8:72].re("p (g e) -> p g e", g=8),
             V(msk.t[:, 8:16].unsqueeze(2).broadcast_to([128, 8, 8]), msk), ALU.add)
        P.red(sm[:, 4:5], lem[:], ALU.max)
        P.ts(msk[:], lem[:], sm[:, 4:5], None, ALU.is_ge)
        P.stt(lem2[:], msk[:], -1e30, lem[:], ALU.mult, ALU.add)
        P.red(sm[:, 5:6], lem2[:], ALU.max)
        P.ts(lem2[:], lem2[:], sm[:, 5:6], None, ALU.is_ge)
        P.tt(sm[:, 6:7], sm[:, 5:6], sm[:, 4:5], ALU.subtract)
        P.act(sm[:, 7:8], sm[:, 6:7], AF.Exp)
        P.ts(sm[:, 8:9], sm[:, 7:8], 1.0, None, ALU.add)
        P.op("dve", lambda e: e.reciprocal(out=sm.t[:, 9:10], in_=sm.t[:, 8:9]), [sm[:]], [sm[:]])
        P.tt(sm[:, 10:11], sm[:, 9:10], sm[:, 3:4], ALU.mult)
        P.tt(sm[:, 11:12], sm[:, 10:11], sm[:, 7:8], ALU.mult)
        P.ts(msk[:], msk[:], sm[:, 10:11], None, ALU.mult)
        P.stt(G[:], lem2[:], sm[:, 11:12], msk[:], ALU.mult, ALU.add)
        P.red(G8[:, i, :], G[:, :].re("p (g e) -> p e g", g=8), ALU.add)
        P.mm(psb[5][:, 0:8], Ls[:], OH[:, i, :], True, False, sig=False)
        P.mm(psb[5][:, 0:8], ones[:], OHs[:], False, True)
        P.copy(R[:, i, :], psb[5][:, 0:8])
        P.tt(OHs[:], OHs[:], OH[:, i, :], ALU.add)
    P.mm(psb[5][:, 8:16], ones[:], OHs[:], True, True)
    P.ts(sc[:, 0:8], psb[5][:, 8:16], float(TS - 1), 1.0 / TS, ALU.add, ALU.mult)
    floor_pos(P, sc[:, 8:16], sc[:, 0:8], sci[:, 0:8], sc[:, 16:24])
    P.copy(sc[:, 24:25], sc[:, 8:9])
    for g in range(1, 8):
        P.tt(sc[:, 24 + g:25 + g], sc[:, 23 + g:24 + g], sc[:, 8 + g:9 + g], ALU.add)
    P.tt(sc[:, 32:40], sc[:, 24:32], sc[:, 8:16], ALU.subtract)
    P.ts(sc[:, 32:40], sc[:, 32:40], float(TS), None, ALU.mult)
    P.tt(Tt[:], R[:], V(sc.t[:, 32:40].unsqueeze(1).broadcast_to([128, NTI, 8]), sc), ALU.add)
    P.tt(Tt[:], Tt[:], OH[:], ALU.mult)
    P.red(slf[:], Tt[:], ALU.add)
    P.copy(slot_i[:], slf[:])
    P.ts(gidf[:], jv[:], sc[:, 24:25], None, ALU.is_ge)
    for g in range(1, 8):
        P.stt(gidf[:], jv[:], sc[:, 24 + g:25 + g], gidf[:], ALU.is_ge, ALU.add)
    P.ts(gidf[:], gidf[:], 7.0, 1024.0, ALU.min, ALU.mult)
    P.tt(idf[:], idf[:], V(gidf.t[:, :].unsqueeze(2).broadcast_to([128, NT, 8]), gidf), ALU.add)
    P.copy(idxW[:, :].re("p (j e) -> p j e", e=8), idf[:])
    for i in range(NTI):
        for (dst, src) in ((HS, hall[:, i, :]), (GS, G8[:, i, :])):
            P._wait("pool", P._deps([src, slot_i[:]], [dst[:, :]]))
            ins = P.nc.gpsimd.indirect_dma_start(out=dst.t[:, :], out_offset=bass.IndirectOffsetOnAxis(ap=slot_i.t[:, i:i + 1], axis=0),
                                                 in_=src.ap, in_offset=None)
            ins.then_inc(P.sem[c.dq["w3"]], 16); P.cnt[c.dq["w3"]] += 1
            P._record(c.dq["w3"], P.cnt[c.dq["w3"]], [src, slot_i[:]], [dst[:, :]])
    P.barrier(); P.es = P_es; P.sb = _sb0; es.close()

    es = ExitStack(); P_es = P.es; P.es = es
    _sb0 = P.sb
    P.sb = lambda name, shape, dt: _sb0(name + L_, shape, dt)
    acc = P.sb("h_acc", [128, nsub, D], F32)
    hs = P.sb("h_hs", [128, nsub, D], BF16)
    hTb = P.sb("h_hTb", [128, KC, TS], BF16)
    actT = P.sb("h_actT", [128, 6, TS], BF16)
    sil = [P.sb("h_sil%d" % i, [128, TS], F32) for i in range(2)]
    gst = P.sb("h_gst", [128, nsub, 8], F32)
    wslot = [P.sb("h_w%d" % i, [128, 6 * D], BF16) for i in range(4)]
    wq = ["w0", "w1", "w2", "w3"]
    slot_n = [0]
    pbf = [V(psb[i].t[:, :].bitcast(BF16), psb[i]) for i in range(2)]

    def gather_piece(j, e, piece):
        s_ = slot_n[0] % 4
        slot_n[0] += 1
        dst = wslot[s_]
        ixa = idxW.t[:, j * 8 + e:j * 8 + e + 1]
        src = wsrc[piece]
        P._wait("pool", P._deps([idxW[:]], [dst[:]]))
        ins = P.nc.gpsimd.indirect_dma_start(out=dst.t[:, :], out_offset=None, in_=src.t[:, :],
                                             in_offset=bass.IndirectOffsetOnAxis(ap=ixa, axis=0))
        ins.then_inc(P.sem[c.dq[wq[s_]]], 16); P.cnt[c.dq[wq[s_]]] += 1
        P._record(c.dq[wq[s_]], P.cnt[c.dq[wq[s_]]], [src[:, :], idxW[:]], [dst[:]])
        return dst

    for j in range(NT):
        t0 = j * TS
        P.dma("sp", c.dq["xin"], hs[:], V(HS.t[t0:t0 + TS, :].rearrange("(s p) d -> p s d", p=128), HS))
        P.dma("sp", c.dq["xin"], gst[:], V(GS.t[t0:t0 + TS, :].rearrange("(s p) e -> p s e", p=128), GS))
        for s in range(nsub):
            for q in range(2):
                for jj in range(8):
                    k = q * 8 + jj
                    P.tr(pbf[q][:, jj * 128:(jj + 1) * 128], hs[:, s, k * 128:(k + 1) * 128], c.identb[:], sig=(jj == 7))
                P.copy(hTb[:, q * 8:(q + 1) * 8, s * 128:(s + 1) * 128], pbf[q][:, :].re("p (j t) -> p j t", j=8), eng="act")
        for e in range(8):
            wg = gather_piece(j, e, 0)
            wu = gather_piece(j, e, 1)
            wd = gather_piece(j, e, 2)
            wgv = wg[:, :].re("p (k f) -> p k f", k=KC)
            wuv = wu[:, :].re("p (k f) -> p k f", k=KC)
            wdv = wd[:, :].re("p (k d) -> p k d", k=6)
            for fc in range(6):
                pg = psb[(fc % 2) * 2]
                pu = psb[(fc % 2) * 2 + 1]
                for k in range(KC):
                    P.mm(pg[:, 0:TS], wgv[:, k, fc * 128:(fc + 1) * 128], hTb[:, k, :], start=(k == 0), stop=(k == KC - 1))
                for k in range(KC):
                    P.mm(pu[:, 0:TS], wuv[:, k, fc * 128:(fc + 1) * 128], hTb[:, k, :], start=(k == 0), stop=(k == KC - 1))
                P.act(sil[fc % 2][:, :], pg[:, 0:TS], AF.Silu)
                P.tt(actT[:, fc, :], sil[fc % 2][:, :], pu[:, 0:TS], ALU.mult)
            i = 0
            for s in range(nsub):
                for dg in range(4):
                    pd = psb[4 + (i % 4)]
                    i += 1
                    for fc in range(6):
                        P.mm(pd[:, :], actT[:, fc, s * 128:(s + 1) * 128], wdv[:, fc, dg * 512:(dg + 1) * 512],
                             start=(fc == 0), stop=(fc == 5))
                    a_ = acc[:, s, dg * 512:(dg + 1) * 512]
                    if e == 0:
                        P.ts(a_, pd[:, :], gst[:, s, 0:1], None, ALU.mult)
                    else:
                        P.stt(a_, pd[:, :], gst[:, s, e:e + 1], a_, ALU.mult, ALU.add)
        P.dma("sp", c.dq["xout"], V(YS.t[t0:t0 + TS, :].rearrange("(s p) d -> p s d", p=128), YS), acc[:])
        if j == 1 and layer == 0:
            P.dbg("g_hs", hs[:], [128, nsub, D], BF16)
            P.dbg("g_gst", gst[:], [128, nsub, 8], F32)
            P.dbg("g_hTb", hTb[:], [128, KC, TS], BF16)
            P.dbg("g_acc", acc[:], [128, nsub, D], F32)
    P.barrier(); P.es = P_es; P.sb = _sb0; es.close()

    es = ExitStack(); P_es = P.es; P.es = es
    _sb0 = P.sb
    P.sb = lambda name, shape, dt: _sb0(name + L_, shape, dt)
    xr = [P.sb("r_x%d" % i, [128, D], F32) for i in range(2)]
    yr = [P.sb("r_y%d" % i, [128, D], F32) for i in range(2)]
    ysem = [c.dq["w0"], c.dq["w1"]]
    for i in range(NTI):
        b = i % 2
        r0 = i * 128
        P.dma("sp", c.dq["xin"], xr[b][:], X[r0:r0 + 128, :])
        P._wait("pool", P._deps([YS[:, :], slot_i[:]], [yr[b][:]]))
        ins = P.nc.gpsimd.indirect_dma_start(out=yr[b].t[:, :], out_offset=None, in_=YS.t[:, :],
                                             in_offset=bass.IndirectOffsetOnAxis(ap=slot_i.t[:, i:i + 1], axis=0))
        ins.then_inc(P.sem[ysem[b]], 16); P.cnt[ysem[b]] += 1
        P._record(ysem[b], P.cnt[ysem[b]], [YS[:, :], slot_i[:]], [yr[b][:]])
        P.tt(xr[b][:], xr[b][:], yr[b][:], ALU.add)
        P.dma("sp", c.dq["xout"], X[r0:r0 + 128, :], xr[b][:])
    P.barrier(); P.es = P_es; P.sb = _sb0; es.close()
    eso.close()


def front_end(P, c, X, t0, nsub, xt, h32, sq, stat, hTb, psb, identb_unused=None):
    for s in range(nsub):
        r0 = t0 + s * 128
        P.dma("sp", c.dq["xin"], xt[:], X[r0:r0 + 128, :])
        rms_h(P, c, xt[:], h32[:], sq[:], stat[:])
        for q in range(4):
            for j in range(4):
                k = q * 4 + j
                P.tr(psb[q][:, j * 128:(j + 1) * 128], h32[:, k * 128:(k + 1) * 128], c.ident[:], sig=(j == 3))
            P.copy(hTb[:, q * 4:(q + 1) * 4, s * 128:(s + 1) * 128], psb[q][:, :].re("p (j t) -> p j t", j=4), eng="act")


def stage_mlstm(P, c, X, io):
    ntok = c.ntok
    NH, DK, DV, L = 8, 128, 256, 64
    KC = D // 128
    TT = 512
    nsub = TT // 128
    psb = c.psb
    win = io["ml_w_in"]
    QT = P.dram("ml_QT", [NH, DK, ntok], BF16)
    KT = P.dram("ml_KT", [NH, DK, ntok], BF16)
    Ktm = P.dram("ml_Ktm", [ntok, NH * DK], F32)
    Vs = P.dram("ml_V", [ntok, NH * DV], BF16)
    Os = P.dram("ml_O", [ntok, NH * DV], F32)
    Gs = P.dram("ml_G", [ntok, 16], F32)
    OUTs = P.dram("ml_OUT", [ntok, NH * DV], BF16)

    es = ExitStack(); P_es = P.es; P.es = es
    xt = P.sb("a_xt", [128, D], F32)
    h32 = P.sb("a_h32", [128, D], F32)
    sq = P.sb("a_sq", [128, D], F32)
    stat = P.sb("a_stat", [128, 8], F32)
    hTb = P.sb("a_hTb", [128, KC, TT], BF16)
    wsl = [P.sb("a_w%d" % i, [128, KC, 512], BF16) for i in range(3)]
    wg = P.sb("a_wg", [128, KC, 16], BF16)
    bg = P.sb("a_bg", [128, 16], F32)
    stb = [P.sb("a_stb%d" % i, [128, 512], BF16) for i in range(3)]
    stf = [P.sb("a_stf%d" % i, [128, 512], F32) for i in range(3)]
    gt = P.sb("a_gt", [128, 48], F32)
    P.dma("sp", c.dq["misc"], c.gvec[:], V(io["norm_mix"].t[1:2, :].partition_broadcast(128), io["norm_mix"]))
    P.dma("pool", c.dq["misc"], wg[:], V(win.t[0, :, 6144:6160].rearrange("(k p) n -> p k n", p=128), win))
    P.dma("sp", c.dq["misc"], bg[:], V(io["ml_b_gate"].t[0:1, :].partition_broadcast(128), io["ml_b_gate"]))
    wq = ["w0", "w1", "w2"]
    cnt = [0, 0, 0]
    for t0 in range(0, ntok, TT):
        front_end(P, c, X, t0, nsub, xt, h32, sq, stat, hTb, psb)
        for s in range(nsub):
            r0 = t0 + s * 128
            for k in range(KC):
                P.mm(psb[4][:, 0:16], hTb[:, k, s * 128:(s + 1) * 128], wg[:, k, :], start=(k == 0), stop=(k == KC - 1))
            P.tt(gt[:, 0:16], psb[4][:, 0:16], bg[:], ALU.add)
            P.act(gt[:, 16:32], gt[:, 0:16], AF.Tanh, scale=1.0 / 15.0)
            P.ts(gt[:, 32:40], gt[:, 16:24], 15.0, None, ALU.mult)
            P.act(gt[:, 0:8], gt[:, 24:32], AF.Exp, scale=-15.0)
            P.ts(gt[:, 0:8], gt[:, 0:8], 1.0, None, ALU.add)
            P.act(gt[:, 8:16], gt[:, 0:8], AF.Ln)
            P.ts(gt[:, 40:48], gt[:, 8:16], -1.0, None, ALU.mult)
            P.dma("sp", c.dq["xout"], Gs[r0:r0 + 128, :], gt[:, 32:48])
        for blk in range(12):
            i = cnt[0] % 3
            cnt[0] += 1
            w = wsl[i]
            for hk in range(2):
                P.dma("pool", c.dq[wq[i]], w[:, hk * 8:(hk + 1) * 8, :],
                      V(win.t[0, :, blk * 512:(blk + 1) * 512].rearrange("(k p) n -> p k n", p=128)[:, hk * 8:(hk + 1) * 8, :], win))
            if blk < 4:
                dst = QT if blk < 2 else KT
                sc = 1.0 if blk < 2 else DK ** -0.5
                for hh in range(4):
                    head = (blk % 2) * 4 + hh
                    pp = psb[4 + (cnt[1] % 2)]
                    for k in range(KC):
                        P.mm(pp[:, :], w[:, k, hh * 128:(hh + 1) * 128], hTb[:, k, :], start=(k == 0), stop=(k == KC - 1))
                    sb_ = stb[cnt[1] % 3]
                    cnt[1] += 1
                    P.act(sb_[:], pp[:, :], AF.Copy, scale=sc)
                    P.dma("sp", c.dq["xout"], dst[head, :, t0:t0 + TT], sb_[:])
            if blk >= 2:
                for s in range(nsub):
                    r0 = t0 + s * 128
                    pp = psb[6 + (cnt[2] % 2)]
                    for k in range(KC):
                        P.mm(pp[:, :], hTb[:, k, s * 128:(s + 1) * 128], w[:, k, :], start=(k == 0), stop=(k == KC - 1))
                    j = cnt[2] % 3
                    cnt[2] += 1
                    if blk < 4:
                        P.act(stf[j][:], pp[:, :], AF.Copy, scale=DK ** -0.5)
                        P.dma("sp", c.dq["xout"], Ktm[r0:r0 + 128, (blk - 2) * 512:(blk - 1) * 512], stf[j][:])
                    elif blk < 8:
                        P.copy(stb[j][:], pp[:, :], eng="act")
                        P.dma("sp", c.dq["xout"], Vs[r0:r0 + 128, (blk - 4) * 512:(blk - 3) * 512], stb[j][:])
                    else:
                        P.act(stf[j][:], pp[:, :], AF.Sigmoid)
                        P.dma("sp", c.dq["xout"], Os[r0:r0 + 128, (blk - 8) * 512:(blk - 7) * 512], stf[j][:])
    P.barrier(); P.es = P_es; es.close()

    es = ExitStack(); P_es = P.es; P.es = es
    triU = P.sb("b_triU", [64, 64], F32)
    negm = P.sb("b_negm", [64, 64], F32)
    ones = P.sb("b_ones", [64, 128], F32)
    onesb = P.sb("b_onesb", [64, 1], BF16)
    Cst = P.sb("b_C", [128, NH, DV], F32)
    Cb = P.sb("b_Cb", [128, NH, DV], BF16)
    nst = P.sb("b_n", [128, NH], F32)
    nb = P.sb("b_nb", [128, NH], BF16)
    ghead = P.sb("b_gh", [64, NH * DV], F32)
    P.memset(triU[:], 1.0, eng="pool")
    P.op("pool", lambda e: e.affine_select(out=triU.t[:], in_=triU.t[:], pattern=[[1, 64]], compare_op=ALU.is_ge,
                                           fill=0.0, base=0, channel_multiplier=-1), [triU[:]], [triU[:]])
    P.ts(negm[:], triU[:], 30000.0, -30000.0, ALU.mult, ALU.add)
    P.memset(ones[:], 1.0)
    P.memset(onesb[:], 1.0)
    P.memset(Cst[:], 0.0); P.memset(Cb[:], 0.0); P.memset(nst[:], 0.0); P.memset(nb[:], 0.0)
    P.dma("sp", c.dq["misc"], ghead[:], V(io["ml_g_head"].t[0:1, :].partition_broadcast(64), io["ml_g_head"]))
    NB = 2
    qT = [P.sb("b_qT%d" % i, [128, NH, L], BF16) for i in range(NB)]
    kT = [P.sb("b_kT%d" % i, [128, NH, L], BF16) for i in range(NB)]
    ktm = [P.sb("b_ktm%d" % i, [64, NH * DK], F32) for i in range(NB)]
    vv = [P.sb("b_v%d" % i, [64, NH, DV], BF16) for i in range(NB)]
    oo = [P.sb("b_o%d" % i, [64, NH * DV], F32) for i in range(NB)]
    gg = [P.sb("b_g%d" % i, [64, 16], F32) for i in range(NB)]
    dsem = [P.dma_sem("bl%d" % i) for i in range(NB)]
    sml = P.sb("b_sml", [128, 96], F32)
    bts = P.sb("b_bts", [128, NH * L], F32)
    lfb = P.sb("b_lfb", [64, NH, 128], F32)
    Eb = P.sb("b_E", [64, NH, L], F32)
    Dm = P.sb("b_Dm", [64, NH, L], F32)
    SD = P.sb("b_SD", [64, NH, L], BF16)
    aT = P.sb("b_aT", [128, NH, L], F32)
    qs = P.sb("b_qs", [128, NH, L], BF16)
    kw = P.sb("b_kw", [64, NH, DK], BF16)
    hh = P.sb("b_hh", [64, NH, DV], F32)
    hsq = P.sb("b_hsq", [64, DV], F32)
    gho = P.sb("b_gho", [64, NH * DV], F32)
    outb = [P.sb("b_out%d" % i, [64, NH * DV], BF16) for i in range(2)]
    nchunk = ntok // L

    def load_chunk(ci):
        b = ci % NB
        t0 = ci * L
        P.dma("sp", dsem[b], qT[b][:], V(QT.t[:, :, t0:t0 + L].rearrange("h d t -> d h t"), QT))
        P.dma("sp", dsem[b], kT[b][:], V(KT.t[:, :, t0:t0 + L].rearrange("h d t -> d h t"), KT))
        P.dma("sp", dsem[b], ktm[b][:], Ktm[t0:t0 + L, :])
        P.dma("sp", dsem[b], vv[b][:, :, :].re("t h d -> t (h d)"), Vs[t0:t0 + L, :])
        P.dma("sp", dsem[b], oo[b][:], Os[t0:t0 + L, :])
        P.dma("sp", dsem[b], gg[b][:], Gs[t0:t0 + L, :])

    load_chunk(0)
    for ci in range(nchunk):
        b = ci % NB
        if ci + 1 < nchunk:
            load_chunk(ci + 1)
        ig = gg[b][:, 0:8]
        lf = gg[b][:, 8:16]
        p0 = psb[0]
        P.mm(p0[0:64, 0:8], triU[:], lf, True, True)
        P.mm(p0[0:64, 8:16], ones[:, 0:64], lf, True, True)
        P.mm(p0[:, 16:24], ones[:, :], lf, True, True)
        P.copy(sml[:, 0:24], p0[:, 0:24])
        P.act(sml[:, 16:24], sml[:, 16:24], AF.Exp)
        P.tt(sml[0:64, 24:32], ig, sml[0:64, 0:8], ALU.subtract)
        P.tt(sml[0:64, 32:40], sml[0:64, 24:32], sml[0:64, 8:16], ALU.add)
        P.act(sml[0:64, 40:48], sml[0:64, 32:40], AF.Exp)
        P.copy(lfb[:], V(gg[b].t[:, 8:16].unsqueeze(2).broadcast_to([64, NH, 128]), gg[b]))
        for h in range(NH):
            P.mm(psb[1][:, h * L:(h + 1) * L], lfb[:, h, :], triU[:], True, True, sig=(h == NH - 1))
        for h in range(NH):
            P.mm(psb[2][0:64, h * L:(h + 1) * L], kT[b][:, h, :], qT[b][:, h, :], True, True, sig=(h == NH - 1))
        P.copy(bts[:], psb[1][:, :])
        P.act(aT[:, :, :].re("p h t -> p (h t)"), bts[:], AF.Exp)
        P.tt(Eb[:], bts[0:64, :].re("p (h t) -> p h t", h=NH), V(negm.t[:, :].unsqueeze(1).broadcast_to([64, NH, L]), negm), ALU.add)
        for h in range(NH):
            P.act(Dm[:, h, :], Eb[:, h, :], AF.Exp, bias=sml[0:64, 24 + h:25 + h])
        P.tt(SD[:], psb[2][0:64, :].re("p (h t) -> p h t", h=NH), Dm[:], ALU.mult)
        P.tt(qs[:], qT[b][:], aT[:], ALU.mult, eng="pool")
        for h in range(NH):
            P.mm(p0[0:64, 24 + h:25 + h], SD[:, h, :], onesb[:], True, False, sig=False)
            P.mm(p0[0:64, 24 + h:25 + h], qs[:, h, :], nb[:, h:h + 1], False, True, sig=(h == NH - 1))
        P.ts(sml[0:64, 48:56], p0[0:64, 24:32], -1.0, None, ALU.mult)
        P.tt(sml[0:64, 48:56], sml[0:64, 48:56], p0[0:64, 24:32], ALU.max)
        P.ts(sml[0:64, 48:56], sml[0:64, 48:56], 1.0, None, ALU.max)
        P.op("dve", lambda e: e.reciprocal(out=sml.t[0:64, 56:64], in_=sml.t[0:64, 48:56]), [sml[:]], [sml[:]])
        for h in range(NH):
            pn = psb[3 + h // 2]
            o_ = (h % 2) * DV
            P.mm(pn[0:64, o_:o_ + DV], SD[:, h, :], vv[b][:, h, :], True, False, sig=False)
            P.mm(pn[0:64, o_:o_ + DV], qs[:, h, :], Cb[:, h, :], False, True)
            P.act(hh[:, h, :], pn[0:64, o_:o_ + DV], AF.Copy, scale=sml[0:64, 56 + h:57 + h])
            P.act(hsq[:], hh[:, h, :], AF.Square, accum=sml[0:64, 64 + h:65 + h])
        P.ts(sml[0:64, 72:80], sml[0:64, 64:72], 1.0 / DV, EPS, ALU.mult, ALU.add)
        P.act(sml[0:64, 80:88], sml[0:64, 72:80], AF.Sqrt)
        P.op("dve", lambda e: e.reciprocal(out=sml.t[0:64, 88:96], in_=sml.t[0:64, 80:88]), [sml[:]], [sml[:]])
        P.tt(gho[:], oo[b][:], ghead[:], ALU.mult, eng="pool")
        ob = outb[ci % 2]
        for h in range(NH):
            P.stt(ob[:, h * DV:(h + 1) * DV], hh[:, h, :], sml[0:64, 88 + h:89 + h], gho[:, h * DV:(h + 1) * DV], ALU.mult, ALU.mult)
        P.dma("sp", c.dq["xout"], OUTs[ci * L:(ci + 1) * L, :], ob[:])
        if ci == 0:
            P.dbg("sml", sml[:], [128, 96], F32)
            P.dbg("lfb", lfb[:], [64, NH, 128], F32)
            P.dbg("bts", bts[:], [128, NH * L], F32)
            P.dbg("Eb", Eb[:], [64, NH, L], F32)
            P.dbg("Dm", Dm[:], [64, NH, L], F32)
            P.dbg("SD", SD[:], [64, NH, L], BF16)
            P.dbg("aT", aT[:], [128, NH, L], F32)
            P.dbg("qs", qs[:], [128, NH, L], BF16)
            P.dbg("qT", qT[b][:], [128, NH, L], BF16)
            P.dbg("kT", kT[b][:], [128, NH, L], BF16)
            P.dbg("vv", vv[b][:], [64, NH, DV], BF16)
            P.dbg("hh", hh[:], [64, NH, DV], F32)
            P.dbg("negm", negm[:], [64, 64], F32)
            P.dbg("triU", triU[:], [64, 64], F32)
        for h in range(NH):
            P.ts(kw[:, h, :], ktm[b][:, h * DK:(h + 1) * DK], sml[0:64, 40 + h:41 + h], None, ALU.mult)
        for h in range(NH):
            P.mm(p0[:, 32 + h:33 + h], kw[:, h, :], onesb[:], True, True, sig=(h == NH - 1))
        for h in range(NH):
            pn = psb[3 + h // 2]
            o_ = (h % 2) * DV
            P.mm(pn[:, o_:o_ + DV], kw[:, h, :], vv[b][:, h, :], True, True)
            P.stt(Cst[:, h, :], Cst[:, h, :], sml[:, 16 + h:17 + h], pn[:, o_:o_ + DV], ALU.mult, ALU.add)
        P.tt(nst[:], nst[:], sml[:, 16:24], ALU.mult)
        P.tt(nst[:], nst[:], p0[:, 32:40], ALU.add)
        P.copy(Cb[:], Cst[:], eng="pool")
        P.copy(nb[:], nst[:])
    P.barrier(); P.es = P_es; es.close()

    es = ExitStack(); P_es = P.es; P.es = es
    wo = P.sb("c_wo", [128, KC, D], BF16)
    ot = P.sb("c_ot", [128, D], BF16)
    oT = P.sb("c_oT", [128, KC, 128], BF16)
    xr = [P.sb("c_x%d" % i, [128, D], F32) for i in range(2)]
    pst = [P.ps("c_pst%d" % i, [128, 1024], BF16) for i in range(0)]
    wov = io["ml_w_out"]
    for hk in range(4):
        P.dma("pool", c.dq["w0"], wo[:, hk * 4:(hk + 1) * 4, :],
              V(wov.t[0].rearrange("(k p) n -> p k n", p=128)[:, hk * 4:(hk + 1) * 4, :], wov))
    pbf = [V(psb[i].t[:, :].bitcast(BF16), psb[i]) for i in range(2)]
    for ti, r0 in enumerate(range(0, ntok, 128)):
        x_ = xr[ti % 2]
        P.dma("sp", c.dq["xin"], ot[:], OUTs[r0:r0 + 128, :])
        P.dma("sp", c.dq["xin"], x_[:], X[r0:r0 + 128, :])
        for q in range(2):
            for j in range(8):
                k = q * 8 + j
                P.tr(pbf[q][:, j * 128:(j + 1) * 128], ot[:, k * 128:(k + 1) * 128], c.identb[:], sig=(j == 7))
            P.copy(oT[:, q * 8:(q + 1) * 8, :], pbf[q][:, :].re("p (j t) -> p j t", j=8), eng="act")
        for dg in range(4):
            pp = psb[4 + dg]
            for k in range(KC):
                P.mm(pp[:, :], oT[:, k, :], wo[:, k, dg * 512:(dg + 1) * 512], start=(k == 0), stop=(k == KC - 1))
            P.tt(x_[:, dg * 512:(dg + 1) * 512], x_[:, dg * 512:(dg + 1) * 512], pp[:, :], ALU.add)
        P.dma("sp", c.dq["xout"], X[r0:r0 + 128, :], x_[:])
    P.barrier(); P.es = P_es; es.close()


import math


def frac_pm_half(P, out, a, itmp, ftmp):
    P.copy(itmp, a)
    P.copy(ftmp, itmp)
    P.tt(out, a, ftmp, ALU.subtract)
    P.ts(ftmp, out, 0.5, None, ALU.is_gt)
    P.tt(out, out, ftmp, ALU.subtract)
    P.ts(ftmp, out, -0.5, None, ALU.is_lt)
    P.tt(out, out, ftmp, ALU.add)


def stage_s5(P, c, X, io):
    ntok = c.ntok
    KC = D // 128
    psb = c.psb
    NT = ntok // 512
    HT = P.dram("s5_HT", [D, ntok], F32)
    GT = P.dram("s5_GT", [D, ntok], BF16)
    PRM = P.dram("s5_PRM", [6, 128, 64], F32)
    es = ExitStack(); P_es = P.es; P.es = es
    xt = P.sb("sa_xt", [128, D], F32)
    h32 = P.sb("sa_h32", [128, D], F32)
    sq = P.sb("sa_sq", [128, D], F32)
    stat = P.sb("sa_stat", [128, 8], F32)
    hT = [P.sb("sa_hT%d" % i, [128, KC, 128], F32) for i in range(2)]
    P.dma("sp", c.dq["misc"], c.gvec[:], V(io["norm_mix"].t[0:1, :].partition_broadcast(128), io["norm_mix"]))
    for ti, r0 in enumerate(range(0, ntok, 128)):
        P.dma("sp", c.dq["xin"], xt[:], X[r0:r0 + 128, :])
        rms_h(P, c, xt[:], h32[:], sq[:], stat[:])
        hb = hT[ti % 2]
        for q in range(4):
            for j in range(4):
                k = q * 4 + j
                P.tr(psb[q][:, j * 128:(j + 1) * 128], h32[:, k * 128:(k + 1) * 128], c.ident[:], sig=(j == 3))
            P.copy(hb[:, q * 4:(q + 1) * 4, :], psb[q][:, :].re("p (j t) -> p j t", j=4), eng="act")
        P.dma("sp", c.dq["xout"], V(HT.t[:, r0:r0 + 128].rearrange("(k p) t -> p k t", p=128), HT), hb[:])
    lr = P.sb("sa_lr", [128, 64], F32); li = P.sb("sa_li", [128, 64], F32); ldt = P.sb("sa_ldt", [128, 1], F32)
    w = [P.sb("sa_w%d" % i, [128, 64], F32) for i in range(12)]
    cpi = P.sb("sa_cpi", [128, 1], F32)
    P.memset(cpi[:], -math.pi)
    P.dma("sp", c.dq["misc"], lr[:], io["s5_lam_re"][:, :])
    P.dma("sp", c.dq["misc"], li[:], io["s5_lam_im"][:, :])
    P.dma("sp", c.dq["misc"], ldt[:], io["s5_log_dt"][:, :])
    P.act(ldt[:], ldt[:], AF.Exp)
    P.ts(w[0][:], lr[:], ldt[:, 0:1], None, ALU.mult)
    P.ts(w[1][:], li[:], ldt[:, 0:1], None, ALU.mult)
    P.act(w[2][:], w[0][:], AF.Exp)
    wi_ = P.sb("sa_wi", [128, 64], mybir.dt.int32)
    P.ts(w[3][:], w[1][:], 1.0 / (2 * math.pi), None, ALU.mult)
    frac_pm_half(P, w[3][:], w[3][:], wi_[:], w[4][:])
    P.ts(w[4][:], w[3][:], 0.25, None, ALU.add)
    frac_pm_half(P, w[4][:], w[4][:], wi_[:], w[5][:])
    P.act(w[6][:], w[4][:], AF.Sin, scale=2 * math.pi)
    P.act(w[5][:], w[3][:], AF.Sin, scale=2 * math.pi)
    P.tt(w[7][:], w[2][:], w[6][:], ALU.mult)
    P.tt(w[8][:], w[2][:], w[5][:], ALU.mult)
    P.ts(w[7][:], w[7][:], -1.0, None, ALU.add)
    P.tt(w[9][:], lr[:], lr[:], ALU.mult)
    P.tt(w[10][:], li[:], li[:], ALU.mult)
    P.tt(w[9][:], w[9][:], w[10][:], ALU.add)
    P.op("dve", lambda e: e.reciprocal(out=w[9].t[:], in_=w[9].t[:]), [w[9][:]], [w[9][:]])
    P.tt(w[10][:], w[7][:], lr[:], ALU.mult)
    P.tt(w[11][:], w[8][:], li[:], ALU.mult)
    P.tt(w[10][:], w[10][:], w[11][:], ALU.add)
    P.tt(w[10][:], w[10][:], w[9][:], ALU.mult)
    P.tt(w[11][:], w[8][:], lr[:], ALU.mult)
    P.tt(w[0][:], w[7][:], li[:], ALU.mult)
    P.tt(w[11][:], w[11][:], w[0][:], ALU.subtract)
    P.tt(w[11][:], w[11][:], w[9][:], ALU.mult)
    P.dma("sp", c.dq["xout"], PRM[0], w[10][:])
    P.dma("sp", c.dq["xout"], PRM[1], w[11][:])
    P.dma("sp", c.dq["xout"], PRM[2], w[2][:])
    P.dma("sp", c.dq["xout"], PRM[3], w[3][:])
    P.barrier(); P.es = P_es; es.close()

    es = ExitStack(); P_es = P.es; P.es = es
    iot = P.sb("sb_iota", [128, ntok], F32)
    P.op("pool", lambda e: e.iota(iot.t[:], pattern=[[1, ntok]], base=0, channel_multiplier=0,
                                  allow_small_or_imprecise_dtypes=True), [], [iot[:]])
    magT = P.sb("sb_magT", [128, 64], F32)
    phiT = P.sb("sb_phiT", [128, 64], F32)
    P.dma("sp", c.dq["misc"], magT[:], V(PRM.t[2].rearrange("(q t) p -> (t p) q", t=2), PRM), allow_slow_non_contiguous=True)
    P.dma("sp", c.dq["misc"], phiT[:], V(PRM.t[3].rearrange("(q t) p -> (t p) q", t=2), PRM), allow_slow_non_contiguous=True)
    dT = P.sb("sb_dT", [128, KC], F32)
    P.dma("sp", c.dq["misc"], dT[:], io["s5_dT"][:, :])
    u = P.sb("sb_u", [128, ntok], F32)
    ysb = P.sb("sb_y", [128, ntok], F32)
    gb = P.sb("sb_gb", [128, ntok], BF16)
    NBF = 2
    bTr = [P.sb("sb_bTr%d" % i, [128, 128], F32) for i in range(NBF)]
    bTi = [P.sb("sb_bTi%d" % i, [128, 128], F32) for i in range(NBF)]
    Qr = [P.sb("sb_Qr%d" % i, [128, 128], F32) for i in range(NBF)]
    Qi = [P.sb("sb_Qi%d" % i, [128, 128], F32) for i in range(NBF)]
    cTr = [P.sb("sb_cTr%d" % i, [128, 128], F32) for i in range(NBF)]
    cTi = [P.sb("sb_cTi%d" % i, [128, 128], F32) for i in range(NBF)]
    Br = P.sb("sb_Br", [128, 128], F32); Bi = P.sb("sb_Bi", [128, 128], F32); t1 = P.sb("sb_t1", [128, 128], F32)
    gsem = [P.dma_sem("s5g%d" % i) for i in range(NBF)]
    bur = P.sb("sb_bur", [128, ntok], F32); bui = P.sb("sb_bui", [128, ntok], F32)
    Cn = P.sb("sb_Cn", [128, ntok], F32); Sn = P.sb("sb_Sn", [128, ntok], F32)
    ta = P.sb("sb_ta", [128, ntok], F32); tb = P.sb("sb_tb", [128, ntok], F32)
    mr = P.sb("sb_mr", [128, ntok], F32); mi = P.sb("sb_mi", [128, ntok], F32)
    tint_v = V(mi.t[:, :].bitcast(mybir.dt.int32), mi)

    def load_pair(q):
        b = q % NBF
        g0 = 2 * q
        P.dma("sp", gsem[b], bTr[b][:, :].re("r (g p) -> r g p", g=2), V(io["s5_bT_re"].t[g0:g0 + 2].rearrange("g r p -> r g p"), io["s5_bT_re"]))
        P.dma("sp", gsem[b], bTi[b][:, :].re("r (g p) -> r g p", g=2), V(io["s5_bT_im"].t[g0:g0 + 2].rearrange("g r p -> r g p"), io["s5_bT_im"]))
        P.dma("sp", gsem[b], cTr[b][:], V(io["s5_cT_re"].t[g0:g0 + 2].rearrange("g p c -> (g p) c"), io["s5_cT_re"]))
        P.dma("sp", gsem[b], cTi[b][:], V(io["s5_cT_im"].t[g0:g0 + 2].rearrange("g p c -> (g p) c"), io["s5_cT_im"]))
        P.dma("sp", gsem[b], Qr[b][:], V(PRM.t[0].rearrange("(q t) p -> q (t p)", t=2)[q:q + 1, :].partition_broadcast(128), PRM))
        P.dma("sp", gsem[b], Qi[b][:], V(PRM.t[1].rearrange("(q t) p -> q (t p)", t=2)[q:q + 1, :].partition_broadcast(128), PRM))

    load_pair(0)
    for fc in range(KC):
        P.dma("sp", c.dq["xin"], u[:], HT[fc * 128:(fc + 1) * 128, :])
        for qi in range(4):
            q = fc * 4 + qi
            b = q % NBF
            if q + 1 < 64:
                load_pair(q + 1)
            P.tt(Br[:], bTr[b][:], Qr[b][:], ALU.mult)
            P.tt(t1[:], bTi[b][:], Qi[b][:], ALU.mult)
            P.tt(Br[:], Br[:], t1[:], ALU.subtract)
            P.tt(Bi[:], bTi[b][:], Qr[b][:], ALU.mult)
            P.tt(t1[:], bTr[b][:], Qi[b][:], ALU.mult)
            P.tt(Bi[:], Bi[:], t1[:], ALU.add)
            P.ts(cTi[b][:], cTi[b][:], -1.0, None, ALU.mult)
            for tbk in range(NT):
                sl = slice(tbk * 512, (tbk + 1) * 512)
                P.mm(psb[tbk % 2][:, :], Br[:], u[:, sl], True, True)
                P.mm(psb[2 + tbk % 2][:, :], Bi[:], u[:, sl], True, True)
                P.copy(bur[:, sl], psb[tbk % 2][:, :], eng="act")
                P.copy(bui[:, sl], psb[2 + tbk % 2][:, :], eng="act")
            P.ts(ta[:], iot[:], phiT[:, q:q + 1], None, ALU.mult)
            frac_pm_half(P, ta[:], ta[:], tint_v, mr[:])
            P.ts(tb[:], ta[:], 0.25, None, ALU.add)
            frac_pm_half(P, tb[:], tb[:], tint_v, mr[:])
            P.act(Sn[:], ta[:], AF.Sin, scale=2 * math.pi)
            P.act(Cn[:], tb[:], AF.Sin, scale=2 * math.pi)
            P.tt(mr[:], Cn[:], bur[:], ALU.mult)
            P.tt(ta[:], Sn[:], bui[:], ALU.mult, eng="pool")
            P.tt(mr[:], mr[:], ta[:], ALU.add)
            P.tt(mi[:], Cn[:], bui[:], ALU.mult)
            P.tt(tb[:], Sn[:], bur[:], ALU.mult, eng="pool")
            P.tt(mi[:], mi[:], tb[:], ALU.subtract)
            dec = V(magT.t[:, q:q + 1].broadcast_to([128, ntok]), magT)
            P.op("dve", lambda e: e.tensor_tensor_scan(out=bur.t[:], data0=dec.ap, data1=mr.t[:], initial=0.0,
                                                       op0=ALU.mult, op1=ALU.add), [dec, mr[:]], [bur[:]])
            P.op("dve", lambda e: e.tensor_tensor_scan(out=bui.t[:], data0=dec.ap, data1=mi.t[:], initial=0.0,
                                                       op0=ALU.mult, op1=ALU.add), [dec, mi[:]], [bui[:]])
            P.tt(mr[:], Cn[:], bur[:], ALU.mult)
            P.tt(ta[:], Sn[:], bui[:], ALU.mult, eng="pool")
            P.tt(mr[:], mr[:], ta[:], ALU.subtract)
            P.tt(mi[:], Cn[:], bui[:], ALU.mult)
            P.tt(tb[:], Sn[:], bur[:], ALU.mult, eng="pool")
            P.tt(mi[:], mi[:], tb[:], ALU.add)
            for tbk in range(NT):
                sl = slice(tbk * 512, (tbk + 1) * 512)
                pp = psb[4 + tbk % 4]
                P.mm(pp[:, :], cTr[b][:], mr[:, sl], True, False, sig=False)
                P.mm(pp[:, :], cTi[b][:], mi[:, sl], False, True)
                if qi == 0:
                    P.copy(ysb[:, sl], pp[:, :])
                else:
                    P.tt(ysb[:, sl], ysb[:, sl], pp[:, :], ALU.add)
        P.stt(ysb[:], u[:], dT[:, fc:fc + 1], ysb[:], ALU.mult, ALU.add)
        P.act(gb[:], ysb[:], AF.Gelu)
        P.dma("sp", c.dq["xout"], GT[fc * 128:(fc + 1) * 128, :], gb[:])
    P.barrier(); P.es = P_es; es.close()

    es = ExitStack(); P_es = P.es; P.es = es
    gT = P.sb("sc_gT", [128, KC, 512], BF16)
    wv = [P.sb("sc_wv%d" % i, [128, KC, 512], BF16) for i in range(2)]
    wg = [P.sb("sc_wg%d" % i, [128, KC, 512], BF16) for i in range(2)]
    xr = P.sb("sc_x", [128, 4, D], F32)
    sg = [P.sb("sc_sg%d" % i, [128, 512], F32) for i in range(2)]
    wgl = io["s5_w_glu"]
    wsem = [c.dq["w0"], c.dq["w1"]]
    n = 0
    for t0 in range(0, ntok, 512):
        P.dma("sp", c.dq["xin"], gT[:], V(GT.t[:, t0:t0 + 512].rearrange("(k p) t -> p k t", p=128), GT))
        for s in range(4):
            P.dma("sp", c.dq["xin"], xr[:, s, :], X[t0 + s * 128:t0 + (s + 1) * 128, :])
        for j in range(4):
            b = n % 2
            n += 1
            for hk in range(2):
                P.dma("pool", wsem[b], wv[b][:, hk * 8:(hk + 1) * 8, :],
                      V(wgl.t[0, :, j * 512:(j + 1) * 512].rearrange("(k p) n -> p k n", p=128)[:, hk * 8:(hk + 1) * 8, :], wgl))
                P.dma("pool", wsem[b], wg[b][:, hk * 8:(hk + 1) * 8, :],
                      V(wgl.t[0, :, D + j * 512:D + (j + 1) * 512].rearrange("(k p) n -> p k n", p=128)[:, hk * 8:(hk + 1) * 8, :], wgl))
            for s in range(4):
                pv = psb[(s % 2) * 2]
                pg = psb[(s % 2) * 2 + 1]
                for k in range(KC):
                    P.mm(pv[:, :], gT[:, k, s * 128:(s + 1) * 128], wv[b][:, k, :], start=(k == 0), stop=(k == KC - 1))
                for k in range(KC):
                    P.mm(pg[:, :], gT[:, k, s * 128:(s + 1) * 128], wg[b][:, k, :], start=(k == 0), stop=(k == KC - 1))
                P.act(sg[s % 2][:], pg[:, :], AF.Sigmoid)
                P.tt(sg[s % 2][:], sg[s % 2][:], pv[:, :], ALU.mult)
                P.tt(xr[:, s, j * 512:(j + 1) * 512], xr[:, s, j * 512:(j + 1) * 512], sg[s % 2][:], ALU.add)
        for s in range(4):
            P.dma("sp", c.dq["xout"], X[t0 + s * 128:t0 + (s + 1) * 128, :], xr[:, s, :])
    P.barrier(); P.es = P_es; es.close()


def stage_final(P, c, X, io, out):
    es = ExitStack()
    P_es = P.es
    P.es = es
    xt = [P.sb("f_x%d" % i, [128, D], F32) for i in range(2)]
    ht = [P.sb("f_h%d" % i, [128, D], F32) for i in range(2)]
    sq = P.sb("f_sq", [128, D], F32)
    stat = P.sb("f_stat", [128, 8], F32)
    P.dma("sp", c.dq["misc"], c.gvec[:], V(io["norm_final"].t.unsqueeze(0).partition_broadcast(128), io["norm_final"]))
    for i, r0 in enumerate(range(0, c.ntok, 128)):
        P.dma("sp", c.dq["xin"], xt[i % 2][:], X[r0:r0 + 128, :])
        rms_h(P, c, xt[i % 2][:], ht[i % 2][:], sq[:], stat[:])
        P.dma("sp", c.dq["xout"], out[r0:r0 + 128, :], ht[i % 2][:])
    P.es = P_es
    return es


IN_SHAPES = {
    "x": [4096, D],
    "norm_ffn": [2, D], "norm_mix": [2, D], "norm_final": [D],
    "moe_w_group": [2, D, 8], "moe_b_group": [2, 8], "moe_w_expert": [2, D, 64], "moe_b_expert": [2, 64],
    "moe_w_gu": [2, NE, D, 2 * FH], "moe_w_down": [2, NE, FH, D],
    "moe_wg_r": [2 * NE * 128, 16 * FH], "moe_wu_r": [2 * NE * 128, 16 * FH], "moe_wd_r": [2 * NE * 128, 6 * D],
    "s5_lam_re": [128, 64], "s5_lam_im": [128, 64], "s5_log_dt": [128, 1], "s5_dT": [128, 16],
    "s5_bT_re": [128, 128, 64], "s5_bT_im": [128, 128, 64], "s5_cT_re": [128, 64, 128], "s5_cT_im": [128, 64, 128],
    "s5_w_glu": [1, D, 2 * D],
    "ml_w_in": [1, D, 6160], "ml_b_gate": [1, 16], "ml_g_head": [1, D], "ml_w_out": [1, D, D],
}


def build(stages=("moe0",), ntok=4096, in_names=None):
    nc = bass.Bass("TRN2", target_bir_lowering=False)
    with ExitStack() as es:
        P = Prog(nc, es)
        io = {}
        for k in (in_names or IN_SHAPES.keys()):
            shp = list(IN_SHAPES[k])
            if k == "x":
                shp[0] = ntok
            if DBG_NE and k in ("moe_w_gu", "moe_w_down"):
                shp[1] = DBG_NE
            io[k] = P.dram(k, shp, F32, kind="ExternalInput")
        out = P.dram("out", [ntok, D], F32, kind="ExternalOutput")
        X = P.dram("Xs", [ntok, D], F32)
        c = setup_common(P, ntok)
        P.dma("sp", c.dq["misc"], X[:, :], io["x"][:, :])
        for st in stages:
            if st == "moe0":
                stage_moe(P, c, X, 0, io).close()
            elif st == "moe1":
                stage_moe(P, c, X, 1, io).close()
            elif st == "mlstm":
                stage_mlstm(P, c, X, io)
            elif st == "moeg0":
                stage_moe_g(P, c, X, 0, io)
            elif st == "moeg1":
                stage_moe_g(P, c, X, 1, io)
            elif st == "s5":
                stage_s5(P, c, X, io)
        stage_final(P, c, X, io, out).close()
        P.finish()
    return nc


def moe_layout(inp):
    o = {}
    wg = inp["moe_w_gu"].reshape(2, NE, 16, 128, 2, FH)
    o["moe_wg_r"] = np.ascontiguousarray(wg[:, :, :, :, 0, :].transpose(0, 1, 3, 2, 4)).reshape(2 * NE * 128, 16 * FH)
    o["moe_wu_r"] = np.ascontiguousarray(wg[:, :, :, :, 1, :].transpose(0, 1, 3, 2, 4)).reshape(2 * NE * 128, 16 * FH)
    wd = inp["moe_w_down"].reshape(2, NE, 6, 128, D)
    o["moe_wd_r"] = np.ascontiguousarray(wd.transpose(0, 1, 3, 2, 4)).reshape(2 * NE * 128, 6 * D)
    return o


def s5_layout(inp):
    G, PS, CG = 128, 64, 16
    o = {}
    o["s5_lam_re"] = np.ascontiguousarray(inp["s5_lam_re"][0])
    o["s5_lam_im"] = np.ascontiguousarray(inp["s5_lam_im"][0])
    o["s5_log_dt"] = np.ascontiguousarray(inp["s5_log_dt"][0].reshape(G, 1))
    o["s5_dT"] = np.ascontiguousarray(inp["s5_d"][0].reshape(16, 128).T)
    for nm, src in (("s5_bT_re", "s5_b_re"), ("s5_bT_im", "s5_b_im")):
        a = np.zeros((G, 128, PS), np.float32)
        bt = np.transpose(inp[src][0], (0, 2, 1))
        for g in range(G):
            gi = g % 8
            a[g, gi * CG:(gi + 1) * CG, :] = bt[g]
        o[nm] = a
    for nm, src in (("s5_cT_re", "s5_c_re"), ("s5_cT_im", "s5_c_im")):
        a = np.zeros((G, PS, 128), np.float32)
        ct = np.transpose(inp[src][0], (0, 2, 1))
        for g in range(G):
            gi = g % 8
            a[g, :, gi * CG:(gi + 1) * CG] = ct[g]
        o[nm] = a
    o["s5_w_glu"] = inp["s5_w_glu"]
    return o


_NC_CACHE = {}


def kernel(**inputs):
    inp = {k: np.asarray(v) for k, v in inputs.items()}
    B = inp["x"].shape[0]
    stages = ("s5", "moeg0", "mlstm", "moeg1")
    shared = s5_layout(inp)
    shared.update(moe_layout(inp))
    for k in ["norm_ffn", "norm_mix", "norm_final", "moe_w_group", "moe_b_group", "moe_w_expert", "moe_b_expert",
              "ml_w_in", "ml_b_gate", "ml_g_head", "ml_w_out"]:
        shared[k] = np.ascontiguousarray(inp[k])
    names = ["x"] + list(shared.keys())
    if "nc" not in _NC_CACHE:
        _NC_CACHE["nc"] = build(stages=stages, ntok=inp["x"].shape[1], in_names=names)
    nc = _NC_CACHE["nc"]
    in_maps = []
    for b in range(B):
        m = dict(shared)
        m["x"] = np.ascontiguousarray(inp["x"][b])
        in_maps.append(m)
    res = run_bass_kernel_spmd(nc, in_maps, core_ids=list(range(B)))
    return np.stack([np.asarray(r["out"]) for r in res.results], axis=0).astype(np.float32)
```

```python
from contextlib import ExitStack
import numpy as np
import concourse.bass as bass
import concourse.mybir as mybir
from concourse.bass_utils import run_bass_kernel_spmd

F32 = mybir.dt.float32
BF16 = mybir.dt.bfloat16
AF = mybir.ActivationFunctionType
ALU = mybir.AluOpType
AX = mybir.AxisListType

D = 2048
EPS = 1e-6
NE = 64
import os
DBG_CUT = int(os.environ.get('DBG_CUT', '0'))
DBG_OUT = int(os.environ.get('DBG_OUT', '0'))
DBG_NE = int(os.environ.get('DBG_NE', '0'))
FH = 768


class Buf:
    def __init__(self, t, name):
        self.t = t
        self.name = name
        self.w = None
        self.r = []

    def __getitem__(self, k):
        return V(self.t[k], self)


class V:
    def __init__(self, ap, buf):
        self.ap = ap
        self.buf = buf

    def re(self, s, **kw):
        return V(self.ap.rearrange(s, **kw), self.buf)

    def __getitem__(self, k):
        return V(self.ap[k], self.buf)


class Prog:
    def __init__(self, nc, es):
        self.nc = nc
        self.es = es
        self.eng = {"pe": nc.tensor, "act": nc.scalar, "dve": nc.vector, "pool": nc.gpsimd, "sp": nc.sync}
        self.sem = {}
        self.cnt = {}
        self.waited = {}
        for k in self.eng:
            self.sem[k] = es.enter_context(nc.semaphore("s_" + k))
            self.cnt[k] = 0
        self.ndma = 0

    def dma_sem(self, name):
        key = "d_" + name
        self.sem[key] = self.es.enter_context(self.nc.semaphore(key))
        self.cnt[key] = 0
        return key

    def sb(self, name, shape, dt):
        return Buf(self.es.enter_context(self.nc.sbuf_tensor(name, list(shape), dt)), name)

    def ps(self, name, shape, dt=F32):
        return Buf(self.es.enter_context(self.nc.psum_tensor(name, list(shape), dt)), name)

    def dram(self, name, shape, dt, kind="Internal"):
        if DBG_OUT and kind == "Internal" and name != "Xs":
            kind = "ExternalOutput"
        t = self.nc.dram_tensor(name, list(shape), dt, kind=kind)
        return Buf(t.ap(), name)

    def _deps(self, reads, writes):
        deps = {}
        def add(d):
            if d is None:
                return
            k, s = d
            if deps.get(k, 0) < s:
                deps[k] = s
        for v in reads:
            add(v.buf.w)
        for v in writes:
            add(v.buf.w)
            for d in v.buf.r:
                add(d)
        return deps

    def _wait(self, ek, deps):
        e = self.eng[ek]
        for k, s in deps.items():
            if k == ek and ek in ("pe", "sp"):
                continue
            if self.waited.get((ek, k), 0) >= s:
                continue
            mult = 1
            if k.startswith("d_"):
                mult = 16
                s = self.cnt[k]
            e.wait_ge(self.sem[k], s * mult)
            self.waited[(ek, k)] = s

    def _record(self, key, seq, reads, writes):
        for v in reads:
            v.buf.r.append((key, seq))
            if len(v.buf.r) > 64:
                m = {}
                for k, s in v.buf.r:
                    if m.get(k, 0) < s:
                        m[k] = s
                v.buf.r = list(m.items())
        for v in writes:
            v.buf.w = (key, seq)
            v.buf.r = []

    def op(self, ek, fn, reads, writes, sig=True):
        self._wait(ek, self._deps(reads, writes))
        ins = fn(self.eng[ek])
        seq = self.cnt[ek] + 1
        if sig:
            ins.then_inc(self.sem[ek], 1)
            self.cnt[ek] = seq
        self._record(ek, seq, reads, writes)
        return ins

    def dma(self, qk, dkey, out, in_, **kw):
        self._wait(qk, self._deps([in_], [out]))
        ins = self.eng[qk].dma_start(out=out.ap, in_=in_.ap, **kw)
        ins.then_inc(self.sem[dkey], 16)
        self.cnt[dkey] += 1
        self._record(dkey, self.cnt[dkey], [in_], [out])
        self.ndma += 1
        return ins

    def mm(self, out, lhsT, rhs, start, stop, sig=None):
        if sig is None:
            sig = stop
        return self.op("pe", lambda e: e.matmul(out.ap, lhsT.ap, rhs.ap, start=start, stop=stop),
                       [lhsT, rhs], [out], sig=sig)

    def tr(self, out, in_, ident, sig=True):
        return self.op("pe", lambda e: e.transpose(out.ap, in_.ap, ident.ap), [in_, ident], [out], sig=sig)

    def act(self, out, in_, func, bias=None, scale=None, accum=None, eng="act"):
        kw = {}
        reads = [in_]
        writes = [out]
        if bias is not None:
            if isinstance(bias, V):
                kw["bias"] = bias.ap
                reads.append(bias)
            else:
                kw["bias"] = bias
        if scale is not None:
            if isinstance(scale, V):
                kw["scale"] = scale.ap
                reads.append(scale)
            else:
                kw["scale"] = scale
        if accum is not None:
            kw["accum_out"] = accum.ap
            writes.append(accum)
        return self.op(eng, lambda e: e.activation(out=out.ap, in_=in_.ap, func=func, **kw), reads, writes)

    def ts(self, out, in0, s1, s2, op0, op1=None, eng="dve", accum=None):
        reads = [in0]
        writes = [out]
        a1 = s1
        a2 = s2
        if isinstance(s1, V):
            reads.append(s1)
            a1 = s1.ap
        if isinstance(s2, V):
            reads.append(s2)
            a2 = s2.ap
        kw = {}
        if op1 is not None:
            kw["op1"] = op1
        if accum is not None:
            kw["accum_out"] = accum.ap
            writes.append(accum)
        return self.op(eng, lambda e: e.tensor_scalar(out=out.ap, in0=in0.ap, scalar1=a1, scalar2=a2, op0=op0, **kw),
                       reads, writes)

    def tt(self, out, in0, in1, op, eng="dve"):
        return self.op(eng, lambda e: e.tensor_tensor(out=out.ap, in0=in0.ap, in1=in1.ap, op=op), [in0, in1], [out])

    def stt(self, out, in0, scalar, in1, op0, op1, eng="dve"):
        reads = [in0, in1]
        a = scalar
        if isinstance(scalar, V):
            reads.append(scalar)
            a = scalar.ap
        return self.op(eng, lambda e: e.scalar_tensor_tensor(out=out.ap, in0=in0.ap, scalar=a, in1=in1.ap,
                                                              op0=op0, op1=op1), reads, [out])

    def copy(self, out, in_, eng="dve"):
        if eng == "act":
            return self.op("act", lambda e: e.copy(out=out.ap, in_=in_.ap), [in_], [out])
        return self.op(eng, lambda e: e.tensor_copy(out=out.ap, in_=in_.ap), [in_], [out])

    def memset(self, out, val, eng="dve"):
        return self.op(eng, lambda e: e.memset(out.ap, val), [], [out])

    def red(self, out, in_, op, eng="dve"):
        return self.op(eng, lambda e: e.tensor_reduce(out=out.ap, in_=in_.ap, axis=AX.X, op=op), [in_], [out])

    def dbg(self, name, v, shape, dt):
        if not DBG_OUT:
            return
        if not hasattr(self, "_dq"):
            self._dq = self.dma_sem("dbg")
        t = self.nc.dram_tensor("dbg_" + name, list(shape), dt, kind="ExternalOutput")
        self.dma("sp", self._dq, V(t.ap(), Buf(t.ap(), name)), v)

    def barrier(self):
        for ek, e in self.eng.items():
            for k, c in self.cnt.items():
                if c > 0 and k != ek and self.waited.get((ek, k), 0) < c:
                    e.wait_ge(self.sem[k], c * (16 if k.startswith("d_") else 1))
                    self.waited[(ek, k)] = c

    def finish(self):
        e = self.eng["sp"]
        for k, c in self.cnt.items():
            if c > 0 and k != "sp":
                e.wait_ge(self.sem[k], c * (16 if k.startswith("d_") else 1))


class Ctx:
    pass


def setup_common(P, ntok):
    c = Ctx()
    c.ntok = ntok
    c.ident = P.sb("ident", [128, 128], F32)
    c.identb = P.sb("identb", [128, 128], BF16)
    nc = P.nc
    P.memset(c.ident[:], 1.0, eng="pool")
    P.op("pool", lambda e: e.affine_select(out=c.ident.t[:], in_=c.ident.t[:], pattern=[[-1, 128]],
                                           compare_op=ALU.is_equal, fill=0.0, base=0, channel_multiplier=1),
         [c.ident[:]], [c.ident[:]])
    P.copy(c.identb[:], c.ident[:], eng="pool")
    c.psb = [P.ps("psb%d" % i, [128, 512], F32) for i in range(8)]
    c.gvec = P.sb("gvec", [128, D], F32)
    c.dq = {k: P.dma_sem(k) for k in ["misc", "xin", "xout", "w0", "w1", "w2", "w3"]}
    return c


def rms_h(P, c, xt, ht, tmp, stat):
    P.act(tmp, xt, AF.Square, accum=stat[:, 0:1])
    P.ts(stat[:, 1:2], stat[:, 0:1], 1.0 / D, EPS, ALU.mult, ALU.add)
    P.act(stat[:, 3:4], stat[:, 1:2], AF.Sqrt)
    P.op("dve", lambda e: e.reciprocal(out=stat.buf.t[:, 2:3], in_=stat.buf.t[:, 3:4]), [stat], [stat])
    P.stt(ht, xt, stat[:, 2:3], c.gvec[:], ALU.mult, ALU.mult)


def stage_moe(P, c, X, layer, io, TT=512):
    ntok = c.ntok
    nsub = TT // 128
    KC = D // 128
    es = ExitStack()
    P_es = P.es
    P.es = es
    _sb0 = P.sb
    P.sb = lambda name, shape, dt: _sb0("%s_l%d" % (name, layer), shape, dt)
    acc = P.sb("m_acc", [128, nsub, D], F32)
    h32 = P.sb("m_h32", [128, D], F32)
    sq = P.sb("m_sq", [128, D], F32)
    stat = P.sb("m_stat", [128, 8], F32)
    hT32 = P.sb("m_hT32", [128, KC, 128], F32)
    hTb = P.sb("m_hTb", [128, KC, TT], BF16)
    actT = P.sb("m_actT", [128, 6, TT], BF16)
    sil = [P.sb("m_sil%d" % i, [128, TT], F32) for i in range(2)]
    wr = P.sb("m_wr", [128, KC, 72], F32)
    br = P.sb("m_br", [128, 72], F32)
    G = P.sb("m_G", [128, nsub, NE], F32)
    lg = P.sb("m_lg", [128, 72], F32)
    lem = P.sb("m_lem", [128, NE], F32)
    lem2 = P.sb("m_lem2", [128, NE], F32)
    msk = P.sb("m_msk", [128, NE], F32)
    sm = P.sb("m_sm", [128, 16], F32)
    wslot = [P.sb("m_w%d" % i, [128, 6 * D], BF16) for i in range(4)]
    wq = ["w0", "w1", "w2", "w3"]
    psb = c.psb

    P.dma("sp", c.dq["misc"], c.gvec[:], V(io["norm_ffn"].t[layer:layer + 1, :].partition_broadcast(128), io["norm_ffn"]))
    P.dma("sp", c.dq["misc"], wr[:, :, 0:8], V(io["moe_w_group"].t[layer].rearrange("(k p) n -> p k n", p=128), io["moe_w_group"]))
    P.dma("sp", c.dq["misc"], wr[:, :, 8:72], V(io["moe_w_expert"].t[layer].rearrange("(k p) n -> p k n", p=128), io["moe_w_expert"]))
    P.dma("sp", c.dq["misc"], br[:, 0:8], V(io["moe_b_group"].t[layer:layer + 1, :].partition_broadcast(128), io["moe_b_group"]))
    P.dma("sp", c.dq["misc"], br[:, 8:72], V(io["moe_b_expert"].t[layer:layer + 1, :].partition_broadcast(128), io["moe_b_expert"]))

    wgu = io.get("moe_w_gu")
    wdn = io.get("moe_w_down")
    slot_i = [0]

    def load_piece(e, piece):
        s = slot_i[0] % 4
        slot_i[0] += 1
        dst = wslot[s]
        if piece < 2:
            src = wgu.t[layer, e, :, piece * FH:(piece + 1) * FH].rearrange("(k p) f -> p k f", p=128)
            dv = dst[:, :].re("p (k f) -> p k f", k=KC)
            for hk in range(2):
                P.dma("pool", c.dq[wq[s]], dv[:, hk * 8:(hk + 1) * 8, :], V(src[:, hk * 8:(hk + 1) * 8, :], wgu))
        else:
            src = wdn.t[layer, e].rearrange("(k p) d -> p k d", p=128)
            dv = dst[:, :].re("p (k d) -> p k d", k=6)
            for hk in range(2):
                P.dma("pool", c.dq[wq[s]], dv[:, hk * 3:(hk + 1) * 3, :], V(src[:, hk * 3:(hk + 1) * 3, :], wdn))
        return dst

    for t0 in range(0, ntok, TT):
        for s in range(nsub):
            r0 = t0 + s * 128
            P.dma("sp", c.dq["xin"], acc[:, s, :], X[r0:r0 + 128, :])
            rms_h(P, c, acc[:, s, :], h32[:], sq[:], stat[:])
            if DBG_CUT == 2:
                continue
            for q in range(4):
                for j in range(4):
                    k = q * 4 + j
                    P.tr(psb[q][:, j * 128:(j + 1) * 128], h32[:, k * 128:(k + 1) * 128], c.ident[:], sig=(j == 3))
                if DBG_CUT == 6:
                    continue
                P.copy(hT32[:, q * 4:(q + 1) * 4, :], psb[q][:, :].re("p (j t) -> p j t", j=4), eng="act")
                if DBG_CUT == 7:
                    continue
                P.copy(hTb[:, q * 4:(q + 1) * 4, s * 128:(s + 1) * 128], hT32[:, q * 4:(q + 1) * 4, :], eng="dve")
            if DBG_CUT in (3, 6, 7):
                continue
            for k in range(KC):
                P.mm(psb[4][:, 0:72], hT32[:, k, :], wr[:, k, :], start=(k == 0), stop=(k == KC - 1))
            if DBG_CUT == 4:
                continue
            P.tt(lg[:], psb[4][:, 0:72], br[:], ALU.add)
            P.red(sm[:, 0:1], lg[:, 0:8], ALU.max)
            P.ts(sm[:, 1:2], sm[:, 0:1], -1.0, None, ALU.mult)
            P.act(msk[:, 0:8], lg[:, 0:8], AF.Exp, bias=sm[:, 1:2], accum=sm[:, 2:3])
            P.op("dve", lambda e: e.reciprocal(out=sm.t[:, 3:4], in_=sm.t[:, 2:3]), [sm[:]], [sm[:]])
            P.ts(msk[:, 8:16], lg[:, 0:8], sm[:, 0:1], None, ALU.is_ge)
            P.ts(msk[:, 8:16], msk[:, 8:16], 1e30, -1e30, ALU.mult, ALU.add)
            P.tt(lem[:, :].re("p (g e) -> p g e", g=8), lg[:, 8:72].re("p (g e) -> p g e", g=8),
                 V(msk.t[:, 8:16].unsqueeze(2).broadcast_to([128, 8, 8]), msk), ALU.add)
            if DBG_CUT == 5:
                continue
            P.red(sm[:, 4:5], lem[:], ALU.max)
            P.ts(msk[:], lem[:], sm[:, 4:5], None, ALU.is_ge)
            P.stt(lem2[:], msk[:], -1e30, lem[:], ALU.mult, ALU.add)
            P.red(sm[:, 5:6], lem2[:], ALU.max)
            P.ts(lem2[:], lem2[:], sm[:, 5:6], None, ALU.is_ge)
            P.tt(sm[:, 6:7], sm[:, 5:6], sm[:, 4:5], ALU.subtract)
            P.act(sm[:, 7:8], sm[:, 6:7], AF.Exp)
            P.ts(sm[:, 8:9], sm[:, 7:8], 1.0, None, ALU.add)
            P.op("dve", lambda e: e.reciprocal(out=sm.t[:, 9:10], in_=sm.t[:, 8:9]), [sm[:]], [sm[:]])
            P.tt(sm[:, 10:11], sm[:, 9:10], sm[:, 3:4], ALU.mult)
            P.tt(sm[:, 11:12], sm[:, 10:11], sm[:, 7:8], ALU.mult)
            P.ts(msk[:], msk[:], sm[:, 10:11], None, ALU.mult)
            P.stt(G[:, s, :], lem2[:], sm[:, 11:12], msk[:], ALU.mult, ALU.add)
        for e in range((DBG_NE or NE) if not DBG_CUT else 0):
            wg = load_piece(e, 0)
            wu = load_piece(e, 1)
            wd = load_piece(e, 2)
            wgv = wg[:, :].re("p (k f) -> p k f", k=KC)
            wuv = wu[:, :].re("p (k f) -> p k f", k=KC)
            wdv = wd[:, :].re("p (k d) -> p k d", k=6)
            for fc in range(6):
                pg = psb[(fc % 2) * 2]
                pu = psb[(fc % 2) * 2 + 1]
                for k in range(KC):
                    P.mm(pg[:, 0:TT], wgv[:, k, fc * 128:(fc + 1) * 128], hTb[:, k, :], start=(k == 0), stop=(k == KC - 1))
                for k in range(KC):
                    P.mm(pu[:, 0:TT], wuv[:, k, fc * 128:(fc + 1) * 128], hTb[:, k, :], start=(k == 0), stop=(k == KC - 1))
                P.act(sil[fc % 2][:, :], pg[:, 0:TT], AF.Silu)
                P.tt(actT[:, fc, :], sil[fc % 2][:, :], pu[:, 0:TT], ALU.mult)
            i = 0
            for s in range(nsub):
                for dg in range(4):
                    pd = psb[4 + (i % 4)]
                    i += 1
                    for fc in range(6):
                        P.mm(pd[:, :], actT[:, fc, s * 128:(s + 1) * 128], wdv[:, fc, dg * 512:(dg + 1) * 512],
                             start=(fc == 0), stop=(fc == 5))
                    P.stt(acc[:, s, dg * 512:(dg + 1) * 512], pd[:, :], G[:, s, e:e + 1],
                          acc[:, s, dg * 512:(dg + 1) * 512], ALU.mult, ALU.add)
        for s in range(nsub):
            r0 = t0 + s * 128
            P.dma("sp", c.dq["xout"], X[r0:r0 + 128, :], acc[:, s, :])
    P.barrier()
    P.es = P_es
    P.sb = _sb0
    return es


I32 = mybir.dt.int32


def floor_pos(P, out, x, itmp, ftmp):
    P.copy(itmp, x)
    P.copy(out, itmp)
    P.tt(ftmp, out, x, ALU.is_gt)
    P.tt(out, out, ftmp, ALU.subtract)


def stage_moe_g(P, c, X, layer, io, TS=512):
    ntok = c.ntok
    nsub = TS // 128
    KC = D // 128
    NTI = ntok // 128
    NT = ntok // TS + 8
    NSLOT = NT * TS
    psb = c.psb
    L_ = "_g%d" % layer
    HS = P.dram("mg_HS" + L_, [NSLOT, D], BF16)
    GS = P.dram("mg_GS" + L_, [NSLOT, 8], F32)
    YS = P.dram("mg_YS" + L_, [NSLOT, D], F32)
    wsrc = [io["moe_wg_r"], io["moe_wu_r"], io["moe_wd_r"]]
    eso = ExitStack(); P_eso = P.es; P.es = eso
    slot_i = P.sb("mg_slot" + L_, [128, NTI], I32)
    idxW = P.sb("mg_idxW" + L_, [128, NT * 8], I32)
    P.es = P_eso
    es = ExitStack(); P_es = P.es; P.es = es
    _sb0 = P.sb
    P.sb = lambda name, shape, dt: _sb0(name + L_, shape, dt)
    hall = P.sb("g_hall", [128, NTI, D], BF16)
    xt = P.sb("g_xt", [128, D], F32)
    h32 = P.sb("g_h32", [128, D], F32)
    sq = P.sb("g_sq", [128, D], F32)
    stat = P.sb("g_stat", [128, 8], F32)
    hT32 = P.sb("g_hT32", [128, KC, 128], F32)
    wr = P.sb("g_wr", [128, KC, 72], F32)
    br = P.sb("g_br", [128, 72], F32)
    G = P.sb("g_G", [128, NE], F32)
    lg = P.sb("g_lg", [128, 72], F32)
    lem = P.sb("g_lem", [128, NE], F32)
    lem2 = P.sb("g_lem2", [128, NE], F32)
    msk = P.sb("g_msk", [128, NE], F32)
    sm = P.sb("g_sm", [128, 16], F32)
    OH = P.sb("g_OH", [128, NTI, 8], F32)
    G8 = P.sb("g_G8", [128, NTI, 8], F32)
    R = P.sb("g_R", [128, NTI, 8], F32)
    OHs = P.sb("g_OHs", [128, 8], F32)
    Ls = P.sb("g_Ls", [128, 128], F32)
    ones = P.sb("g_ones", [128, 128], F32)
    zb = P.sb("g_zb", [128, D], BF16)
    sc = P.sb("g_sc", [128, 64], F32)
    sci = P.sb("g_sci", [128, 64], I32)
    jv = P.sb("g_jv", [128, NT], F32)
    gidf = P.sb("g_gidf", [128, NT], F32)
    idf = P.sb("g_idf", [128, NT, 8], F32)
    Tt = P.sb("g_Tt", [128, NTI, 8], F32)
    slf = P.sb("g_slf", [128, NTI], F32)
    P.memset(Ls[:], 1.0, eng="pool")
    P.op("pool", lambda e: e.affine_select(out=Ls.t[:], in_=Ls.t[:], pattern=[[1, 128]], compare_op=ALU.is_gt,
                                           fill=0.0, base=0, channel_multiplier=-1), [Ls[:]], [Ls[:]])
    P.memset(ones[:], 1.0)
    P.memset(OHs[:], 0.0)
    P.memset(zb[:], 0.0)
    P.op("pool", lambda e: e.iota(jv.t[:], pattern=[[1, NT]], base=0, channel_multiplier=0,
                                  allow_small_or_imprecise_dtypes=True), [], [jv[:]])
    P.op("pool", lambda e: e.iota(idf.t[:], pattern=[[0, NT], [128, 8]], base=layer * NE * 128, channel_multiplier=1,
                                  allow_small_or_imprecise_dtypes=True), [], [idf[:]])
    for r0 in range(0, NSLOT, 128):
        P.dma("sp", c.dq["xout"], HS[r0:r0 + 128, :], zb[:])
    P.dma("sp", c.dq["xout"], V(GS.t.rearrange("(a p) e -> p a e", p=128), GS),
          V(zb.t[:, 0:NSLOT // 128 * 8 * 2].bitcast(F32).rearrange("p (a e) -> p a e", e=8), zb))
    P.dma("sp", c.dq["misc"], c.gvec[:], V(io["norm_ffn"].t[layer:layer + 1, :].partition_broadcast(128), io["norm_ffn"]))
    P.dma("sp", c.dq["misc"], wr[:, :, 0:8], V(io["moe_w_group"].t[layer].rearrange("(k p) n -> p k n", p=128), io["moe_w_group"]))
    P.dma("sp", c.dq["misc"], wr[:, :, 8:72], V(io["moe_w_expert"].t[layer].rearrange("(k p) n -> p k n", p=128), io["moe_w_expert"]))
    P.dma("sp", c.dq["misc"], br[:, 0:8], V(io["moe_b_group"].t[layer:layer + 1, :].partition_broadcast(128), io["moe_b_group"]))
    P.dma("sp", c.dq["misc"], br[:, 8:72], V(io["moe_b_expert"].t[layer:layer + 1, :].partition_broadcast(128), io["moe_b_expert"]))
    for i in range(NTI):
        r0 = i * 128
        P.dma("sp", c.dq["xin"], xt[:], X[r0:r0 + 128, :])
        rms_h(P, c, xt[:], h32[:], sq[:], stat[:])
        P.copy(hall[:, i, :], h32[:], eng="pool")
        for q in range(4):
            for j in range(4):
                k = q * 4 + j
                P.tr(psb[q][:, j * 128:(j + 1) * 128], h32[:, k * 128:(k + 1) * 128], c.ident[:], sig=(j == 3))
            P.copy(hT32[:, q * 4:(q + 1) * 4, :], psb[q][:, :].re("p (j t) -> p j t", j=4), eng="act")
        for k in range(KC):
            P.mm(psb[4][:, 0:72], hT32[:, k, :], wr[:, k, :], start=(k == 0), stop=(k == KC - 1))
        P.tt(lg[:], psb[4][:, 0:72], br[:], ALU.add)
        P.red(sm[:, 0:1], lg[:, 0:8], ALU.max)
        P.ts(sm[:, 1:2], sm[:, 0:1], -1.0, None, ALU.mult)
        P.act(msk[:, 0:8], lg[:, 0:8], AF.Exp, bias=sm[:, 1:2], accum=sm[:, 2:3])
        P.op("dve", lambda e: e.reciprocal(out=sm.t[:, 3:4], in_=sm.t[:, 2:3]), [sm[:]], [sm[:]])
        P.ts(OH[:, i, :], lg[:, 0:8], sm[:, 0:1], None, ALU.is_ge)
        P.ts(msk[:, 8:16], OH[:, i, :], 1e30, -1e30, ALU.mult, ALU.add)
        P.tt(lem[:, :].re("p (g e) -> p g e", g=8), lg[:, 8:72].re("p (g e) -> p g e", g=8),
             V(msk.t[:, 8:16].unsqueeze(2).broadcast_to([128, 8, 8]), msk), ALU.add)
        P.red(sm[:, 4:5], lem[:], ALU.max)
        P.ts(msk[:], lem[:], sm[:, 4:5], None, ALU.is_ge)
        P.stt(lem2[:], msk[:], -1e30, lem[:], ALU.mult, ALU.add)
        P.red(sm[:, 5:6], lem2[:], ALU.max)
        P.ts(lem2[:], lem2[:], sm[:, 5:6], None, ALU.is_ge)
        P.tt(sm[:, 6:7], sm[:, 5:6], sm[:, 4:5], ALU.subtract)
        P.act(sm[:, 7:8], sm[:, 6:7], AF.Exp)
        P.ts(sm[:, 8:9], sm[:, 7:8], 1.0, None, ALU.add)
        P.op("dve", lambda e: e.reciprocal(out=sm.t[:, 9:10], in_=sm.t[:, 8:9]), [sm[:]], [sm[:]])
        P.tt(sm[:, 10:11], sm[:, 9:10], sm[:, 3:4], ALU.mult)
        P.tt(sm[:, 11:12], sm[:, 10:11], sm[:, 7:8], ALU.mult)
        P.ts(msk[:], msk[:], sm[:, 10:11], None, ALU.mult)
        P.stt(G[:], lem2[:], sm[:, 11:12], msk[:], ALU.mult, ALU.add)
        P.red(G8[:, i, :], G[:, :].re("p (g e) -> p e g", g=8), ALU.add)
        P.mm(psb[5][:, 0:8], Ls[:], OH[:, i, :], True, False, sig=False)
        P.mm(psb[5][:, 0:8], ones[:], OHs[:], False, True)
        P.copy(R[:, i, :], psb[5][:, 0:8])
        P.tt(OHs[:], OHs[:], OH[:, i, :], ALU.add)
    P.mm(psb[5][:, 8:16], ones[:], OHs[:], True, True)
    P.ts(sc[:, 0:8], psb[5][:, 8:16], float(TS - 1), 1.0 / TS, ALU.add, ALU.mult)
    floor_pos(P, sc[:, 8:16], sc[:, 0:8], sci[:, 0:8], sc[:, 16:24])
    P.copy(sc[:, 24:25], sc[:, 8:9])
    for g in range(1, 8):
        P.tt(sc[:, 24 + g:25 + g], sc[:, 23 + g:24 + g], sc[:, 8 + g:9 + g], ALU.add)
    P.tt(sc[:, 32:40], sc[:, 24:32], sc[:, 8:16], ALU.subtract)
    P.ts(sc[:, 32:40], sc[:, 32:40], float(TS), None, ALU.mult)
    P.tt(Tt[:], R[:], V(sc.t[:, 32:40].unsqueeze(1).broadcast_to([128, NTI, 8]), sc), ALU.add)
    P.tt(Tt[:], Tt[:], OH[:], ALU.mult)
    P.red(slf[:], Tt[:], ALU.add)
    P.copy(slot_i[:], slf[:])
    P.ts(gidf[:], jv[:], sc[:, 24:25], None, ALU.is_ge)
    for g in range(1, 8):
        P.stt(gidf[:], jv[:], sc[:, 24 + g:25 + g], gidf[:], ALU.is_ge, ALU.add)
    P.ts(gidf[:], gidf[:], 7.0, 1024.0, ALU.min, ALU.mult)
    P.tt(idf[:], idf[:], V(gidf.t[:, :].unsqueeze(2).broadcast_to([128, NT, 8]), gidf), ALU.add)
    P.copy(idxW[:, :].re("p (j e) -> p j e", e=8), idf[:])
    for i in range(NTI):
        for (dst, src) in ((HS, hall[:, i, :]), (GS, G8[:, i, :])):
            P._wait("pool", P._deps([src, slot_i[:]], [dst[:, :]]))
            ins = P.nc.gpsimd.indirect_dma_start(out=dst.t[:, :], out_offset=bass.IndirectOffsetOnAxis(ap=slot_i.t[:, i:i + 1], axis=0),
                                                 in_=src.ap, in_offset=None)
            ins.then_inc(P.sem[c.dq["w3"]], 16); P.cnt[c.dq["w3"]] += 1
            P._record(c.dq["w3"], P.cnt[c.dq["w3"]], [src, slot_i[:]], [dst[:, :]])
    P.barrier(); P.es = P_es; P.sb = _sb0; es.close()

    es = ExitStack(); P_es = P.es; P.es = es
    _sb0 = P.sb
    P.sb = lambda name, shape, dt: _sb0(name + L_, shape, dt)
    acc = P.sb("h_acc", [128, nsub, D], F32)
    hs = P.sb("h_hs", [128, nsub, D], BF16)
    hTb = P.sb("h_hTb", [128, KC, TS], BF16)
    actT = P.sb("h_actT", [128, 6, TS], BF16)
    sil = [P.sb("h_sil%d" % i, [128, TS], F32) for i in range(2)]
    gst = P.sb("h_gst", [128, nsub, 8], F32)
    wslot = [P.sb("h_w%d" % i, [128, 6 * D], BF16) for i in range(4)]
    wq = ["w0", "w1", "w2", "w3"]
    slot_n = [0]
    pbf = [V(psb[i].t[:, :].bitcast(BF16), psb[i]) for i in range(2)]

    def gather_piece(j, e, piece):
        s_ = slot_n[0] % 4
        slot_n[0] += 1
        dst = wslot[s_]
        ixa = idxW.t[:, j * 8 + e:j * 8 + e + 1]
        src = wsrc[piece]
        P._wait("pool", P._deps([idxW[:]], [dst[:]]))
        ins = P.nc.gpsimd.indirect_dma_start(out=dst.t[:, :], out_offset=None, in_=src.t[:, :],
                                             in_offset=bass.IndirectOffsetOnAxis(ap=ixa, axis=0))
        ins.then_inc(P.sem[c.dq[wq[s_]]], 16); P.cnt[c.dq[wq[s_]]] += 1
        P._record(c.dq[wq[s_]], P.cnt[c.dq[wq[s_]]], [src[:, :], idxW[:]], [dst[:]])
        return dst

    for j in range(NT):
        t0 = j * TS
        P.dma("sp", c.dq["xin"], hs[:], V(HS.t[t0:t0 + TS, :].rearrange("(s p) d -> p s d", p=128), HS))
        P.dma("sp", c.dq["xin"], gst[:], V(GS.t[t0:t0 + TS, :].rearrange("(s p) e -> p s e", p=128), GS))
        for s in range(nsub):
            for q in range(2):
                for jj in range(8):
                    k = q * 8 + jj
                    P.tr(pbf[q][:, jj * 128:(jj + 1) * 128], hs[:, s, k * 128:(k + 1) * 128], c.identb[:], sig=(jj == 7))
                P.copy(hTb[:, q * 8:(q + 1) * 8, s * 128:(s + 1) * 128], pbf[q][:, :].re("p (j t) -> p j t", j=8), eng="act")
        for e in range(8):
            wg = gather_piece(j, e, 0)
            wu = gather_piece(j, e, 1)
            wd = gather_piece(j, e, 2)
            wgv = wg[:, :].re("p (k f) -> p k f", k=KC)
            wuv = wu[:, :].re("p (k f) -> p k f", k=KC)
            wdv = wd[:, :].re("p (k d) -> p k d", k=6)
            for fc in range(6):
                pg = psb[(fc % 2) * 2]
                pu = psb[(fc % 2) * 2 + 1]
                for k in range(KC):
                    P.mm(pg[:, 0:TS], wgv[:, k, fc * 128:(fc + 1) * 128], hTb[:, k, :], start=(k == 0), stop=(k == KC - 1))
                for k in range(KC):
                    P.mm(pu[:, 0:TS], wuv[:, k, fc * 128:(fc + 1) * 128], hTb[:, k, :], start=(k == 0), stop=(k == KC - 1))
                P.act(sil[fc % 2][:, :], pg[:, 0:TS], AF.Silu)
                P.tt(actT[:, fc, :], sil[fc % 2][:, :], pu[:, 0:TS], ALU.mult)
            i = 0
            for s in range(nsub):
                for dg in range(4):
                    pd = psb[4 + (i % 4)]
                    i += 1
                    for fc in range(6):
                        P.mm(pd[:, :], actT[:, fc, s * 128:(s + 1) * 128], wdv[:, fc, dg * 512:(dg + 1) * 512],
                             start=(fc == 0), stop=(fc == 5))
                    a_ = acc[:, s, dg * 512:(dg + 1) * 512]
                    if e == 0:
                        P.ts(a_, pd[:, :], gst[:, s, 0:1], None, ALU.mult)
                    else:
                        P.stt(a_, pd[:, :], gst[:, s, e:e + 1], a_, ALU.mult, ALU.add)
        P.dma("sp", c.dq["xout"], V(YS.t[t0:t0 + TS, :].rearrange("(s p) d -> p s d", p=128), YS), acc[:])
        if j == 1 and layer == 0:
            P.dbg("g_hs", hs[:], [128, nsub, D], BF16)
            P.dbg("g_gst", gst[:], [128, nsub, 8], F32)
            P.dbg("g_hTb", hTb[:], [128, KC, TS], BF16)
            P.dbg("g_acc", acc[:], [128, nsub, D], F32)
    P.barrier(); P.es = P_es; P.sb = _sb0; es.close()

    es = ExitStack(); P_es = P.es; P.es = es
    _sb0 = P.sb
    P.sb = lambda name, shape, dt: _sb0(name + L_, shape, dt)
    xr = [P.sb("r_x%d" % i, [128, D], F32) for i in range(2)]
    yr = [P.sb("r_y%d" % i, [128, D], F32) for i in range(2)]
    ysem = [c.dq["w0"], c.dq["w1"]]
    for i in range(NTI):
        b = i % 2
        r0 = i * 128
        P.dma("sp", c.dq["xin"], xr[b][:], X[r0:r0 + 128, :])
        P._wait("pool", P._deps([YS[:, :], slot_i[:]], [yr[b][:]]))
        ins = P.nc.gpsimd.indirect_dma_start(out=yr[b].t[:, :], out_offset=None, in_=YS.t[:, :],
                                             in_offset=bass.IndirectOffsetOnAxis(ap=slot_i.t[:, i:i + 1], axis=0))
        ins.then_inc(P.sem[ysem[b]], 16); P.cnt[ysem[b]] += 1
        P._record(ysem[b], P.cnt[ysem[b]], [YS[:, :], slot_i[:]], [yr[b][:]])
        P.tt(xr[b][:], xr[b][:], yr[b][:], ALU.add)
        P.dma("sp", c.dq["xout"], X[r0:r0 + 128, :], xr[b][:])
    P.barrier(); P.es = P_es; P.sb = _sb0; es.close()
    eso.close()


def front_end(P, c, X, t0, nsub, xt, h32, sq, stat, hTb, psb, identb_unused=None):
    for s in range(nsub):
        r0 = t0 + s * 128
        P.dma("sp", c.dq["xin"], xt[:], X[r0:r0 + 128, :])
        rms_h(P, c, xt[:], h32[:], sq[:], stat[:])
        for q in range(4):
            for j in range(4):
                k = q * 4 + j
                P.tr(psb[q][:, j * 128:(j + 1) * 128], h32[:, k * 128:(k + 1) * 128], c.ident[:], sig=(j == 3))
            P.copy(hTb[:, q * 4:(q + 1) * 4, s * 128:(s + 1) * 128], psb[q][:, :].re("p (j t) -> p j t", j=4), eng="act")


def stage_mlstm(P, c, X, io):
    ntok = c.ntok
    NH, DK, DV, L = 8, 128, 256, 64
    KC = D // 128
    TT = 512
    nsub = TT // 128
    psb = c.psb
    win = io["ml_w_in"]
    QT = P.dram("ml_QT", [NH, DK, ntok], BF16)
    KT = P.dram("ml_KT", [NH, DK, ntok], BF16)
    Ktm = P.dram("ml_Ktm", [ntok, NH * DK], F32)
    Vs = P.dram("ml_V", [ntok, NH * DV], BF16)
    Os = P.dram("ml_O", [ntok, NH * DV], F32)
    Gs = P.dram("ml_G", [ntok, 16], F32)
    OUTs = P.dram("ml_OUT", [ntok, NH * DV], BF16)

    es = ExitStack(); P_es = P.es; P.es = es
    xt = P.sb("a_xt", [128, D], F32)
    h32 = P.sb("a_h32", [128, D], F32)
    sq = P.sb("a_sq", [128, D], F32)
    stat = P.sb("a_stat", [128, 8], F32)
    hTb = P.sb("a_hTb", [128, KC, TT], BF16)
    wsl = [P.sb("a_w%d" % i, [128, KC, 512], BF16) for i in range(3)]
    wg = P.sb("a_wg", [128, KC, 16], BF16)
    bg = P.sb("a_bg", [128, 16], F32)
    stb = [P.sb("a_stb%d" % i, [128, 512], BF16) for i in range(3)]
    stf = [P.sb("a_stf%d" % i, [128, 512], F32) for i in range(3)]
    gt = P.sb("a_gt", [128, 48], F32)
    P.dma("sp", c.dq["misc"], c.gvec[:], V(io["norm_mix"].t[1:2, :].partition_broadcast(128), io["norm_mix"]))
    P.dma("pool", c.dq["misc"], wg[:], V(win.t[0, :, 6144:6160].rearrange("(k p) n -> p k n", p=128), win))
    P.dma("sp", c.dq["misc"], bg[:], V(io["ml_b_gate"].t[0:1, :].partition_broadcast(128), io["ml_b_gate"]))
    wq = ["w0", "w1", "w2"]
    cnt = [0, 0, 0]
    for t0 in range(0, ntok, TT):
        front_end(P, c, X, t0, nsub, xt, h32, sq, stat, hTb, psb)
        for s in range(nsub):
            r0 = t0 + s * 128
            for k in range(KC):
                P.mm(psb[4][:, 0:16], hTb[:, k, s * 128:(s + 1) * 128], wg[:, k, :], start=(k == 0), stop=(k == KC - 1))
            P.tt(gt[:, 0:16], psb[4][:, 0:16], bg[:], ALU.add)
            P.act(gt[:, 16:32], gt[:, 0:16], AF.Tanh, scale=1.0 / 15.0)
            P.ts(gt[:, 32:40], gt[:, 16:24], 15.0, None, ALU.mult)
            P.act(gt[:, 0:8], gt[:, 24:32], AF.Exp, scale=-15.0)
            P.ts(gt[:, 0:8], gt[:, 0:8], 1.0, None, ALU.add)
            P.act(gt[:, 8:16], gt[:, 0:8], AF.Ln)
            P.ts(gt[:, 40:48], gt[:, 8:16], -1.0, None, ALU.mult)
            P.dma("sp", c.dq["xout"], Gs[r0:r0 + 128, :], gt[:, 32:48])
        for blk in range(12):
            i = cnt[0] % 3
            cnt[0] += 1
            w = wsl[i]
            for hk in range(2):
                P.dma("pool", c.dq[wq[i]], w[:, hk * 8:(hk + 1) * 8, :],
                      V(win.t[0, :, blk * 512:(blk + 1) * 512].rearrange("(k p) n -> p k n", p=128)[:, hk * 8:(hk + 1) * 8, :], win))
            if blk < 4:
                dst = QT if blk < 2 else KT
                sc = 1.0 if blk < 2 else DK ** -0.5
                for hh in range(4):
                    head = (blk % 2) * 4 + hh
                    pp = psb[4 + (cnt[1] % 2)]
                    for k in range(KC):
                        P.mm(pp[:, :], w[:, k, hh * 128:(hh + 1) * 128], hTb[:, k, :], start=(k == 0), stop=(k == KC - 1))
                    sb_ = stb[cnt[1] % 3]
                    cnt[1] += 1
                    P.act(sb_[:], pp[:, :], AF.Copy, scale=sc)
                    P.dma("sp", c.dq["xout"], dst[head, :, t0:t0 + TT], sb_[:])
            if blk >= 2:
                for s in range(nsub):
                    r0 = t0 + s * 128
                    pp = psb[6 + (cnt[2] % 2)]
                    for k in range(KC):
                        P.mm(pp[:, :], hTb[:, k, s * 128:(s + 1) * 128], w[:, k, :], start=(k == 0), stop=(k == KC - 1))
                    j = cnt[2] % 3
                    cnt[2] += 1
                    if blk < 4:
                        P.act(stf[j][:], pp[:, :], AF.Copy, scale=DK ** -0.5)
                        P.dma("sp", c.dq["xout"], Ktm[r0:r0 + 128, (blk - 2) * 512:(blk - 1) * 512], stf[j][:])
                    elif blk < 8:
                        P.copy(stb[j][:], pp[:, :], eng="act")
                        P.dma("sp", c.dq["xout"], Vs[r0:r0 + 128, (blk - 4) * 512:(blk - 3) * 512], stb[j][:])
                    else:
                        P.act(stf[j][:], pp[:, :], AF.Sigmoid)
                        P.dma("sp", c.dq["xout"], Os[r0:r0 + 128, (blk - 8) * 512:(blk - 7) * 512], stf[j][:])
    P.barrier(); P.es = P_es; es.close()

    es = ExitStack(); P_es = P.es; P.es = es
    triU = P.sb("b_triU", [64, 64], F32)
    negm = P.sb("b_negm", [64, 64], F32)
    ones = P.sb("b_ones", [64, 128], F32)
    onesb = P.sb("b_onesb", [64, 1], BF16)
    Cst = P.sb("b_C", [128, NH, DV], F32)
    Cb = P.sb("b_Cb", [128, NH, DV], BF16)
    nst = P.sb("b_n", [128, NH], F32)
    nb = P.sb("b_nb", [128, NH], BF16)
    ghead = P.sb("b_gh", [64, NH * DV], F32)
    P.memset(triU[:], 1.0, eng="pool")
    P.op("pool", lambda e: e.affine_select(out=triU.t[:], in_=triU.t[:], pattern=[[1, 64]], compare_op=ALU.is_ge,
                                           fill=0.0, base=0, channel_multiplier=-1), [triU[:]], [triU[:]])
    P.ts(negm[:], triU[:], 30000.0, -30000.0, ALU.mult, ALU.add)
    P.memset(ones[:], 1.0)
    P.memset(onesb[:], 1.0)
    P.memset(Cst[:], 0.0); P.memset(Cb[:], 0.0); P.memset(nst[:], 0.0); P.memset(nb[:], 0.0)
    P.dma("sp", c.dq["misc"], ghead[:], V(io["ml_g_head"].t[0:1, :].partition_broadcast(64), io["ml_g_head"]))
    NB = 2
    qT = [P.sb("b_qT%d" % i, [128, NH, L], BF16) for i in range(NB)]
    kT = [P.sb("b_kT%d" % i, [128, NH, L], BF16) for i in range(NB)]
    ktm = [P.sb("b_ktm%d" % i, [64, NH * DK], F32) for i in range(NB)]
    vv = [P.sb("b_v%d" % i, [64, NH, DV], BF16) for i in range(NB)]
    oo = [P.sb("b_o%d" % i, [64, NH * DV], F32) for i in range(NB)]
    gg = [P.sb("b_g%d" % i, [64, 16], F32) for i in range(NB)]
    dsem = [P.dma_sem("bl%d" % i) for i in range(NB)]
    sml = P.sb("b_sml", [128, 96], F32)
    bts = P.sb("b_bts", [128, NH * L], F32)
    lfb = P.sb("b_lfb", [64, NH, 128], F32)
    Eb = P.sb("b_E", [64, NH, L], F32)
    Dm = P.sb("b_Dm", [64, NH, L], F32)
    SD = P.sb("b_SD", [64, NH, L], BF16)
    aT = P.sb("b_aT", [128, NH, L], F32)
    qs = P.sb("b_qs", [128, NH, L], BF16)
    kw = P.sb("b_kw", [64, NH, DK], BF16)
    hh = P.sb("b_hh", [64, NH, DV], F32)
    hsq = P.sb("b_hsq", [64, DV], F32)
    gho = P.sb("b_gho", [64, NH * DV], F32)
    outb = [P.sb("b_out%d" % i, [64, NH * DV], BF16) for i in range(2)]
    nchunk = ntok // L

    def load_chunk(ci):
        b = ci % NB
        t0 = ci * L
        P.dma("sp", dsem[b], qT[b][:], V(QT.t[:, :, t0:t0 + L].rearrange("h d t -> d h t"), QT))
        P.dma("sp", dsem[b], kT[b][:], V(KT.t[:, :, t0:t0 + L].rearrange("h d t -> d h t"), KT))
        P.dma("sp", dsem[b], ktm[b][:], Ktm[t0:t0 + L, :])
        P.dma("sp", dsem[b], vv[b][:, :, :].re("t h d -> t (h d)"), Vs[t0:t0 + L, :])
        P.dma("sp", dsem[b], oo[b][:], Os[t0:t0 + L, :])
        P.dma("sp", dsem[b], gg[b][:], Gs[t0:t0 + L, :])

    load_chunk(0)
    for ci in range(nchunk):
        b = ci % NB
        if ci + 1 < nchunk:
            load_chunk(ci + 1)
        ig = gg[b][:, 0:8]
        lf = gg[b][:, 8:16]
        p0 = psb[0]
        P.mm(p0[0:64, 0:8], triU[:], lf, True, True)
        P.mm(p0[0:64, 8:16], ones[:, 0:64], lf, True, True)
        P.mm(p0[:, 16:24], ones[:, :], lf, True, True)
        P.copy(sml[:, 0:24], p0[:, 0:24])
        P.act(sml[:, 16:24], sml[:, 16:24], AF.Exp)
        P.tt(sml[0:64, 24:32], ig, sml[0:64, 0:8], ALU.subtract)
        P.tt(sml[0:64, 32:40], sml[0:64, 24:32], sml[0:64, 8:16], ALU.add)
        P.act(sml[0:64, 40:48], sml[0:64, 32:40], AF.Exp)
        P.copy(lfb[:], V(gg[b].t[:, 8:16].unsqueeze(2).broadcast_to([64, NH, 128]), gg[b]))
        for h in range(NH):
            P.mm(psb[1][:, h * L:(h + 1) * L], lfb[:, h, :], triU[:], True, True, sig=(h == NH - 1))
        for h in range(NH):
            P.mm(psb[2][0:64, h * L:(h + 1) * L], kT[b][:, h, :], qT[b][:, h, :], True, True, sig=(h == NH - 1))
        P.copy(bts[:], psb[1][:, :])
        P.act(aT[:, :, :].re("p h t -> p (h t)"), bts[:], AF.Exp)
        P.tt(Eb[:], bts[0:64, :].re("p (h t) -> p h t", h=NH), V(negm.t[:, :].unsqueeze(1).broadcast_to([64, NH, L]), negm), ALU.add)
        for h in range(NH):
            P.act(Dm[:, h, :], Eb[:, h, :], AF.Exp, bias=sml[0:64, 24 + h:25 + h])
        P.tt(SD[:], psb[2][0:64, :].re("p (h t) -> p h t", h=NH), Dm[:], ALU.mult)
        P.tt(qs[:], qT[b][:], aT[:], ALU.mult, eng="pool")
        for h in range(NH):
            P.mm(p0[0:64, 24 + h:25 + h], SD[:, h, :], onesb[:], True, False, sig=False)
            P.mm(p0[0:64, 24 + h:25 + h], qs[:, h, :], nb[:, h:h + 1], False, True, sig=(h == NH - 1))
        P.ts(sml[0:64, 48:56], p0[0:64, 24:32], -1.0, None, ALU.mult)
        P.tt(sml[0:64, 48:56], sml[0:64, 48:56], p0[0:64, 24:32], ALU.max)
        P.ts(sml[0:64, 48:56], sml[0:64, 48:56], 1.0, None, ALU.max)
        P.op("dve", lambda e: e.reciprocal(out=sml.t[0:64, 56:64], in_=sml.t[0:64, 48:56]), [sml[:]], [sml[:]])
        for h in range(NH):
            pn = psb[3 + h // 2]
            o_ = (h % 2) * DV
            P.mm(pn[0:64, o_:o_ + DV], SD[:, h, :], vv[b][:, h, :], True, False, sig=False)
            P.mm(pn[0:64, o_:o_ + DV], qs[:, h, :], Cb[:, h, :], False, True)
            P.act(hh[:, h, :], pn[0:64, o_:o_ + DV], AF.Copy, scale=sml[0:64, 56 + h:57 + h])
            P.act(hsq[:], hh[:, h, :], AF.Square, accum=sml[0:64, 64 + h:65 + h])
        P.ts(sml[0:64, 72:80], sml[0:64, 64:72], 1.0 / DV, EPS, ALU.mult, ALU.add)
        P.act(sml[0:64, 80:88], sml[0:64, 72:80], AF.Sqrt)
        P.op("dve", lambda e: e.reciprocal(out=sml.t[0:64, 88:96], in_=sml.t[0:64, 80:88]), [sml[:]], [sml[:]])
        P.tt(gho[:], oo[b][:], ghead[:], ALU.mult, eng="pool")
        ob = outb[ci % 2]
        for h in range(NH):
            P.stt(ob[:, h * DV:(h + 1) * DV], hh[:, h, :], sml[0:64, 88 + h:89 + h], gho[:, h * DV:(h + 1) * DV], ALU.mult, ALU.mult)
        P.dma("sp", c.dq["xout"], OUTs[ci * L:(ci + 1) * L, :], ob[:])
        if ci == 0:
            P.dbg("sml", sml[:], [128, 96], F32)
            P.dbg("lfb", lfb[:], [64, NH, 128], F32)
            P.dbg("bts", bts[:], [128, NH * L], F32)
            P.dbg("Eb", Eb[:], [64, NH, L], F32)
            P.dbg("Dm", Dm[:], [64, NH, L], F32)
            P.dbg("SD", SD[:], [64, NH, L], BF16)
            P.dbg("aT", aT[:], [128, NH, L], F32)
            P.dbg("qs", qs[:], [128, NH, L], BF16)
            P.dbg("qT", qT[b][:], [128, NH, L], BF16)
            P.dbg("kT", kT[b][:], [128, NH, L], BF16)
            P.dbg("vv", vv[b][:], [64, NH, DV], BF16)
            P.dbg("hh", hh[:], [64, NH, DV], F32)
            P.dbg("negm", negm[:], [64, 64], F32)
            P.dbg("triU", triU[:], [64, 64], F32)
        for h in range(NH):
            P.ts(kw[:, h, :], ktm[b][:, h * DK:(h + 1) * DK], sml[0:64, 40 + h:41 + h], None, ALU.mult)
        for h in range(NH):
            P.mm(p0[:, 32 + h:33 + h], kw[:, h, :], onesb[:], True, True, sig=(h == NH - 1))
        for h in range(NH):
            pn = psb[3 + h // 2]
            o_ = (h % 2) * DV
            P.mm(pn[:, o_:o_ + DV], kw[:, h, :], vv[b][:, h, :], True, True)
            P.stt(Cst[:, h, :], Cst[:, h, :], sml[:, 16 + h:17 + h], pn[:, o_:o_ + DV], ALU.mult, ALU.add)
        P.tt(nst[:], nst[:], sml[:, 16:24], ALU.mult)
        P.tt(nst[:], nst[:], p0[:, 32:40], ALU.add)
        P.copy(Cb[:], Cst[:], eng="pool")
        P.copy(nb[:], nst[:])
    P.barrier(); P.es = P_es; es.close()

    es = ExitStack(); P_es = P.es; P.es = es
    wo = P.sb("c_wo", [128, KC, D], BF16)
    ot = P.sb("c_ot", [128, D], BF16)
    oT = P.sb("c_oT", [128, KC, 128], BF16)
    xr = [P.sb("c_x%d" % i, [128, D], F32) for i in range(2)]
    pst = [P.ps("c_pst%d" % i, [128, 1024], BF16) for i in range(0)]
    wov = io["ml_w_out"]
    for hk in range(4):
        P.dma("pool", c.dq["w0"], wo[:, hk * 4:(hk + 1) * 4, :],
              V(wov.t[0].rearrange("(k p) n -> p k n", p=128)[:, hk * 4:(hk + 1) * 4, :], wov))
    pbf = [V(psb[i].t[:, :].bitcast(BF16), psb[i]) for i in range(2)]
    for ti, r0 in enumerate(range(0, ntok, 128)):
        x_ = xr[ti % 2]
        P.dma("sp", c.dq["xin"], ot[:], OUTs[r0:r0 + 128, :])
        P.dma("sp", c.dq["xin"], x_[:], X[r0:r0 + 128, :])
        for q in range(2):
            for j in range(8):
                k = q * 8 + j
                P.tr(pbf[q][:, j * 128:(j + 1) * 128], ot[:, k * 128:(k + 1) * 128], c.identb[:], sig=(j == 7))
            P.copy(oT[:, q * 8:(q + 1) * 8, :], pbf[q][:, :].re("p (j t) -> p j t", j=8), eng="act")
        for dg in range(4):
            pp = psb[4 + dg]
            for k in range(KC):
                P.mm(pp[:, :], oT[:, k, :], wo[:, k, dg * 512:(dg + 1) * 512], start=(k == 0), stop=(k == KC - 1))
            P.tt(x_[:, dg * 512:(dg + 1) * 512], x_[:, dg * 512:(dg + 1) * 512], pp[:, :], ALU.add)
        P.dma("sp", c.dq["xout"], X[r0:r0 + 128, :], x_[:])
    P.barrier(); P.es = P_es; es.close()


import math


def frac_pm_half(P, out, a, itmp, ftmp):
    P.copy(itmp, a)
    P.copy(ftmp, itmp)
    P.tt(out, a, ftmp, ALU.subtract)
    P.ts(ftmp, out, 0.5, None, ALU.is_gt)
    P.tt(out, out, ftmp, ALU.subtract)
    P.ts(ftmp, out, -0.5, None, ALU.is_lt)
    P.tt(out, out, ftmp, ALU.add)


def stage_s5(P, c, X, io):
    ntok = c.ntok
    KC = D // 128
    psb = c.psb
    NT = ntok // 512
    HT = P.dram("s5_HT", [D, ntok], F32)
    GT = P.dram("s5_GT", [D, ntok], BF16)
    PRM = P.dram("s5_PRM", [6, 128, 64], F32)
    es = ExitStack(); P_es = P.es; P.es = es
    xt = P.sb("sa_xt", [128, D], F32)
    h32 = P.sb("sa_h32", [128, D], F32)
    sq = P.sb("sa_sq", [128, D], F32)
    stat = P.sb("sa_stat", [128, 8], F32)
    hT = [P.sb("sa_hT%d" % i, [128, KC, 128], F32) for i in range(2)]
    P.dma("sp", c.dq["misc"], c.gvec[:], V(io["norm_mix"].t[0:1, :].partition_broadcast(128), io["norm_mix"]))
    for ti, r0 in enumerate(range(0, ntok, 128)):
        P.dma("sp", c.dq["xin"], xt[:], X[r0:r0 + 128, :])
        rms_h(P, c, xt[:], h32[:], sq[:], stat[:])
        hb = hT[ti % 2]
        for q in range(4):
            for j in range(4):
                k = q * 4 + j
                P.tr(psb[q][:, j * 128:(j + 1) * 128], h32[:, k * 128:(k + 1) * 128], c.ident[:], sig=(j == 3))
            P.copy(hb[:, q * 4:(q + 1) * 4, :], psb[q][:, :].re("p (j t) -> p j t", j=4), eng="act")
        P.dma("sp", c.dq["xout"], V(HT.t[:, r0:r0 + 128].rearrange("(k p) t -> p k t", p=128), HT), hb[:])
    lr = P.sb("sa_lr", [128, 64], F32); li = P.sb("sa_li", [128, 64], F32); ldt = P.sb("sa_ldt", [128, 1], F32)
    w = [P.sb("sa_w%d" % i, [128, 64], F32) for i in range(12)]
    cpi = P.sb("sa_cpi", [128, 1], F32)
    P.memset(cpi[:], -math.pi)
    P.dma("sp", c.dq["misc"], lr[:], io["s5_lam_re"][:, :])
    P.dma("sp", c.dq["misc"], li[:], io["s5_lam_im"][:, :])
    P.dma("sp", c.dq["misc"], ldt[:], io["s5_log_dt"][:, :])
    P.act(ldt[:], ldt[:], AF.Exp)
    P.ts(w[0][:], lr[:], ldt[:, 0:1], None, ALU.mult)
    P.ts(w[1][:], li[:], ldt[:, 0:1], None, ALU.mult)
    P.act(w[2][:], w[0][:], AF.Exp)
    wi_ = P.sb("sa_wi", [128, 64], mybir.dt.int32)
    P.ts(w[3][:], w[1][:], 1.0 / (2 * math.pi), None, ALU.mult)
    frac_pm_half(P, w[3][:], w[3][:], wi_[:], w[4][:])
    P.ts(w[4][:], w[3][:], 0.25, None, ALU.add)
    frac_pm_half(P, w[4][:], w[4][:], wi_[:], w[5][:])
    P.act(w[6][:], w[4][:], AF.Sin, scale=2 * math.pi)
    P.act(w[5][:], w[3][:], AF.Sin, scale=2 * math.pi)
    P.tt(w[7][:], w[2][:], w[6][:], ALU.mult)
    P.tt(w[8][:], w[2][:], w[5][:], ALU.mult)
    P.ts(w[7][:], w[7][:], -1.0, None, ALU.add)
    P.tt(w[9][:], lr[:], lr[:], ALU.mult)
    P.tt(w[10][:], li[:], li[:], ALU.mult)
    P.tt(w[9][:], w[9][:], w[10][:], ALU.add)
    P.op("dve", lambda e: e.reciprocal(out=w[9].t[:], in_=w[9].t[:]), [w[9][:]], [w[9][:]])
    P.tt(w[10][:], w[7][:], lr[:], ALU.mult)
    P.tt(w[11][:], w[8][:], li[:], ALU.mult)
    P.tt(w[10][:], w[10][:], w[11][:], ALU.add)
    P.tt(w[10][:], w[10][:], w[9][:], ALU.mult)
    P.tt(w[11][:], w[8][:], lr[:], ALU.mult)
    P.tt(w[0][:], w[7][:], li[:], ALU.mult)
    P.tt(w[11][:], w[11][:], w[0][:], ALU.subtract)
    P.tt(w[11][:], w[11][:], w[9][:], ALU.mult)
    P.dma("sp", c.dq["xout"], PRM[0], w[10][:])
    P.dma("sp", c.dq["xout"], PRM[1], w[11][:])
    P.dma("sp", c.dq["xout"], PRM[2], w[2][:])
    P.dma("sp", c.dq["xout"], PRM[3], w[3][:])
    P.barrier(); P.es = P_es; es.close()

    es = ExitStack(); P_es = P.es; P.es = es
    iot = P.sb("sb_iota", [128, ntok], F32)
    P.op("pool", lambda e: e.iota(iot.t[:], pattern=[[1, ntok]], base=0, channel_multiplier=0,
                                  allow_small_or_imprecise_dtypes=True), [], [iot[:]])
    MAGIC = 12582912.0
    hpi = P.sb("sb_hpi", [128, 1], F32)
    P.memset(hpi[:], math.pi / 2)
    magT = P.sb("sb_magT", [128, 64], F32)
    phiT = P.sb("sb_phiT", [128, 64], F32)
    P.dma("sp", c.dq["misc"], magT[:], V(PRM.t[2].rearrange("(q t) p -> (t p) q", t=2), PRM), allow_slow_non_contiguous=True)
    P.dma("sp", c.dq["misc"], phiT[:], V(PRM.t[3].rearrange("(q t) p -> (t p) q", t=2), PRM), allow_slow_non_contiguous=True)
    dT = P.sb("sb_dT", [128, KC], F32)
    P.dma("sp", c.dq["misc"], dT[:], io["s5_dT"][:, :])
    u = P.sb("sb_u", [128, ntok], F32)
    ysb = P.sb("sb_y", [128, ntok], F32)
    gb = P.sb("sb_gb", [128, ntok], BF16)
    NBF = 2
    bTr = [P.sb("sb_bTr%d" % i, [128, 128], F32) for i in range(NBF)]
    bTi = [P.sb("sb_bTi%d" % i, [128, 128], F32) for i in range(NBF)]
    Qr = [P.sb("sb_Qr%d" % i, [128, 128], F32) for i in range(NBF)]
    Qi = [P.sb("sb_Qi%d" % i, [128, 128], F32) for i in range(NBF)]
    cTr = [P.sb("sb_cTr%d" % i, [128, 128], F32) for i in range(NBF)]
    cTi = [P.sb("sb_cTi%d" % i, [128, 128], F32) for i in range(NBF)]
    Br = P.sb("sb_Br", [128, 128], F32); Bi = P.sb("sb_Bi", [128, 128], F32); t1 = P.sb("sb_t1", [128, 128], F32)
    gsem = [P.dma_sem("s5g%d" % i) for i in range(NBF)]
    bur = P.sb("sb_bur", [128, ntok], F32); bui = P.sb("sb_bui", [128, ntok], F32)
    Cn = P.sb("sb_Cn", [128, ntok], F32); Sn = P.sb("sb_Sn", [128, ntok], F32)
    ta = P.sb("sb_ta", [128, ntok], F32); tb = P.sb("sb_tb", [128, ntok], F32)
    mr = P.sb("sb_mr", [128, ntok], F32); mi = P.sb("sb_mi", [128, ntok], F32)
    tint_v = V(mi.t[:, :].bitcast(mybir.dt.int32), mi)

    def load_pair(q):
        b = q % NBF
        g0 = 2 * q
        P.dma("sp", gsem[b], bTr[b][:, :].re("r (g p) -> r g p", g=2), V(io["s5_bT_re"].t[g0:g0 + 2].rearrange("g r p -> r g p"), io["s5_bT_re"]))
        P.dma("sp", gsem[b], bTi[b][:, :].re("r (g p) -> r g p", g=2), V(io["s5_bT_im"].t[g0:g0 + 2].rearrange("g r p -> r g p"), io["s5_bT_im"]))
        P.dma("sp", gsem[b], cTr[b][:], V(io["s5_cT_re"].t[g0:g0 + 2].rearrange("g p c -> (g p) c"), io["s5_cT_re"]))
        P.dma("sp", gsem[b], cTi[b][:], V(io["s5_cT_im"].t[g0:g0 + 2].rearrange("g p c -> (g p) c"), io["s5_cT_im"]))
        P.dma("sp", gsem[b], Qr[b][:], V(PRM.t[0].rearrange("(q t) p -> q (t p)", t=2)[q:q + 1, :].partition_broadcast(128), PRM))
        P.dma("sp", gsem[b], Qi[b][:], V(PRM.t[1].rearrange("(q t) p -> q (t p)", t=2)[q:q + 1, :].partition_broadcast(128), PRM))

    load_pair(0)
    for fc in range(KC):
        P.dma("sp", c.dq["xin"], u[:], HT[fc * 128:(fc + 1) * 128, :])
        for qi in range(4):
            q = fc * 4 + qi
            b = q % NBF
            if q + 1 < 64:
                load_pair(q + 1)
            P.tt(Br[:], bTr[b][:], Qr[b][:], ALU.mult)
            P.tt(t1[:], bTi[b][:], Qi[b][:], ALU.mult)
            P.tt(Br[:], Br[:], t1[:], ALU.subtract)
            P.tt(Bi[:], bTi[b][:], Qr[b][:], ALU.mult)
            P.tt(t1[:], bTr[b][:], Qi[b][:], ALU.mult)
            P.tt(Bi[:], Bi[:], t1[:], ALU.add)
            P.ts(cTi[b][:], cTi[b][:], -1.0, None, ALU.mult)
            for tbk in range(NT):
                sl = slice(tbk * 512, (tbk + 1) * 512)
                P.mm(psb[tbk % 2][:, :], Br[:], u[:, sl], True, True)
                P.mm(psb[2 + tbk % 2][:, :], Bi[:], u[:, sl], True, True)
                P.copy(bur[:, sl], psb[tbk % 2][:, :], eng="act")
                P.copy(bui[:, sl], psb[2 + tbk % 2][:, :], eng="act")
            P.act(ta[:], iot[:], AF.Copy, scale=phiT[:, q:q + 1])
            P.ts(tb[:], ta[:], MAGIC, None, ALU.add)
            P.stt(mr[:], tb[:], MAGIC, ta[:], ALU.subtract, ALU.subtract)
            P.act(Sn[:], mr[:], AF.Sin, scale=-2 * math.pi)
            P.act(mi[:], mr[:], AF.Abs)
            P.act(Cn[:], mi[:], AF.Sin, scale=-2 * math.pi, bias=hpi[:, 0:1])
            P.tt(mr[:], Cn[:], bur[:], ALU.mult)
            P.tt(ta[:], Sn[:], bui[:], ALU.mult, eng="pool")
            P.tt(mr[:], mr[:], ta[:], ALU.add)
            P.tt(mi[:], Cn[:], bui[:], ALU.mult)
            P.tt(tb[:], Sn[:], bur[:], ALU.mult, eng="pool")
            P.tt(mi[:], mi[:], tb[:], ALU.subtract)
            dec = V(magT.t[:, q:q + 1].broadcast_to([128, ntok]), magT)
            P.op("dve", lambda e: e.tensor_tensor_scan(out=bur.t[:], data0=dec.ap, data1=mr.t[:], initial=0.0,
                                                       op0=ALU.mult, op1=ALU.add), [dec, mr[:]], [bur[:]])
            P.op("dve", lambda e: e.tensor_tensor_scan(out=bui.t[:], data0=dec.ap, data1=mi.t[:], initial=0.0,
                                                       op0=ALU.mult, op1=ALU.add), [dec, mi[:]], [bui[:]])
            P.tt(mr[:], Cn[:], bur[:], ALU.mult)
            P.tt(ta[:], Sn[:], bui[:], ALU.mult, eng="pool")
            P.tt(mr[:], mr[:], ta[:], ALU.subtract)
            P.tt(mi[:], Cn[:], bui[:], ALU.mult)
            P.tt(tb[:], Sn[:], bur[:], ALU.mult, eng="pool")
            P.tt(mi[:], mi[:], tb[:], ALU.add)
            for tbk in range(NT):
                sl = slice(tbk * 512, (tbk + 1) * 512)
                pp = psb[4 + tbk % 4]
                P.mm(pp[:, :], cTr[b][:], mr[:, sl], True, False, sig=False)
                P.mm(pp[:, :], cTi[b][:], mi[:, sl], False, True)
                if qi == 0:
                    P.copy(ysb[:, sl], pp[:, :])
                else:
                    P.tt(ysb[:, sl], ysb[:, sl], pp[:, :], ALU.add)
        P.stt(ysb[:], u[:], dT[:, fc:fc + 1], ysb[:], ALU.mult, ALU.add)
        P.act(gb[:], ysb[:], AF.Gelu)
        P.dma("sp", c.dq["xout"], GT[fc * 128:(fc + 1) * 128, :], gb[:])
    P.barrier(); P.es = P_es; es.close()

    es = ExitStack(); P_es = P.es; P.es = es
    gT = P.sb("sc_gT", [128, KC, 512], BF16)
    wv = [P.sb("sc_wv%d" % i, [128, KC, 512], BF16) for i in range(2)]
    wg = [P.sb("sc_wg%d" % i, [128, KC, 512], BF16) for i in range(2)]
    xr = P.sb("sc_x", [128, 4, D], F32)
    sg = [P.sb("sc_sg%d" % i, [128, 512], F32) for i in range(2)]
    wgl = io["s5_w_glu"]
    wsem = [c.dq["w0"], c.dq["w1"]]
    n = 0
    for t0 in range(0, ntok, 512):
        P.dma("sp", c.dq["xin"], gT[:], V(GT.t[:, t0:t0 + 512].rearrange("(k p) t -> p k t", p=128), GT))
        for s in range(4):
            P.dma("sp", c.dq["xin"], xr[:, s, :], X[t0 + s * 128:t0 + (s + 1) * 128, :])
        for j in range(4):
            b = n % 2
            n += 1
            for hk in range(2):
                P.dma("pool", wsem[b], wv[b][:, hk * 8:(hk + 1) * 8, :],
                      V(wgl.t[0, :, j * 512:(j + 1) * 512].rearrange("(k p) n -> p k n", p=128)[:, hk * 8:(hk + 1) * 8, :], wgl))
                P.dma("pool", wsem[b], wg[b][:, hk * 8:(hk + 1) * 8, :],
                      V(wgl.t[0, :, D + j * 512:D + (j + 1) * 512].rearrange("(k p) n -> p k n", p=128)[:, hk * 8:(hk + 1) * 8, :], wgl))
            for s in range(4):
                pv = psb[(s % 2) * 2]
                pg = psb[(s % 2) * 2 + 1]
                for k in range(KC):
                    P.mm(pv[:, :], gT[:, k, s * 128:(s + 1) * 128], wv[b][:, k, :], start=(k == 0), stop=(k == KC - 1))
                for k in range(KC):
                    P.mm(pg[:, :], gT[:, k, s * 128:(s + 1) * 128], wg[b][:, k, :], start=(k == 0), stop=(k == KC - 1))
                P.act(sg[s % 2][:], pg[:, :], AF.Sigmoid)
                P.tt(sg[s % 2][:], sg[s % 2][:], pv[:, :], ALU.mult)
                P.tt(xr[:, s, j * 512:(j + 1) * 512], xr[:, s, j * 512:(j + 1) * 512], sg[s % 2][:], ALU.add)
        for s in range(4):
            P.dma("sp", c.dq["xout"], X[t0 + s * 128:t0 + (s + 1) * 128, :], xr[:, s, :])
    P.barrier(); P.es = P_es; es.close()


def stage_final(P, c, X, io, out):
    es = ExitStack()
    P_es = P.es
    P.es = es
    xt = [P.sb("f_x%d" % i, [128, D], F32) for i in range(2)]
    ht = [P.sb("f_h%d" % i, [128, D], F32) for i in range(2)]
    sq = P.sb("f_sq", [128, D], F32)
    stat = P.sb("f_stat", [128, 8], F32)
    P.dma("sp", c.dq["misc"], c.gvec[:], V(io["norm_final"].t.unsqueeze(0).partition_broadcast(128), io["norm_final"]))
    for i, r0 in enumerate(range(0, c.ntok, 128)):
        P.dma("sp", c.dq["xin"], xt[i % 2][:], X[r0:r0 + 128, :])
        rms_h(P, c, xt[i % 2][:], ht[i % 2][:], sq[:], stat[:])
        P.dma("sp", c.dq["xout"], out[r0:r0 + 128, :], ht[i % 2][:])
    P.es = P_es
    return es


IN_SHAPES = {
    "x": [4096, D],
    "norm_ffn": [2, D], "norm_mix": [2, D], "norm_final": [D],
    "moe_w_group": [2, D, 8], "moe_b_group": [2, 8], "moe_w_expert": [2, D, 64], "moe_b_expert": [2, 64],
    "moe_w_gu": [2, NE, D, 2 * FH], "moe_w_down": [2, NE, FH, D],
    "moe_wg_r": [2 * NE * 128, 16 * FH], "moe_wu_r": [2 * NE * 128, 16 * FH], "moe_wd_r": [2 * NE * 128, 6 * D],
    "s5_lam_re": [128, 64], "s5_lam_im": [128, 64], "s5_log_dt": [128, 1], "s5_dT": [128, 16],
    "s5_bT_re": [128, 128, 64], "s5_bT_im": [128, 128, 64], "s5_cT_re": [128, 64, 128], "s5_cT_im": [128, 64, 128],
    "s5_w_glu": [1, D, 2 * D],
    "ml_w_in": [1, D, 6160], "ml_b_gate": [1, 16], "ml_g_head": [1, D], "ml_w_out": [1, D, D],
}


def build(stages=("moe0",), ntok=4096, in_names=None):
    nc = bass.Bass("TRN2", target_bir_lowering=False)
    with ExitStack() as es:
        P = Prog(nc, es)
        io = {}
        for k in (in_names or IN_SHAPES.keys()):
            shp = list(IN_SHAPES[k])
            if k == "x":
                shp[0] = ntok
            if DBG_NE and k in ("moe_w_gu", "moe_w_down"):
                shp[1] = DBG_NE
            io[k] = P.dram(k, shp, F32, kind="ExternalInput")
        out = P.dram("out", [ntok, D], F32, kind="ExternalOutput")
        X = P.dram("Xs", [ntok, D], F32)
        c = setup_common(P, ntok)
        P.dma("sp", c.dq["misc"], X[:, :], io["x"][:, :])
        for st in stages:
            if st == "moe0":
                stage_moe(P, c, X, 0, io).close()
            elif st == "moe1":
                stage_moe(P, c, X, 1, io).close()
            elif st == "mlstm":
                stage_mlstm(P, c, X, io)
            elif st == "moeg0":
                stage_moe_g(P, c, X, 0, io)
            elif st == "moeg1":
                stage_moe_g(P, c, X, 1, io)
            elif st == "s5":
                stage_s5(P, c, X, io)
        stage_final(P, c, X, io, out).close()
        P.finish()
    return nc


def moe_layout(inp):
    o = {}
    wg = inp["moe_w_gu"].reshape(2, NE, 16, 128, 2, FH)
    o["moe_wg_r"] = np.ascontiguousarray(wg[:, :, :, :, 0, :].transpose(0, 1, 3, 2, 4)).reshape(2 * NE * 128, 16 * FH)
    o["moe_wu_r"] = np.ascontiguousarray(wg[:, :, :, :, 1, :].transpose(0, 1, 3, 2, 4)).reshape(2 * NE * 128, 16 * FH)
    wd = inp["moe_w_down"].reshape(2, NE, 6, 128, D)
    o["moe_wd_r"] = np.ascontiguousarray(wd.transpose(0, 1, 3, 2, 4)).reshape(2 * NE * 128, 6 * D)
    return o


def s5_layout(inp):
    G, PS, CG = 128, 64, 16
    o = {}
    o["s5_lam_re"] = np.ascontiguousarray(inp["s5_lam_re"][0])
    o["s5_lam_im"] = np.ascontiguousarray(inp["s5_lam_im"][0])
    o["s5_log_dt"] = np.ascontiguousarray(inp["s5_log_dt"][0].reshape(G, 1))
    o["s5_dT"] = np.ascontiguousarray(inp["s5_d"][0].reshape(16, 128).T)
    for nm, src in (("s5_bT_re", "s5_b_re"), ("s5_bT_im", "s5_b_im")):
        a = np.zeros((G, 128, PS), np.float32)
        bt = np.transpose(inp[src][0], (0, 2, 1))
        for g in range(G):
            gi = g % 8
            a[g, gi * CG:(gi + 1) * CG, :] = bt[g]
        o[nm] = a
    for nm, src in (("s5_cT_re", "s5_c_re"), ("s5_cT_im", "s5_c_im")):
        a = np.zeros((G, PS, 128), np.float32)
        ct = np.transpose(inp[src][0], (0, 2, 1))
        for g in range(G):
            gi = g % 8
            a[g, :, gi * CG:(gi + 1) * CG] = ct[g]
        o[nm] = a
    o["s5_w_glu"] = inp["s5_w_glu"]
    return o


_NC_CACHE = {}


def kernel(**inputs):
    inp = {k: np.asarray(v) for k, v in inputs.items()}
    B = inp["x"].shape[0]
    stages = ("s5", "moeg0", "mlstm", "moeg1")
    shared = s5_layout(inp)
    shared.update(moe_layout(inp))
    for k in ["norm_ffn", "norm_mix", "norm_final", "moe_w_group", "moe_b_group", "moe_w_expert", "moe_b_expert",
              "ml_w_in", "ml_b_gate", "ml_g_head", "ml_w_out"]:
        shared[k] = np.ascontiguousarray(inp[k])
    names = ["x"] + list(shared.keys())
    if "nc" not in _NC_CACHE:
        _NC_CACHE["nc"] = build(stages=stages, ntok=inp["x"].shape[1], in_names=names)
    nc = _NC_CACHE["nc"]
    in_maps = []
    for b in range(B):
        m = dict(shared)
        m["x"] = np.ascontiguousarray(inp["x"][b])
        in_maps.append(m)
    res = run_bass_kernel_spmd(nc, in_maps, core_ids=list(range(B)))
    return np.stack([np.asarray(r["out"]) for r in res.results], axis=0).astype(np.float32)
```

```python
from contextlib import ExitStack
import numpy as np
import concourse.bass as bass
import concourse.mybir as mybir
from concourse.bass_utils import run_bass_kernel_spmd

F32 = mybir.dt.float32
BF16 = mybir.dt.bfloat16
AF = mybir.ActivationFunctionType
ALU = mybir.AluOpType
AX = mybir.AxisListType

D = 2048
EPS = 1e-6
NE = 64
import os
DBG_CUT = int(os.environ.get('DBG_CUT', '0'))
DBG_OUT = int(os.environ.get('DBG_OUT', '0'))
DBG_NE = int(os.environ.get('DBG_NE', '0'))
FH = 768


class Buf:
    def __init__(self, t, name):
        self.t = t
        self.name = name
        self.w = None
        self.r = []

    def __getitem__(self, k):
        return V(self.t[k], self)


class V:
    def __init__(self, ap, buf):
        self.ap = ap
        self.buf = buf

    def re(self, s, **kw):
        return V(self.ap.rearrange(s, **kw), self.buf)

    def __getitem__(self, k):
        return V(self.ap[k], self.buf)


class Prog:
    def __init__(self, nc, es):
        self.nc = nc
        self.es = es
        self.eng = {"pe": nc.tensor, "act": nc.scalar, "dve": nc.vector, "pool": nc.gpsimd, "sp": nc.sync}
        self.sem = {}
        self.cnt = {}
        self.waited = {}
        for k in self.eng:
            self.sem[k] = es.enter_context(nc.semaphore("s_" + k))
            self.cnt[k] = 0
        self.ndma = 0

    def dma_sem(self, name):
        key = "d_" + name
        self.sem[key] = self.es.enter_context(self.nc.semaphore(key))
        self.cnt[key] = 0
        return key

    def sb(self, name, shape, dt):
        return Buf(self.es.enter_context(self.nc.sbuf_tensor(name, list(shape), dt)), name)

    def ps(self, name, shape, dt=F32):
        return Buf(self.es.enter_context(self.nc.psum_tensor(name, list(shape), dt)), name)

    def dram(self, name, shape, dt, kind="Internal"):
        if DBG_OUT and kind == "Internal" and name != "Xs":
            kind = "ExternalOutput"
        t = self.nc.dram_tensor(name, list(shape), dt, kind=kind)
        return Buf(t.ap(), name)

    def _deps(self, reads, writes):
        deps = {}
        def add(d):
            if d is None:
                return
            k, s = d
            if deps.get(k, 0) < s:
                deps[k] = s
        for v in reads:
            add(v.buf.w)
        for v in writes:
            add(v.buf.w)
            for d in v.buf.r:
                add(d)
        return deps

    def _wait(self, ek, deps):
        e = self.eng[ek]
        for k, s in deps.items():
            if k == ek and ek in ("pe", "sp"):
                continue
            if self.waited.get((ek, k), 0) >= s:
                continue
            mult = 1
            if k.startswith("d_"):
                mult = 16
                s = self.cnt[k]
            e.wait_ge(self.sem[k], s * mult)
            self.waited[(ek, k)] = s

    def _record(self, key, seq, reads, writes):
        for v in reads:
            v.buf.r.append((key, seq))
            if len(v.buf.r) > 64:
                m = {}
                for k, s in v.buf.r:
                    if m.get(k, 0) < s:
                        m[k] = s
                v.buf.r = list(m.items())
        for v in writes:
            v.buf.w = (key, seq)
            v.buf.r = []

    def op(self, ek, fn, reads, writes, sig=True):
        self._wait(ek, self._deps(reads, writes))
        ins = fn(self.eng[ek])
        seq = self.cnt[ek] + 1
        if sig:
            ins.then_inc(self.sem[ek], 1)
            self.cnt[ek] = seq
        self._record(ek, seq, reads, writes)
        return ins

    def dma(self, qk, dkey, out, in_, **kw):
        self._wait(qk, self._deps([in_], [out]))
        ins = self.eng[qk].dma_start(out=out.ap, in_=in_.ap, **kw)
        ins.then_inc(self.sem[dkey], 16)
        self.cnt[dkey] += 1
        self._record(dkey, self.cnt[dkey], [in_], [out])
        self.ndma += 1
        return ins

    def mm(self, out, lhsT, rhs, start, stop, sig=None):
        if sig is None:
            sig = stop
        return self.op("pe", lambda e: e.matmul(out.ap, lhsT.ap, rhs.ap, start=start, stop=stop),
                       [lhsT, rhs], [out], sig=sig)

    def tr(self, out, in_, ident, sig=True):
        return self.op("pe", lambda e: e.transpose(out.ap, in_.ap, ident.ap), [in_, ident], [out], sig=sig)

    def act(self, out, in_, func, bias=None, scale=None, accum=None, eng="act"):
        kw = {}
        reads = [in_]
        writes = [out]
        if bias is not None:
            if isinstance(bias, V):
                kw["bias"] = bias.ap
                reads.append(bias)
            else:
                kw["bias"] = bias
        if scale is not None:
            if isinstance(scale, V):
                kw["scale"] = scale.ap
                reads.append(scale)
            else:
                kw["scale"] = scale
        if accum is not None:
            kw["accum_out"] = accum.ap
            writes.append(accum)
        return self.op(eng, lambda e: e.activation(out=out.ap, in_=in_.ap, func=func, **kw), reads, writes)

    def ts(self, out, in0, s1, s2, op0, op1=None, eng="dve", accum=None):
        reads = [in0]
        writes = [out]
        a1 = s1
        a2 = s2
        if isinstance(s1, V):
            reads.append(s1)
            a1 = s1.ap
        if isinstance(s2, V):
            reads.append(s2)
            a2 = s2.ap
        kw = {}
        if op1 is not None:
            kw["op1"] = op1
        if accum is not None:
            kw["accum_out"] = accum.ap
            writes.append(accum)
        return self.op(eng, lambda e: e.tensor_scalar(out=out.ap, in0=in0.ap, scalar1=a1, scalar2=a2, op0=op0, **kw),
                       reads, writes)

    def tt(self, out, in0, in1, op, eng="dve"):
        return self.op(eng, lambda e: e.tensor_tensor(out=out.ap, in0=in0.ap, in1=in1.ap, op=op), [in0, in1], [out])

    def stt(self, out, in0, scalar, in1, op0, op1, eng="dve"):
        reads = [in0, in1]
        a = scalar
        if isinstance(scalar, V):
            reads.append(scalar)
            a = scalar.ap
        return self.op(eng, lambda e: e.scalar_tensor_tensor(out=out.ap, in0=in0.ap, scalar=a, in1=in1.ap,
                                                              op0=op0, op1=op1), reads, [out])

    def copy(self, out, in_, eng="dve"):
        if eng == "act":
            return self.op("act", lambda e: e.copy(out=out.ap, in_=in_.ap), [in_], [out])
        return self.op(eng, lambda e: e.tensor_copy(out=out.ap, in_=in_.ap), [in_], [out])

    def memset(self, out, val, eng="dve"):
        return self.op(eng, lambda e: e.memset(out.ap, val), [], [out])

    def red(self, out, in_, op, eng="dve"):
        return self.op(eng, lambda e: e.tensor_reduce(out=out.ap, in_=in_.ap, axis=AX.X, op=op), [in_], [out])

    def dbg(self, name, v, shape, dt):
        if not DBG_OUT:
            return
        if not hasattr(self, "_dq"):
            self._dq = self.dma_sem("dbg")
        t = self.nc.dram_tensor("dbg_" + name, list(shape), dt, kind="ExternalOutput")
        self.dma("sp", self._dq, V(t.ap(), Buf(t.ap(), name)), v)

    def barrier(self):
        for ek, e in self.eng.items():
            for k, c in self.cnt.items():
                if c > 0 and k != ek and self.waited.get((ek, k), 0) < c:
                    e.wait_ge(self.sem[k], c * (16 if k.startswith("d_") else 1))
                    self.waited[(ek, k)] = c

    def finish(self):
        e = self.eng["sp"]
        for k, c in self.cnt.items():
            if c > 0 and k != "sp":
                e.wait_ge(self.sem[k], c * (16 if k.startswith("d_") else 1))


class Ctx:
    pass


def setup_common(P, ntok):
    c = Ctx()
    c.ntok = ntok
    c.ident = P.sb("ident", [128, 128], F32)
    c.identb = P.sb("identb", [128, 128], BF16)
    nc = P.nc
    P.memset(c.ident[:], 1.0, eng="pool")
    P.op("pool", lambda e: e.affine_select(out=c.ident.t[:], in_=c.ident.t[:], pattern=[[-1, 128]],
                                           compare_op=ALU.is_equal, fill=0.0, base=0, channel_multiplier=1),
         [c.ident[:]], [c.ident[:]])
    P.copy(c.identb[:], c.ident[:], eng="pool")
    c.psb = [P.ps("psb%d" % i, [128, 512], F32) for i in range(8)]
    c.gvec = P.sb("gvec", [128, D], F32)
    c.dq = {k: P.dma_sem(k) for k in ["misc", "xin", "xout", "w0", "w1", "w2", "w3", "w4"]}
    return c


def rms_h(P, c, xt, ht, tmp, stat):
    P.act(tmp, xt, AF.Square, accum=stat[:, 0:1])
    P.ts(stat[:, 1:2], stat[:, 0:1], 1.0 / D, EPS, ALU.mult, ALU.add)
    P.act(stat[:, 3:4], stat[:, 1:2], AF.Sqrt)
    P.op("dve", lambda e: e.reciprocal(out=stat.buf.t[:, 2:3], in_=stat.buf.t[:, 3:4]), [stat], [stat])
    P.stt(ht, xt, stat[:, 2:3], c.gvec[:], ALU.mult, ALU.mult)


def stage_moe(P, c, X, layer, io, TT=512):
    ntok = c.ntok
    nsub = TT // 128
    KC = D // 128
    es = ExitStack()
    P_es = P.es
    P.es = es
    _sb0 = P.sb
    P.sb = lambda name, shape, dt: _sb0("%s_l%d" % (name, layer), shape, dt)
    acc = P.sb("m_acc", [128, nsub, D], F32)
    h32 = P.sb("m_h32", [128, D], F32)
    sq = P.sb("m_sq", [128, D], F32)
    stat = P.sb("m_stat", [128, 8], F32)
    hT32 = P.sb("m_hT32", [128, KC, 128], F32)
    hTb = P.sb("m_hTb", [128, KC, TT], BF16)
    actT = P.sb("m_actT", [128, 6, TT], BF16)
    sil = [P.sb("m_sil%d" % i, [128, TT], F32) for i in range(2)]
    wr = P.sb("m_wr", [128, KC, 72], F32)
    br = P.sb("m_br", [128, 72], F32)
    G = P.sb("m_G", [128, nsub, NE], F32)
    lg = P.sb("m_lg", [128, 72], F32)
    lem = P.sb("m_lem", [128, NE], F32)
    lem2 = P.sb("m_lem2", [128, NE], F32)
    msk = P.sb("m_msk", [128, NE], F32)
    sm = P.sb("m_sm", [128, 16], F32)
    wslot = [P.sb("m_w%d" % i, [128, 6 * D], BF16) for i in range(4)]
    wq = ["w0", "w1", "w2", "w3"]
    psb = c.psb

    P.dma("sp", c.dq["misc"], c.gvec[:], V(io["norm_ffn"].t[layer:layer + 1, :].partition_broadcast(128), io["norm_ffn"]))
    P.dma("sp", c.dq["misc"], wr[:, :, 0:8], V(io["moe_w_group"].t[layer].rearrange("(k p) n -> p k n", p=128), io["moe_w_group"]))
    P.dma("sp", c.dq["misc"], wr[:, :, 8:72], V(io["moe_w_expert"].t[layer].rearrange("(k p) n -> p k n", p=128), io["moe_w_expert"]))
    P.dma("sp", c.dq["misc"], br[:, 0:8], V(io["moe_b_group"].t[layer:layer + 1, :].partition_broadcast(128), io["moe_b_group"]))
    P.dma("sp", c.dq["misc"], br[:, 8:72], V(io["moe_b_expert"].t[layer:layer + 1, :].partition_broadcast(128), io["moe_b_expert"]))

    wgu = io.get("moe_w_gu")
    wdn = io.get("moe_w_down")
    slot_i = [0]

    def load_piece(e, piece):
        s = slot_i[0] % 4
        slot_i[0] += 1
        dst = wslot[s]
        if piece < 2:
            src = wgu.t[layer, e, :, piece * FH:(piece + 1) * FH].rearrange("(k p) f -> p k f", p=128)
            dv = dst[:, :].re("p (k f) -> p k f", k=KC)
            for hk in range(2):
                P.dma("pool", c.dq[wq[s]], dv[:, hk * 8:(hk + 1) * 8, :], V(src[:, hk * 8:(hk + 1) * 8, :], wgu))
        else:
            src = wdn.t[layer, e].rearrange("(k p) d -> p k d", p=128)
            dv = dst[:, :].re("p (k d) -> p k d", k=6)
            for hk in range(2):
                P.dma("pool", c.dq[wq[s]], dv[:, hk * 3:(hk + 1) * 3, :], V(src[:, hk * 3:(hk + 1) * 3, :], wdn))
        return dst

    for t0 in range(0, ntok, TT):
        for s in range(nsub):
            r0 = t0 + s * 128
            P.dma("sp", c.dq["xin"], acc[:, s, :], X[r0:r0 + 128, :])
            rms_h(P, c, acc[:, s, :], h32[:], sq[:], stat[:])
            if DBG_CUT == 2:
                continue
            for q in range(4):
                for j in range(4):
                    k = q * 4 + j
                    P.tr(psb[q][:, j * 128:(j + 1) * 128], h32[:, k * 128:(k + 1) * 128], c.ident[:], sig=(j == 3))
                if DBG_CUT == 6:
                    continue
                P.copy(hT32[:, q * 4:(q + 1) * 4, :], psb[q][:, :].re("p (j t) -> p j t", j=4), eng="act")
                if DBG_CUT == 7:
                    continue
                P.copy(hTb[:, q * 4:(q + 1) * 4, s * 128:(s + 1) * 128], hT32[:, q * 4:(q + 1) * 4, :], eng="dve")
            if DBG_CUT in (3, 6, 7):
                continue
            for k in range(KC):
                P.mm(psb[4][:, 0:72], hT32[:, k, :], wr[:, k, :], start=(k == 0), stop=(k == KC - 1))
            if DBG_CUT == 4:
                continue
            P.tt(lg[:], psb[4][:, 0:72], br[:], ALU.add)
            P.red(sm[:, 0:1], lg[:, 0:8], ALU.max)
            P.ts(sm[:, 1:2], sm[:, 0:1], -1.0, None, ALU.mult)
            P.act(msk[:, 0:8], lg[:, 0:8], AF.Exp, bias=sm[:, 1:2], accum=sm[:, 2:3])
            P.op("dve", lambda e: e.reciprocal(out=sm.t[:, 3:4], in_=sm.t[:, 2:3]), [sm[:]], [sm[:]])
            P.ts(msk[:, 8:16], lg[:, 0:8], sm[:, 0:1], None, ALU.is_ge)
            P.ts(msk[:, 8:16], msk[:, 8:16], 1e30, -1e30, ALU.mult, ALU.add)
            P.tt(lem[:, :].re("p (g e) -> p g e", g=8), lg[:, 8:72].re("p (g e) -> p g e", g=8),
                 V(msk.t[:, 8:16].unsqueeze(2).broadcast_to([128, 8, 8]), msk), ALU.add)
            if DBG_CUT == 5:
                continue
            P.red(sm[:, 4:5], lem[:], ALU.max)
            P.ts(msk[:], lem[:], sm[:, 4:5], None, ALU.is_ge)
            P.stt(lem2[:], msk[:], -1e30, lem[:], ALU.mult, ALU.add)
            P.red(sm[:, 5:6], lem2[:], ALU.max)
            P.ts(lem2[:], lem2[:], sm[:, 5:6], None, ALU.is_ge)
            P.tt(sm[:, 6:7], sm[:, 5:6], sm[:, 4:5], ALU.subtract)
            P.act(sm[:, 7:8], sm[:, 6:7], AF.Exp)
            P.ts(sm[:, 8:9], sm[:, 7:8], 1.0, None, ALU.add)
            P.op("dve", lambda e: e.reciprocal(out=sm.t[:, 9:10], in_=sm.t[:, 8:9]), [sm[:]], [sm[:]])
            P.tt(sm[:, 10:11], sm[:, 9:10], sm[:, 3:4], ALU.mult)
            P.tt(sm[:, 11:12], sm[:, 10:11], sm[:, 7:8], ALU.mult)
            P.ts(msk[:], msk[:], sm[:, 10:11], None, ALU.mult)
            P.stt(G[:, s, :], lem2[:], sm[:, 11:12], msk[:], ALU.mult, ALU.add)
        for e in range((DBG_NE or NE) if not DBG_CUT else 0):
            wg = load_piece(e, 0)
            wu = load_piece(e, 1)
            wd = load_piece(e, 2)
            wgv = wg[:, :].re("p (k f) -> p k f", k=KC)
            wuv = wu[:, :].re("p (k f) -> p k f", k=KC)
            wdv = wd[:, :].re("p (k d) -> p k d", k=6)
            for fc in range(6):
                pg = psb[(fc % 2) * 2]
                pu = psb[(fc % 2) * 2 + 1]
                for k in range(KC):
                    P.mm(pg[:, 0:TT], wgv[:, k, fc * 128:(fc + 1) * 128], hTb[:, k, :], start=(k == 0), stop=(k == KC - 1))
                for k in range(KC):
                    P.mm(pu[:, 0:TT], wuv[:, k, fc * 128:(fc + 1) * 128], hTb[:, k, :], start=(k == 0), stop=(k == KC - 1))
                P.act(sil[fc % 2][:, :], pg[:, 0:TT], AF.Silu)
                P.tt(actT[:, fc, :], sil[fc % 2][:, :], pu[:, 0:TT], ALU.mult)
            i = 0
            for s in range(nsub):
                for dg in range(4):
                    pd = psb[4 + (i % 4)]
                    i += 1
                    for fc in range(6):
                        P.mm(pd[:, :], actT[:, fc, s * 128:(s + 1) * 128], wdv[:, fc, dg * 512:(dg + 1) * 512],
                             start=(fc == 0), stop=(fc == 5))
                    P.stt(acc[:, s, dg * 512:(dg + 1) * 512], pd[:, :], G[:, s, e:e + 1],
                          acc[:, s, dg * 512:(dg + 1) * 512], ALU.mult, ALU.add)
        for s in range(nsub):
            r0 = t0 + s * 128
            P.dma("sp", c.dq["xout"], X[r0:r0 + 128, :], acc[:, s, :])
    P.barrier()
    P.es = P_es
    P.sb = _sb0
    return es


I32 = mybir.dt.int32


def floor_pos(P, out, x, itmp, ftmp):
    P.copy(itmp, x)
    P.copy(out, itmp)
    P.tt(ftmp, out, x, ALU.is_gt)
    P.tt(out, out, ftmp, ALU.subtract)


def stage_moe_g(P, c, X, layer, io, TS=512):
    ntok = c.ntok
    nsub = TS // 128
    KC = D // 128
    NTI = ntok // 128
    NT = ntok // TS + 8
    NSLOT = NT * TS
    psb = c.psb
    L_ = "_g%d" % layer
    HS = P.dram("mg_HS" + L_, [NSLOT, D], BF16)
    GS = P.dram("mg_GS" + L_, [NSLOT, 8], F32)
    YS = P.dram("mg_YS" + L_, [NSLOT, D], F32)
    wsrc = [io["moe_wg_r"], io["moe_wu_r"], io["moe_wd_r"]]
    eso = ExitStack(); P_eso = P.es; P.es = eso
    slot_i = P.sb("mg_slot" + L_, [128, NTI], I32)
    idxW = P.sb("mg_idxW" + L_, [128, NT * 8], I32)
    P.es = P_eso
    es = ExitStack(); P_es = P.es; P.es = es
    _sb0 = P.sb
    P.sb = lambda name, shape, dt: _sb0(name + L_, shape, dt)
    hall = P.sb("g_hall", [128, NTI, D], BF16)
    xt = P.sb("g_xt", [128, D], F32)
    h32 = P.sb("g_h32", [128, D], F32)
    sq = P.sb("g_sq", [128, D], F32)
    stat = P.sb("g_stat", [128, 8], F32)
    hT32 = P.sb("g_hT32", [128, KC, 128], F32)
    wr = P.sb("g_wr", [128, KC, 72], F32)
    br = P.sb("g_br", [128, 72], F32)
    G = P.sb("g_G", [128, NE], F32)
    lg = P.sb("g_lg", [128, 72], F32)
    lem = P.sb("g_lem", [128, NE], F32)
    lem2 = P.sb("g_lem2", [128, NE], F32)
    msk = P.sb("g_msk", [128, NE], F32)
    sm = P.sb("g_sm", [128, 16], F32)
    OH = P.sb("g_OH", [128, NTI, 8], F32)
    G8 = P.sb("g_G8", [128, NTI, 8], F32)
    R = P.sb("g_R", [128, NTI, 8], F32)
    OHs = P.sb("g_OHs", [128, 8], F32)
    Ls = P.sb("g_Ls", [128, 128], F32)
    ones = P.sb("g_ones", [128, 128], F32)
    zb = P.sb("g_zb", [128, D], BF16)
    sc = P.sb("g_sc", [128, 64], F32)
    sci = P.sb("g_sci", [128, 64], I32)
    jv = P.sb("g_jv", [128, NT], F32)
    gidf = P.sb("g_gidf", [128, NT], F32)
    idf = P.sb("g_idf", [128, NT, 8], F32)
    Tt = P.sb("g_Tt", [128, NTI, 8], F32)
    slf = P.sb("g_slf", [128, NTI], F32)
    P.memset(Ls[:], 1.0, eng="pool")
    P.op("pool", lambda e: e.affine_select(out=Ls.t[:], in_=Ls.t[:], pattern=[[1, 128]], compare_op=ALU.is_gt,
                                           fill=0.0, base=0, channel_multiplier=-1), [Ls[:]], [Ls[:]])
    P.memset(ones[:], 1.0)
    P.memset(OHs[:], 0.0)
    P.memset(zb[:], 0.0)
    P.op("pool", lambda e: e.iota(jv.t[:], pattern=[[1, NT]], base=0, channel_multiplier=0,
                                  allow_small_or_imprecise_dtypes=True), [], [jv[:]])
    P.op("pool", lambda e: e.iota(idf.t[:], pattern=[[0, NT], [128, 8]], base=layer * NE * 128, channel_multiplier=1,
                                  allow_small_or_imprecise_dtypes=True), [], [idf[:]])
    for r0 in range(0, NSLOT, 128):
        P.dma("sp", c.dq["xout"], HS[r0:r0 + 128, :], zb[:])
    P.dma("sp", c.dq["xout"], V(GS.t.rearrange("(a p) e -> p a e", p=128), GS),
          V(zb.t[:, 0:NSLOT // 128 * 8 * 2].bitcast(F32).rearrange("p (a e) -> p a e", e=8), zb))
    P.dma("sp", c.dq["misc"], c.gvec[:], V(io["norm_ffn"].t[layer:layer + 1, :].partition_broadcast(128), io["norm_ffn"]))
    P.dma("sp", c.dq["misc"], wr[:, :, 0:8], V(io["moe_w_group"].t[layer].rearrange("(k p) n -> p k n", p=128), io["moe_w_group"]))
    P.dma("sp", c.dq["misc"], wr[:, :, 8:72], V(io["moe_w_expert"].t[layer].rearrange("(k p) n -> p k n", p=128), io["moe_w_expert"]))
    P.dma("sp", c.dq["misc"], br[:, 0:8], V(io["moe_b_group"].t[layer:layer + 1, :].partition_broadcast(128), io["moe_b_group"]))
    P.dma("sp", c.dq["misc"], br[:, 8:72], V(io["moe_b_expert"].t[layer:layer + 1, :].partition_broadcast(128), io["moe_b_expert"]))
    for i in range(NTI):
        r0 = i * 128
        P.dma("sp", c.dq["xin"], xt[:], X[r0:r0 + 128, :])
        rms_h(P, c, xt[:], h32[:], sq[:], stat[:])
        P.copy(hall[:, i, :], h32[:], eng="pool")
        for q in range(4):
            for j in range(4):
                k = q * 4 + j
                P.tr(psb[q][:, j * 128:(j + 1) * 128], h32[:, k * 128:(k + 1) * 128], c.ident[:], sig=(j == 3))
            P.copy(hT32[:, q * 4:(q + 1) * 4, :], psb[q][:, :].re("p (j t) -> p j t", j=4), eng="act")
        for k in range(KC):
            P.mm(psb[4][:, 0:72], hT32[:, k, :], wr[:, k, :], start=(k == 0), stop=(k == KC - 1))
        P.tt(lg[:], psb[4][:, 0:72], br[:], ALU.add)
        P.red(sm[:, 0:1], lg[:, 0:8], ALU.max)
        P.ts(sm[:, 1:2], sm[:, 0:1], -1.0, None, ALU.mult)
        P.act(msk[:, 0:8], lg[:, 0:8], AF.Exp, bias=sm[:, 1:2], accum=sm[:, 2:3])
        P.op("dve", lambda e: e.reciprocal(out=sm.t[:, 3:4], in_=sm.t[:, 2:3]), [sm[:]], [sm[:]])
        P.ts(OH[:, i, :], lg[:, 0:8], sm[:, 0:1], None, ALU.is_ge)
        P.ts(msk[:, 8:16], OH[:, i, :], 1e30, -1e30, ALU.mult, ALU.add)
        P.tt(lem[:, :].re("p (g e) -> p g e", g=8), lg[:, 8:72].re("p (g e) -> p g e", g=8),
             V(msk.t[:, 8:16].unsqueeze(2).broadcast_to([128, 8, 8]), msk), ALU.add)
        P.red(sm[:, 4:5], lem[:], ALU.max)
        P.ts(msk[:], lem[:], sm[:, 4:5], None, ALU.is_ge)
        P.stt(lem2[:], msk[:], -1e30, lem[:], ALU.mult, ALU.add)
        P.red(sm[:, 5:6], lem2[:], ALU.max)
        P.ts(lem2[:], lem2[:], sm[:, 5:6], None, ALU.is_ge)
        P.tt(sm[:, 6:7], sm[:, 5:6], sm[:, 4:5], ALU.subtract)
        P.act(sm[:, 7:8], sm[:, 6:7], AF.Exp)
        P.ts(sm[:, 8:9], sm[:, 7:8], 1.0, None, ALU.add)
        P.op("dve", lambda e: e.reciprocal(out=sm.t[:, 9:10], in_=sm.t[:, 8:9]), [sm[:]], [sm[:]])
        P.tt(sm[:, 10:11], sm[:, 9:10], sm[:, 3:4], ALU.mult)
        P.tt(sm[:, 11:12], sm[:, 10:11], sm[:, 7:8], ALU.mult)
        P.ts(msk[:], msk[:], sm[:, 10:11], None, ALU.mult)
        P.stt(G[:], lem2[:], sm[:, 11:12], msk[:], ALU.mult, ALU.add)
        P.red(G8[:, i, :], G[:, :].re("p (g e) -> p e g", g=8), ALU.add)
        P.mm(psb[5][:, 0:8], Ls[:], OH[:, i, :], True, False, sig=False)
        P.mm(psb[5][:, 0:8], ones[:], OHs[:], False, True)
        P.copy(R[:, i, :], psb[5][:, 0:8])
        P.tt(OHs[:], OHs[:], OH[:, i, :], ALU.add)
    P.mm(psb[5][:, 8:16], ones[:], OHs[:], True, True)
    P.ts(sc[:, 0:8], psb[5][:, 8:16], float(TS - 1), 1.0 / TS, ALU.add, ALU.mult)
    floor_pos(P, sc[:, 8:16], sc[:, 0:8], sci[:, 0:8], sc[:, 16:24])
    P.copy(sc[:, 24:25], sc[:, 8:9])
    for g in range(1, 8):
        P.tt(sc[:, 24 + g:25 + g], sc[:, 23 + g:24 + g], sc[:, 8 + g:9 + g], ALU.add)
    P.tt(sc[:, 32:40], sc[:, 24:32], sc[:, 8:16], ALU.subtract)
    P.ts(sc[:, 32:40], sc[:, 32:40], float(TS), None, ALU.mult)
    P.tt(Tt[:], R[:], V(sc.t[:, 32:40].unsqueeze(1).broadcast_to([128, NTI, 8]), sc), ALU.add)
    P.tt(Tt[:], Tt[:], OH[:], ALU.mult)
    P.red(slf[:], Tt[:], ALU.add)
    P.copy(slot_i[:], slf[:])
    P.ts(gidf[:], jv[:], sc[:, 24:25], None, ALU.is_ge)
    for g in range(1, 8):
        P.stt(gidf[:], jv[:], sc[:, 24 + g:25 + g], gidf[:], ALU.is_ge, ALU.add)
    P.ts(gidf[:], gidf[:], 7.0, 1024.0, ALU.min, ALU.mult)
    P.tt(idf[:], idf[:], V(gidf.t[:, :].unsqueeze(2).broadcast_to([128, NT, 8]), gidf), ALU.add)
    P.copy(idxW[:, :].re("p (j e) -> p j e", e=8), idf[:])
    for i in range(NTI):
        for (dst, src) in ((HS, hall[:, i, :]), (GS, G8[:, i, :])):
            P._wait("pool", P._deps([src, slot_i[:]], [dst[:, :]]))
            ins = P.nc.gpsimd.indirect_dma_start(out=dst.t[:, :], out_offset=bass.IndirectOffsetOnAxis(ap=slot_i.t[:, i:i + 1], axis=0),
                                                 in_=src.ap, in_offset=None)
            ins.then_inc(P.sem[c.dq["w3"]], 16); P.cnt[c.dq["w3"]] += 1
            P._record(c.dq["w3"], P.cnt[c.dq["w3"]], [src, slot_i[:]], [dst[:, :]])
    P.barrier(); P.es = P_es; P.sb = _sb0; es.close()

    es = ExitStack(); P_es = P.es; P.es = es
    _sb0 = P.sb
    P.sb = lambda name, shape, dt: _sb0(name + L_, shape, dt)
    acc = P.sb("h_acc", [128, nsub, D], F32)
    hs = P.sb("h_hs", [128, nsub, D], BF16)
    hTb = P.sb("h_hTb", [128, KC, TS], BF16)
    actT = P.sb("h_actT", [128, 6, TS], BF16)
    sil = [P.sb("h_sil%d" % i, [128, TS], F32) for i in range(2)]
    gst = P.sb("h_gst", [128, nsub, 8], F32)
    wslot = [P.sb("h_w%d" % i, [128, 6 * D], BF16) for i in range(5)]
    wq = ["w0", "w1", "w2", "w3", "w4"]
    slot_n = [0]
    pbf = [V(psb[i].t[:, :].bitcast(BF16), psb[i]) for i in range(2)]

    def gather_piece(j, e, piece):
        s_ = slot_n[0] % 5
        slot_n[0] += 1
        dst = wslot[s_]
        ixa = idxW.t[:, j * 8 + e:j * 8 + e + 1]
        src = wsrc[piece]
        P._wait("pool", P._deps([idxW[:]], [dst[:]]))
        ins = P.nc.gpsimd.indirect_dma_start(out=dst.t[:, :], out_offset=None, in_=src.t[:, :],
                                             in_offset=bass.IndirectOffsetOnAxis(ap=ixa, axis=0))
        ins.then_inc(P.sem[c.dq[wq[s_]]], 16); P.cnt[c.dq[wq[s_]]] += 1
        P._record(c.dq[wq[s_]], P.cnt[c.dq[wq[s_]]], [src[:, :], idxW[:]], [dst[:]])
        return dst

    for j in range(NT):
        t0 = j * TS
        P.dma("sp", c.dq["xin"], hs[:], V(HS.t[t0:t0 + TS, :].rearrange("(s p) d -> p s d", p=128), HS))
        P.dma("sp", c.dq["xin"], gst[:], V(GS.t[t0:t0 + TS, :].rearrange("(s p) e -> p s e", p=128), GS))
        for s in range(nsub):
            for q in range(2):
                for jj in range(8):
                    k = q * 8 + jj
                    P.tr(pbf[q][:, jj * 128:(jj + 1) * 128], hs[:, s, k * 128:(k + 1) * 128], c.identb[:], sig=(jj == 7))
                P.copy(hTb[:, q * 8:(q + 1) * 8, s * 128:(s + 1) * 128], pbf[q][:, :].re("p (j t) -> p j t", j=8), eng="act")
        for e in range(8):
            wg = gather_piece(j, e, 0)
            wu = gather_piece(j, e, 1)
            wd = gather_piece(j, e, 2)
            wgv = wg[:, :].re("p (k f) -> p k f", k=KC)
            wuv = wu[:, :].re("p (k f) -> p k f", k=KC)
            wdv = wd[:, :].re("p (k d) -> p k d", k=6)
            for fc in range(6):
                pg = psb[(fc % 2) * 2]
                pu = psb[(fc % 2) * 2 + 1]
                for k in range(KC):
                    P.mm(pg[:, 0:TS], wgv[:, k, fc * 128:(fc + 1) * 128], hTb[:, k, :], start=(k == 0), stop=(k == KC - 1))
                for k in range(KC):
                    P.mm(pu[:, 0:TS], wuv[:, k, fc * 128:(fc + 1) * 128], hTb[:, k, :], start=(k == 0), stop=(k == KC - 1))
                P.act(sil[fc % 2][:, :], pg[:, 0:TS], AF.Silu)
                P.tt(actT[:, fc, :], sil[fc % 2][:, :], pu[:, 0:TS], ALU.mult)
            i = 0
            for s in range(nsub):
                for dg in range(4):
                    pd = psb[4 + (i % 4)]
                    i += 1
                    for fc in range(6):
                        P.mm(pd[:, :], actT[:, fc, s * 128:(s + 1) * 128], wdv[:, fc, dg * 512:(dg + 1) * 512],
                             start=(fc == 0), stop=(fc == 5))
                    a_ = acc[:, s, dg * 512:(dg + 1) * 512]
                    if e == 0:
                        P.ts(a_, pd[:, :], gst[:, s, 0:1], None, ALU.mult)
                    else:
                        P.stt(a_, pd[:, :], gst[:, s, e:e + 1], a_, ALU.mult, ALU.add)
        P.dma("sp", c.dq["xout"], V(YS.t[t0:t0 + TS, :].rearrange("(s p) d -> p s d", p=128), YS), acc[:])
        if j == 1 and layer == 0:
            P.dbg("g_hs", hs[:], [128, nsub, D], BF16)
            P.dbg("g_gst", gst[:], [128, nsub, 8], F32)
            P.dbg("g_hTb", hTb[:], [128, KC, TS], BF16)
            P.dbg("g_acc", acc[:], [128, nsub, D], F32)
    P.barrier(); P.es = P_es; P.sb = _sb0; es.close()

    es = ExitStack(); P_es = P.es; P.es = es
    _sb0 = P.sb
    P.sb = lambda name, shape, dt: _sb0(name + L_, shape, dt)
    xr = [P.sb("r_x%d" % i, [128, D], F32) for i in range(2)]
    yr = [P.sb("r_y%d" % i, [128, D], F32) for i in range(2)]
    ysem = [c.dq["w0"], c.dq["w1"]]
    for i in range(NTI):
        b = i % 2
        r0 = i * 128
        P.dma("sp", c.dq["xin"], xr[b][:], X[r0:r0 + 128, :])
        P._wait("pool", P._deps([YS[:, :], slot_i[:]], [yr[b][:]]))
        ins = P.nc.gpsimd.indirect_dma_start(out=yr[b].t[:, :], out_offset=None, in_=YS.t[:, :],
                                             in_offset=bass.IndirectOffsetOnAxis(ap=slot_i.t[:, i:i + 1], axis=0))
        ins.then_inc(P.sem[ysem[b]], 16); P.cnt[ysem[b]] += 1
        P._record(ysem[b], P.cnt[ysem[b]], [YS[:, :], slot_i[:]], [yr[b][:]])
        P.tt(xr[b][:], xr[b][:], yr[b][:], ALU.add)
        P.dma("sp", c.dq["xout"], X[r0:r0 + 128, :], xr[b][:])
    P.barrier(); P.es = P_es; P.sb = _sb0; es.close()
    eso.close()


def front_end(P, c, X, t0, nsub, xt, h32, sq, stat, hTb, psb, identb_unused=None):
    for s in range(nsub):
        r0 = t0 + s * 128
        P.dma("sp", c.dq["xin"], xt[:], X[r0:r0 + 128, :])
        rms_h(P, c, xt[:], h32[:], sq[:], stat[:])
        for q in range(4):
            for j in range(4):
                k = q * 4 + j
                P.tr(psb[q][:, j * 128:(j + 1) * 128], h32[:, k * 128:(k + 1) * 128], c.ident[:], sig=(j == 3))
            P.copy(hTb[:, q * 4:(q + 1) * 4, s * 128:(s + 1) * 128], psb[q][:, :].re("p (j t) -> p j t", j=4), eng="act")


def stage_mlstm(P, c, X, io):
    ntok = c.ntok
    NH, DK, DV, L = 8, 128, 256, 64
    KC = D // 128
    TT = 512
    nsub = TT // 128
    psb = c.psb
    win = io["ml_w_in"]
    QT = P.dram("ml_QT", [NH, DK, ntok], BF16)
    KT = P.dram("ml_KT", [NH, DK, ntok], BF16)
    Ktm = P.dram("ml_Ktm", [ntok, NH * DK], F32)
    Vs = P.dram("ml_V", [ntok, NH * DV], BF16)
    Os = P.dram("ml_O", [ntok, NH * DV], F32)
    Gs = P.dram("ml_G", [ntok, 16], F32)
    OUTs = P.dram("ml_OUT", [ntok, NH * DV], BF16)

    es = ExitStack(); P_es = P.es; P.es = es
    xt = P.sb("a_xt", [128, D], F32)
    h32 = P.sb("a_h32", [128, D], F32)
    sq = P.sb("a_sq", [128, D], F32)
    stat = P.sb("a_stat", [128, 8], F32)
    hTb = P.sb("a_hTb", [128, KC, TT], BF16)
    wsl = [P.sb("a_w%d" % i, [128, KC, 512], BF16) for i in range(3)]
    wg = P.sb("a_wg", [128, KC, 16], BF16)
    bg = P.sb("a_bg", [128, 16], F32)
    stb = [P.sb("a_stb%d" % i, [128, 512], BF16) for i in range(3)]
    stf = [P.sb("a_stf%d" % i, [128, 512], F32) for i in range(3)]
    gt = P.sb("a_gt", [128, 48], F32)
    P.dma("sp", c.dq["misc"], c.gvec[:], V(io["norm_mix"].t[1:2, :].partition_broadcast(128), io["norm_mix"]))
    P.dma("pool", c.dq["misc"], wg[:], V(win.t[0, :, 6144:6160].rearrange("(k p) n -> p k n", p=128), win))
    P.dma("sp", c.dq["misc"], bg[:], V(io["ml_b_gate"].t[0:1, :].partition_broadcast(128), io["ml_b_gate"]))
    wq = ["w0", "w1", "w2"]
    cnt = [0, 0, 0]
    for t0 in range(0, ntok, TT):
        front_end(P, c, X, t0, nsub, xt, h32, sq, stat, hTb, psb)
        for s in range(nsub):
            r0 = t0 + s * 128
            for k in range(KC):
                P.mm(psb[4][:, 0:16], hTb[:, k, s * 128:(s + 1) * 128], wg[:, k, :], start=(k == 0), stop=(k == KC - 1))
            P.tt(gt[:, 0:16], psb[4][:, 0:16], bg[:], ALU.add)
            P.act(gt[:, 16:32], gt[:, 0:16], AF.Tanh, scale=1.0 / 15.0)
            P.ts(gt[:, 32:40], gt[:, 16:24], 15.0, None, ALU.mult)
            P.act(gt[:, 0:8], gt[:, 24:32], AF.Exp, scale=-15.0)
            P.ts(gt[:, 0:8], gt[:, 0:8], 1.0, None, ALU.add)
            P.act(gt[:, 8:16], gt[:, 0:8], AF.Ln)
            P.ts(gt[:, 40:48], gt[:, 8:16], -1.0, None, ALU.mult)
            P.dma("sp", c.dq["xout"], Gs[r0:r0 + 128, :], gt[:, 32:48])
        for blk in range(12):
            i = cnt[0] % 3
            cnt[0] += 1
            w = wsl[i]
            for hk in range(2):
                P.dma("pool", c.dq[wq[i]], w[:, hk * 8:(hk + 1) * 8, :],
                      V(win.t[0, :, blk * 512:(blk + 1) * 512].rearrange("(k p) n -> p k n", p=128)[:, hk * 8:(hk + 1) * 8, :], win))
            if blk < 4:
                dst = QT if blk < 2 else KT
                sc = 1.0 if blk < 2 else DK ** -0.5
                for hh in range(4):
                    head = (blk % 2) * 4 + hh
                    pp = psb[4 + (cnt[1] % 2)]
                    for k in range(KC):
                        P.mm(pp[:, :], w[:, k, hh * 128:(hh + 1) * 128], hTb[:, k, :], start=(k == 0), stop=(k == KC - 1))
                    sb_ = stb[cnt[1] % 3]
                    cnt[1] += 1
                    P.act(sb_[:], pp[:, :], AF.Copy, scale=sc)
                    P.dma("sp", c.dq["xout"], dst[head, :, t0:t0 + TT], sb_[:])
            if blk >= 2:
                for s in range(nsub):
                    r0 = t0 + s * 128
                    pp = psb[6 + (cnt[2] % 2)]
                    for k in range(KC):
                        P.mm(pp[:, :], hTb[:, k, s * 128:(s + 1) * 128], w[:, k, :], start=(k == 0), stop=(k == KC - 1))
                    j = cnt[2] % 3
                    cnt[2] += 1
                    if blk < 4:
                        P.act(stf[j][:], pp[:, :], AF.Copy, scale=DK ** -0.5)
                        P.dma("sp", c.dq["xout"], Ktm[r0:r0 + 128, (blk - 2) * 512:(blk - 1) * 512], stf[j][:])
                    elif blk < 8:
                        P.copy(stb[j][:], pp[:, :], eng="act")
                        P.dma("sp", c.dq["xout"], Vs[r0:r0 + 128, (blk - 4) * 512:(blk - 3) * 512], stb[j][:])
                    else:
                        P.act(stf[j][:], pp[:, :], AF.Sigmoid)
                        P.dma("sp", c.dq["xout"], Os[r0:r0 + 128, (blk - 8) * 512:(blk - 7) * 512], stf[j][:])
    P.barrier(); P.es = P_es; es.close()

    es = ExitStack(); P_es = P.es; P.es = es
    triU = P.sb("b_triU", [64, 64], F32)
    negm = P.sb("b_negm", [64, 64], F32)
    ones = P.sb("b_ones", [64, 128], F32)
    onesb = P.sb("b_onesb", [64, 1], BF16)
    Cst = P.sb("b_C", [128, NH, DV], F32)
    Cb = P.sb("b_Cb", [128, NH, DV], BF16)
    nst = P.sb("b_n", [128, NH], F32)
    nb = P.sb("b_nb", [128, NH], BF16)
    ghead = P.sb("b_gh", [64, NH * DV], F32)
    P.memset(triU[:], 1.0, eng="pool")
    P.op("pool", lambda e: e.affine_select(out=triU.t[:], in_=triU.t[:], pattern=[[1, 64]], compare_op=ALU.is_ge,
                                           fill=0.0, base=0, channel_multiplier=-1), [triU[:]], [triU[:]])
    P.ts(negm[:], triU[:], 30000.0, -30000.0, ALU.mult, ALU.add)
    P.memset(ones[:], 1.0)
    P.memset(onesb[:], 1.0)
    P.memset(Cst[:], 0.0); P.memset(Cb[:], 0.0); P.memset(nst[:], 0.0); P.memset(nb[:], 0.0)
    P.dma("sp", c.dq["misc"], ghead[:], V(io["ml_g_head"].t[0:1, :].partition_broadcast(64), io["ml_g_head"]))
    NB = 2
    qT = [P.sb("b_qT%d" % i, [128, NH, L], BF16) for i in range(NB)]
    kT = [P.sb("b_kT%d" % i, [128, NH, L], BF16) for i in range(NB)]
    ktm = [P.sb("b_ktm%d" % i, [64, NH * DK], F32) for i in range(NB)]
    vv = [P.sb("b_v%d" % i, [64, NH, DV], BF16) for i in range(NB)]
    oo = [P.sb("b_o%d" % i, [64, NH * DV], F32) for i in range(NB)]
    gg = [P.sb("b_g%d" % i, [64, 16], F32) for i in range(NB)]
    dsem = [P.dma_sem("bl%d" % i) for i in range(NB)]
    sml = P.sb("b_sml", [128, 96], F32)
    bts = P.sb("b_bts", [128, NH * L], F32)
    lfb = P.sb("b_lfb", [64, NH, 128], F32)
    Eb = P.sb("b_E", [64, NH, L], F32)
    Dm = P.sb("b_Dm", [64, NH, L], F32)
    SD = P.sb("b_SD", [64, NH, L], BF16)
    aT = P.sb("b_aT", [128, NH, L], F32)
    qs = P.sb("b_qs", [128, NH, L], BF16)
    kw = P.sb("b_kw", [64, NH, DK], BF16)
    hh = P.sb("b_hh", [64, NH, DV], F32)
    hsq = P.sb("b_hsq", [64, DV], F32)
    gho = P.sb("b_gho", [64, NH * DV], F32)
    outb = [P.sb("b_out%d" % i, [64, NH * DV], BF16) for i in range(2)]
    nchunk = ntok // L

    def load_chunk(ci):
        b = ci % NB
        t0 = ci * L
        P.dma("sp", dsem[b], qT[b][:], V(QT.t[:, :, t0:t0 + L].rearrange("h d t -> d h t"), QT))
        P.dma("sp", dsem[b], kT[b][:], V(KT.t[:, :, t0:t0 + L].rearrange("h d t -> d h t"), KT))
        P.dma("sp", dsem[b], ktm[b][:], Ktm[t0:t0 + L, :])
        P.dma("sp", dsem[b], vv[b][:, :, :].re("t h d -> t (h d)"), Vs[t0:t0 + L, :])
        P.dma("sp", dsem[b], oo[b][:], Os[t0:t0 + L, :])
        P.dma("sp", dsem[b], gg[b][:], Gs[t0:t0 + L, :])

    load_chunk(0)
    for ci in range(nchunk):
        b = ci % NB
        if ci + 1 < nchunk:
            load_chunk(ci + 1)
        ig = gg[b][:, 0:8]
        lf = gg[b][:, 8:16]
        p0 = psb[0]
        P.mm(p0[0:64, 0:8], triU[:], lf, True, True)
        P.mm(p0[0:64, 8:16], ones[:, 0:64], lf, True, True)
        P.mm(p0[:, 16:24], ones[:, :], lf, True, True)
        P.copy(sml[:, 0:24], p0[:, 0:24])
        P.act(sml[:, 16:24], sml[:, 16:24], AF.Exp)
        P.tt(sml[0:64, 24:32], ig, sml[0:64, 0:8], ALU.subtract)
        P.tt(sml[0:64, 32:40], sml[0:64, 24:32], sml[0:64, 8:16], ALU.add)
        P.act(sml[0:64, 40:48], sml[0:64, 32:40], AF.Exp)
        P.copy(lfb[:], V(gg[b].t[:, 8:16].unsqueeze(2).broadcast_to([64, NH, 128]), gg[b]))
        for h in range(NH):
            P.mm(psb[1][:, h * L:(h + 1) * L], lfb[:, h, :], triU[:], True, True, sig=(h == NH - 1))
        for h in range(NH):
            P.mm(psb[2][0:64, h * L:(h + 1) * L], kT[b][:, h, :], qT[b][:, h, :], True, True, sig=(h == NH - 1))
        P.copy(bts[:], psb[1][:, :])
        P.act(aT[:, :, :].re("p h t -> p (h t)"), bts[:], AF.Exp)
        P.tt(Eb[:], bts[0:64, :].re("p (h t) -> p h t", h=NH), V(negm.t[:, :].unsqueeze(1).broadcast_to([64, NH, L]), negm), ALU.add)
        for h in range(NH):
            P.act(Dm[:, h, :], Eb[:, h, :], AF.Exp, bias=sml[0:64, 24 + h:25 + h])
        P.tt(SD[:], psb[2][0:64, :].re("p (h t) -> p h t", h=NH), Dm[:], ALU.mult)
        P.tt(qs[:], qT[b][:], aT[:], ALU.mult, eng="pool")
        for h in range(NH):
            P.mm(p0[0:64, 24 + h:25 + h], SD[:, h, :], onesb[:], True, False, sig=False)
            P.mm(p0[0:64, 24 + h:25 + h], qs[:, h, :], nb[:, h:h + 1], False, True, sig=(h == NH - 1))
        P.ts(sml[0:64, 48:56], p0[0:64, 24:32], -1.0, None, ALU.mult)
        P.tt(sml[0:64, 48:56], sml[0:64, 48:56], p0[0:64, 24:32], ALU.max)
        P.ts(sml[0:64, 48:56], sml[0:64, 48:56], 1.0, None, ALU.max)
        P.op("dve", lambda e: e.reciprocal(out=sml.t[0:64, 56:64], in_=sml.t[0:64, 48:56]), [sml[:]], [sml[:]])
        for h in range(NH):
            pn = psb[3 + h // 2]
            o_ = (h % 2) * DV
            P.mm(pn[0:64, o_:o_ + DV], SD[:, h, :], vv[b][:, h, :], True, False, sig=False)
            P.mm(pn[0:64, o_:o_ + DV], qs[:, h, :], Cb[:, h, :], False, True)
            P.act(hh[:, h, :], pn[0:64, o_:o_ + DV], AF.Copy, scale=sml[0:64, 56 + h:57 + h])
            P.act(hsq[:], hh[:, h, :], AF.Square, accum=sml[0:64, 64 + h:65 + h])
        P.ts(sml[0:64, 72:80], sml[0:64, 64:72], 1.0 / DV, EPS, ALU.mult, ALU.add)
        P.act(sml[0:64, 80:88], sml[0:64, 72:80], AF.Sqrt)
        P.op("dve", lambda e: e.reciprocal(out=sml.t[0:64, 88:96], in_=sml.t[0:64, 80:88]), [sml[:]], [sml[:]])
        P.tt(gho[:], oo[b][:], ghead[:], ALU.mult, eng="pool")
        ob = outb[ci % 2]
        for h in range(NH):
            P.stt(ob[:, h * DV:(h + 1) * DV], hh[:, h, :], sml[0:64, 88 + h:89 + h], gho[:, h * DV:(h + 1) * DV], ALU.mult, ALU.mult)
        P.dma("sp", c.dq["xout"], OUTs[ci * L:(ci + 1) * L, :], ob[:])
        if ci == 0:
            P.dbg("sml", sml[:], [128, 96], F32)
            P.dbg("lfb", lfb[:], [64, NH, 128], F32)
            P.dbg("bts", bts[:], [128, NH * L], F32)
            P.dbg("Eb", Eb[:], [64, NH, L], F32)
            P.dbg("Dm", Dm[:], [64, NH, L], F32)
            P.dbg("SD", SD[:], [64, NH, L], BF16)
            P.dbg("aT", aT[:], [128, NH, L], F32)
            P.dbg("qs", qs[:], [128, NH, L], BF16)
            P.dbg("qT", qT[b][:], [128, NH, L], BF16)
            P.dbg("kT", kT[b][:], [128, NH, L], BF16)
            P.dbg("vv", vv[b][:], [64, NH, DV], BF16)
            P.dbg("hh", hh[:], [64, NH, DV], F32)
            P.dbg("negm", negm[:], [64, 64], F32)
            P.dbg("triU", triU[:], [64, 64], F32)
        for h in range(NH):
            P.ts(kw[:, h, :], ktm[b][:, h * DK:(h + 1) * DK], sml[0:64, 40 + h:41 + h], None, ALU.mult)
        for h in range(NH):
            P.mm(p0[:, 32 + h:33 + h], kw[:, h, :], onesb[:], True, True, sig=(h == NH - 1))
        for h in range(NH):
            pn = psb[3 + h // 2]
            o_ = (h % 2) * DV
            P.mm(pn[:, o_:o_ + DV], kw[:, h, :], vv[b][:, h, :], True, True)
            P.stt(Cst[:, h, :], Cst[:, h, :], sml[:, 16 + h:17 + h], pn[:, o_:o_ + DV], ALU.mult, ALU.add)
        P.tt(nst[:], nst[:], sml[:, 16:24], ALU.mult)
        P.tt(nst[:], nst[:], p0[:, 32:40], ALU.add)
        P.copy(Cb[:], Cst[:], eng="pool")
        P.copy(nb[:], nst[:])
    P.barrier(); P.es = P_es; es.close()

    es = ExitStack(); P_es = P.es; P.es = es
    wo = P.sb("c_wo", [128, KC, D], BF16)
    ot = P.sb("c_ot", [128, D], BF16)
    oT = P.sb("c_oT", [128, KC, 128], BF16)
    xr = [P.sb("c_x%d" % i, [128, D], F32) for i in range(2)]
    pst = [P.ps("c_pst%d" % i, [128, 1024], BF16) for i in range(0)]
    wov = io["ml_w_out"]
    for hk in range(4):
        P.dma("pool", c.dq["w0"], wo[:, hk * 4:(hk + 1) * 4, :],
              V(wov.t[0].rearrange("(k p) n -> p k n", p=128)[:, hk * 4:(hk + 1) * 4, :], wov))
    pbf = [V(psb[i].t[:, :].bitcast(BF16), psb[i]) for i in range(2)]
    for ti, r0 in enumerate(range(0, ntok, 128)):
        x_ = xr[ti % 2]
        P.dma("sp", c.dq["xin"], ot[:], OUTs[r0:r0 + 128, :])
        P.dma("sp", c.dq["xin"], x_[:], X[r0:r0 + 128, :])
        for q in range(2):
            for j in range(8):
                k = q * 8 + j
                P.tr(pbf[q][:, j * 128:(j + 1) * 128], ot[:, k * 128:(k + 1) * 128], c.identb[:], sig=(j == 7))
            P.copy(oT[:, q * 8:(q + 1) * 8, :], pbf[q][:, :].re("p (j t) -> p j t", j=8), eng="act")
        for dg in range(4):
            pp = psb[4 + dg]
            for k in range(KC):
                P.mm(pp[:, :], oT[:, k, :], wo[:, k, dg * 512:(dg + 1) * 512], start=(k == 0), stop=(k == KC - 1))
            P.tt(x_[:, dg * 512:(dg + 1) * 512], x_[:, dg * 512:(dg + 1) * 512], pp[:, :], ALU.add)
        P.dma("sp", c.dq["xout"], X[r0:r0 + 128, :], x_[:])
    P.barrier(); P.es = P_es; es.close()


import math


def frac_pm_half(P, out, a, itmp, ftmp):
    P.copy(itmp, a)
    P.copy(ftmp, itmp)
    P.tt(out, a, ftmp, ALU.subtract)
    P.ts(ftmp, out, 0.5, None, ALU.is_gt)
    P.tt(out, out, ftmp, ALU.subtract)
    P.ts(ftmp, out, -0.5, None, ALU.is_lt)
    P.tt(out, out, ftmp, ALU.add)


def stage_s5(P, c, X, io):
    ntok = c.ntok
    KC = D // 128
    psb = c.psb
    NT = ntok // 512
    HT = P.dram("s5_HT", [D, ntok], F32)
    GT = P.dram("s5_GT", [D, ntok], BF16)
    PRM = P.dram("s5_PRM", [6, 128, 64], F32)
    es = ExitStack(); P_es = P.es; P.es = es
    xt = P.sb("sa_xt", [128, D], F32)
    h32 = P.sb("sa_h32", [128, D], F32)
    sq = P.sb("sa_sq", [128, D], F32)
    stat = P.sb("sa_stat", [128, 8], F32)
    hT = [P.sb("sa_hT%d" % i, [128, KC, 128], F32) for i in range(2)]
    P.dma("sp", c.dq["misc"], c.gvec[:], V(io["norm_mix"].t[0:1, :].partition_broadcast(128), io["norm_mix"]))
    for ti, r0 in enumerate(range(0, ntok, 128)):
        P.dma("sp", c.dq["xin"], xt[:], X[r0:r0 + 128, :])
        rms_h(P, c, xt[:], h32[:], sq[:], stat[:])
        hb = hT[ti % 2]
        for q in range(4):
            for j in range(4):
                k = q * 4 + j
                P.tr(psb[q][:, j * 128:(j + 1) * 128], h32[:, k * 128:(k + 1) * 128], c.ident[:], sig=(j == 3))
            P.copy(hb[:, q * 4:(q + 1) * 4, :], psb[q][:, :].re("p (j t) -> p j t", j=4), eng="act")
        P.dma("sp", c.dq["xout"], V(HT.t[:, r0:r0 + 128].rearrange("(k p) t -> p k t", p=128), HT), hb[:])
    lr = P.sb("sa_lr", [128, 64], F32); li = P.sb("sa_li", [128, 64], F32); ldt = P.sb("sa_ldt", [128, 1], F32)
    w = [P.sb("sa_w%d" % i, [128, 64], F32) for i in range(12)]
    cpi = P.sb("sa_cpi", [128, 1], F32)
    P.memset(cpi[:], -math.pi)
    P.dma("sp", c.dq["misc"], lr[:], io["s5_lam_re"][:, :])
    P.dma("sp", c.dq["misc"], li[:], io["s5_lam_im"][:, :])
    P.dma("sp", c.dq["misc"], ldt[:], io["s5_log_dt"][:, :])
    P.act(ldt[:], ldt[:], AF.Exp)
    P.ts(w[0][:], lr[:], ldt[:, 0:1], None, ALU.mult)
    P.ts(w[1][:], li[:], ldt[:, 0:1], None, ALU.mult)
    P.act(w[2][:], w[0][:], AF.Exp)
    wi_ = P.sb("sa_wi", [128, 64], mybir.dt.int32)
    P.ts(w[3][:], w[1][:], 1.0 / (2 * math.pi), None, ALU.mult)
    frac_pm_half(P, w[3][:], w[3][:], wi_[:], w[4][:])
    P.ts(w[4][:], w[3][:], 0.25, None, ALU.add)
    frac_pm_half(P, w[4][:], w[4][:], wi_[:], w[5][:])
    P.act(w[6][:], w[4][:], AF.Sin, scale=2 * math.pi)
    P.act(w[5][:], w[3][:], AF.Sin, scale=2 * math.pi)
    P.tt(w[7][:], w[2][:], w[6][:], ALU.mult)
    P.tt(w[8][:], w[2][:], w[5][:], ALU.mult)
    P.ts(w[7][:], w[7][:], -1.0, None, ALU.add)
    P.tt(w[9][:], lr[:], lr[:], ALU.mult)
    P.tt(w[10][:], li[:], li[:], ALU.mult)
    P.tt(w[9][:], w[9][:], w[10][:], ALU.add)
    P.op("dve", lambda e: e.reciprocal(out=w[9].t[:], in_=w[9].t[:]), [w[9][:]], [w[9][:]])
    P.tt(w[10][:], w[7][:], lr[:], ALU.mult)
    P.tt(w[11][:], w[8][:], li[:], ALU.mult)
    P.tt(w[10][:], w[10][:], w[11][:], ALU.add)
    P.tt(w[10][:], w[10][:], w[9][:], ALU.mult)
    P.tt(w[11][:], w[8][:], lr[:], ALU.mult)
    P.tt(w[0][:], w[7][:], li[:], ALU.mult)
    P.tt(w[11][:], w[11][:], w[0][:], ALU.subtract)
    P.tt(w[11][:], w[11][:], w[9][:], ALU.mult)
    P.dma("sp", c.dq["xout"], PRM[0], w[10][:])
    P.dma("sp", c.dq["xout"], PRM[1], w[11][:])
    P.dma("sp", c.dq["xout"], PRM[2], w[2][:])
    P.dma("sp", c.dq["xout"], PRM[3], w[3][:])
    P.barrier(); P.es = P_es; es.close()

    es = ExitStack(); P_es = P.es; P.es = es
    iot = P.sb("sb_iota", [128, ntok], F32)
    P.op("pool", lambda e: e.iota(iot.t[:], pattern=[[1, ntok]], base=0, channel_multiplier=0,
                                  allow_small_or_imprecise_dtypes=True), [], [iot[:]])
    MAGIC = 12582912.0
    hpi = P.sb("sb_hpi", [128, 1], F32)
    P.memset(hpi[:], math.pi / 2)
    magT = P.sb("sb_magT", [128, 64], F32)
    phiT = P.sb("sb_phiT", [128, 64], F32)
    P.dma("sp", c.dq["misc"], magT[:], V(PRM.t[2].rearrange("(q t) p -> (t p) q", t=2), PRM), allow_slow_non_contiguous=True)
    P.dma("sp", c.dq["misc"], phiT[:], V(PRM.t[3].rearrange("(q t) p -> (t p) q", t=2), PRM), allow_slow_non_contiguous=True)
    dT = P.sb("sb_dT", [128, KC], F32)
    P.dma("sp", c.dq["misc"], dT[:], io["s5_dT"][:, :])
    u = P.sb("sb_u", [128, ntok], F32)
    ysb = P.sb("sb_y", [128, ntok], F32)
    gb = P.sb("sb_gb", [128, ntok], BF16)
    NBF = 2
    bTr = [P.sb("sb_bTr%d" % i, [128, 128], F32) for i in range(NBF)]
    bTi = [P.sb("sb_bTi%d" % i, [128, 128], F32) for i in range(NBF)]
    Qr = [P.sb("sb_Qr%d" % i, [128, 128], F32) for i in range(NBF)]
    Qi = [P.sb("sb_Qi%d" % i, [128, 128], F32) for i in range(NBF)]
    cTr = [P.sb("sb_cTr%d" % i, [128, 128], F32) for i in range(NBF)]
    cTi = [P.sb("sb_cTi%d" % i, [128, 128], F32) for i in range(NBF)]
    Br = P.sb("sb_Br", [128, 128], F32); Bi = P.sb("sb_Bi", [128, 128], F32); t1 = P.sb("sb_t1", [128, 128], F32)
    gsem = [P.dma_sem("s5g%d" % i) for i in range(NBF)]
    bur = P.sb("sb_bur", [128, ntok], F32); bui = P.sb("sb_bui", [128, ntok], F32)
    Cn = P.sb("sb_Cn", [128, ntok], F32); Sn = P.sb("sb_Sn", [128, ntok], F32)
    ta = P.sb("sb_ta", [128, ntok], F32); tb = P.sb("sb_tb", [128, ntok], F32)
    mr = P.sb("sb_mr", [128, ntok], F32); mi = P.sb("sb_mi", [128, ntok], F32)
    tint_v = V(mi.t[:, :].bitcast(mybir.dt.int32), mi)

    def load_pair(q):
        b = q % NBF
        g0 = 2 * q
        P.dma("sp", gsem[b], bTr[b][:, :].re("r (g p) -> r g p", g=2), V(io["s5_bT_re"].t[g0:g0 + 2].rearrange("g r p -> r g p"), io["s5_bT_re"]))
        P.dma("sp", gsem[b], bTi[b][:, :].re("r (g p) -> r g p", g=2), V(io["s5_bT_im"].t[g0:g0 + 2].rearrange("g r p -> r g p"), io["s5_bT_im"]))
        P.dma("sp", gsem[b], cTr[b][:], V(io["s5_cT_re"].t[g0:g0 + 2].rearrange("g p c -> (g p) c"), io["s5_cT_re"]))
        P.dma("sp", gsem[b], cTi[b][:], V(io["s5_cT_im"].t[g0:g0 + 2].rearrange("g p c -> (g p) c"), io["s5_cT_im"]))
        P.dma("sp", gsem[b], Qr[b][:], V(PRM.t[0].rearrange("(q t) p -> q (t p)", t=2)[q:q + 1, :].partition_broadcast(128), PRM))
        P.dma("sp", gsem[b], Qi[b][:], V(PRM.t[1].rearrange("(q t) p -> q (t p)", t=2)[q:q + 1, :].partition_broadcast(128), PRM))

    load_pair(0)
    for fc in range(KC):
        P.dma("sp", c.dq["xin"], u[:], HT[fc * 128:(fc + 1) * 128, :])
        for qi in range(4):
            q = fc * 4 + qi
            b = q % NBF
            if q + 1 < 64:
                load_pair(q + 1)
            P.tt(Br[:], bTr[b][:], Qr[b][:], ALU.mult)
            P.tt(t1[:], bTi[b][:], Qi[b][:], ALU.mult)
            P.tt(Br[:], Br[:], t1[:], ALU.subtract)
            P.tt(Bi[:], bTi[b][:], Qr[b][:], ALU.mult)
            P.tt(t1[:], bTr[b][:], Qi[b][:], ALU.mult)
            P.tt(Bi[:], Bi[:], t1[:], ALU.add)
            P.ts(cTi[b][:], cTi[b][:], -1.0, None, ALU.mult)
            for tbk in range(NT):
                sl = slice(tbk * 512, (tbk + 1) * 512)
                P.mm(psb[tbk % 2][:, :], Br[:], u[:, sl], True, True)
                P.mm(psb[2 + tbk % 2][:, :], Bi[:], u[:, sl], True, True)
                P.copy(bur[:, sl], psb[tbk % 2][:, :], eng="act")
                P.copy(bui[:, sl], psb[2 + tbk % 2][:, :], eng="act")
            P.act(ta[:], iot[:], AF.Copy, scale=phiT[:, q:q + 1])
            P.ts(tb[:], ta[:], MAGIC, None, ALU.add)
            P.stt(mr[:], tb[:], MAGIC, ta[:], ALU.subtract, ALU.subtract)
            P.act(Sn[:], mr[:], AF.Sin, scale=-2 * math.pi)
            P.act(mi[:], mr[:], AF.Abs)
            P.act(Cn[:], mi[:], AF.Sin, scale=-2 * math.pi, bias=hpi[:, 0:1])
            P.tt(mr[:], Cn[:], bur[:], ALU.mult)
            P.tt(ta[:], Sn[:], bui[:], ALU.mult, eng="pool")
            P.tt(mr[:], mr[:], ta[:], ALU.add)
            P.tt(mi[:], Cn[:], bui[:], ALU.mult)
            P.tt(tb[:], Sn[:], bur[:], ALU.mult, eng="pool")
            P.tt(mi[:], mi[:], tb[:], ALU.subtract)
            dec = V(magT.t[:, q:q + 1].broadcast_to([128, ntok]), magT)
            P.op("dve", lambda e: e.tensor_tensor_scan(out=bur.t[:], data0=dec.ap, data1=mr.t[:], initial=0.0,
                                                       op0=ALU.mult, op1=ALU.add), [dec, mr[:]], [bur[:]])
            P.op("dve", lambda e: e.tensor_tensor_scan(out=bui.t[:], data0=dec.ap, data1=mi.t[:], initial=0.0,
                                                       op0=ALU.mult, op1=ALU.add), [dec, mi[:]], [bui[:]])
            P.tt(mr[:], Cn[:], bur[:], ALU.mult)
            P.tt(ta[:], Sn[:], bui[:], ALU.mult, eng="pool")
            P.tt(mr[:], mr[:], ta[:], ALU.subtract)
            P.tt(mi[:], Cn[:], bui[:], ALU.mult)
            P.tt(tb[:], Sn[:], bur[:], ALU.mult, eng="pool")
            P.tt(mi[:], mi[:], tb[:], ALU.add)
            for tbk in range(NT):
                sl = slice(tbk * 512, (tbk + 1) * 512)
                pp = psb[4 + tbk % 4]
                P.mm(pp[:, :], cTr[b][:], mr[:, sl], True, False, sig=False)
                P.mm(pp[:, :], cTi[b][:], mi[:, sl], False, True)
                if qi == 0:
                    P.copy(ysb[:, sl], pp[:, :])
                else:
                    P.tt(ysb[:, sl], ysb[:, sl], pp[:, :], ALU.add)
        P.stt(ysb[:], u[:], dT[:, fc:fc + 1], ysb[:], ALU.mult, ALU.add)
        P.act(gb[:], ysb[:], AF.Gelu)
        P.dma("sp", c.dq["xout"], GT[fc * 128:(fc + 1) * 128, :], gb[:])
    P.barrier(); P.es = P_es; es.close()

    es = ExitStack(); P_es = P.es; P.es = es
    gT = P.sb("sc_gT", [128, KC, 512], BF16)
    wv = [P.sb("sc_wv%d" % i, [128, KC, 512], BF16) for i in range(2)]
    wg = [P.sb("sc_wg%d" % i, [128, KC, 512], BF16) for i in range(2)]
    xr = P.sb("sc_x", [128, 4, D], F32)
    sg = [P.sb("sc_sg%d" % i, [128, 512], F32) for i in range(2)]
    wgl = io["s5_w_glu"]
    wsem = [c.dq["w0"], c.dq["w1"]]
    n = 0
    for t0 in range(0, ntok, 512):
        P.dma("sp", c.dq["xin"], gT[:], V(GT.t[:, t0:t0 + 512].rearrange("(k p) t -> p k t", p=128), GT))
        for s in range(4):
            P.dma("sp", c.dq["xin"], xr[:, s, :], X[t0 + s * 128:t0 + (s + 1) * 128, :])
        for j in range(4):
            b = n % 2
            n += 1
            for hk in range(2):
                P.dma("pool", wsem[b], wv[b][:, hk * 8:(hk + 1) * 8, :],
                      V(wgl.t[0, :, j * 512:(j + 1) * 512].rearrange("(k p) n -> p k n", p=128)[:, hk * 8:(hk + 1) * 8, :], wgl))
                P.dma("pool", wsem[b], wg[b][:, hk * 8:(hk + 1) * 8, :],
                      V(wgl.t[0, :, D + j * 512:D + (j + 1) * 512].rearrange("(k p) n -> p k n", p=128)[:, hk * 8:(hk + 1) * 8, :], wgl))
            for s in range(4):
                pv = psb[(s % 2) * 2]
                pg = psb[(s % 2) * 2 + 1]
                for k in range(KC):
                    P.mm(pv[:, :], gT[:, k, s * 128:(s + 1) * 128], wv[b][:, k, :], start=(k == 0), stop=(k == KC - 1))
                for k in range(KC):
                    P.mm(pg[:, :], gT[:, k, s * 128:(s + 1) * 128], wg[b][:, k, :], start=(k == 0), stop=(k == KC - 1))
                P.act(sg[s % 2][:], pg[:, :], AF.Sigmoid)
                P.tt(sg[s % 2][:], sg[s % 2][:], pv[:, :], ALU.mult)
                P.tt(xr[:, s, j * 512:(j + 1) * 512], xr[:, s, j * 512:(j + 1) * 512], sg[s % 2][:], ALU.add)
        for s in range(4):
            P.dma("sp", c.dq["xout"], X[t0 + s * 128:t0 + (s + 1) * 128, :], xr[:, s, :])
    P.barrier(); P.es = P_es; es.close()


def stage_final(P, c, X, io, out):
    es = ExitStack()
    P_es = P.es
    P.es = es
    xt = [P.sb("f_x%d" % i, [128, D], F32) for i in range(2)]
    ht = [P.sb("f_h%d" % i, [128, D], F32) for i in range(2)]
    sq = P.sb("f_sq", [128, D], F32)
    stat = P.sb("f_stat", [128, 8], F32)
    P.dma("sp", c.dq["misc"], c.gvec[:], V(io["norm_final"].t.unsqueeze(0).partition_broadcast(128), io["norm_final"]))
    for i, r0 in enumerate(range(0, c.ntok, 128)):
        P.dma("sp", c.dq["xin"], xt[i % 2][:], X[r0:r0 + 128, :])
        rms_h(P, c, xt[i % 2][:], ht[i % 2][:], sq[:], stat[:])
        P.dma("sp", c.dq["xout"], out[r0:r0 + 128, :], ht[i % 2][:])
    P.es = P_es
    return es


IN_SHAPES = {
    "x": [4096, D],
    "norm_ffn": [2, D], "norm_mix": [2, D], "norm_final": [D],
    "moe_w_group": [2, D, 8], "moe_b_group": [2, 8], "moe_w_expert": [2, D, 64], "moe_b_expert": [2, 64],
    "moe_w_gu": [2, NE, D, 2 * FH], "moe_w_down": [2, NE, FH, D],
    "moe_wg_r": [2 * NE * 128, 16 * FH], "moe_wu_r": [2 * NE * 128, 16 * FH], "moe_wd_r": [2 * NE * 128, 6 * D],
    "s5_lam_re": [128, 64], "s5_lam_im": [128, 64], "s5_log_dt": [128, 1], "s5_dT": [128, 16],
    "s5_bT_re": [128, 128, 64], "s5_bT_im": [128, 128, 64], "s5_cT_re": [128, 64, 128], "s5_cT_im": [128, 64, 128],
    "s5_w_glu": [1, D, 2 * D],
    "ml_w_in": [1, D, 6160], "ml_b_gate": [1, 16], "ml_g_head": [1, D], "ml_w_out": [1, D, D],
}


def build(stages=("moe0",), ntok=4096, in_names=None):
    nc = bass.Bass("TRN2", target_bir_lowering=False)
    with ExitStack() as es:
        P = Prog(nc, es)
        io = {}
        for k in (in_names or IN_SHAPES.keys()):
            shp = list(IN_SHAPES[k])
            if k == "x":
                shp[0] = ntok
            if DBG_NE and k in ("moe_w_gu", "moe_w_down"):
                shp[1] = DBG_NE
            io[k] = P.dram(k, shp, F32, kind="ExternalInput")
        out = P.dram("out", [ntok, D], F32, kind="ExternalOutput")
        X = P.dram("Xs", [ntok, D], F32)
        c = setup_common(P, ntok)
        P.dma("sp", c.dq["misc"], X[:, :], io["x"][:, :])
        for st in stages:
            if st == "moe0":
                stage_moe(P, c, X, 0, io).close()
            elif st == "moe1":
                stage_moe(P, c, X, 1, io).close()
            elif st == "mlstm":
                stage_mlstm(P, c, X, io)
            elif st == "moeg0":
                stage_moe_g(P, c, X, 0, io)
            elif st == "moeg1":
                stage_moe_g(P, c, X, 1, io)
            elif st == "s5":
                stage_s5(P, c, X, io)
        stage_final(P, c, X, io, out).close()
        P.finish()
    return nc


def moe_layout(inp):
    o = {}
    wg = inp["moe_w_gu"].reshape(2, NE, 16, 128, 2, FH)
    o["moe_wg_r"] = np.ascontiguousarray(wg[:, :, :, :, 0, :].transpose(0, 1, 3, 2, 4)).reshape(2 * NE * 128, 16 * FH)
    o["moe_wu_r"] = np.ascontiguousarray(wg[:, :, :, :, 1, :].transpose(0, 1, 3, 2, 4)).reshape(2 * NE * 128, 16 * FH)
    wd = inp["moe_w_down"].reshape(2, NE, 6, 128, D)
    o["moe_wd_r"] = np.ascontiguousarray(wd.transpose(0, 1, 3, 2, 4)).reshape(2 * NE * 128, 6 * D)
    return o


def s5_layout(inp):
    G, PS, CG = 128, 64, 16
    o = {}
    o["s5_lam_re"] = np.ascontiguousarray(inp["s5_lam_re"][0])
    o["s5_lam_im"] = np.ascontiguousarray(inp["s5_lam_im"][0])
    o["s5_log_dt"] = np.ascontiguousarray(inp["s5_log_dt"][0].reshape(G, 1))
    o["s5_dT"] = np.ascontiguousarray(inp["s5_d"][0].reshape(16, 128).T)
    for nm, src in (("s5_bT_re", "s5_b_re"), ("s5_bT_im", "s5_b_im")):
        a = np.zeros((G, 128, PS), np.float32)
        bt = np.transpose(inp[src][0], (0, 2, 1))
        for g in range(G):
            gi = g % 8
            a[g, gi * CG:(gi + 1) * CG, :] = bt[g]
        o[nm] = a
    for nm, src in (("s5_cT_re", "s5_c_re"), ("s5_cT_im", "s5_c_im")):
        a = np.zeros((G, PS, 128), np.float32)
        ct = np.transpose(inp[src][0], (0, 2, 1))
        for g in range(G):
            gi = g % 8
            a[g, :, gi * CG:(gi + 1) * CG] = ct[g]
        o[nm] = a
    o["s5_w_glu"] = inp["s5_w_glu"]
    return o


_NC_CACHE = {}


def kernel(**inputs):
    inp = {k: np.asarray(v) for k, v in inputs.items()}
    B = inp["x"].shape[0]
    stages = ("s5", "moeg0", "mlstm", "moeg1")
    shared = s5_layout(inp)
    shared.update(moe_layout(inp))
    for k in ["norm_ffn", "norm_mix", "norm_final", "moe_w_group", "moe_b_group", "moe_w_expert", "moe_b_expert",
              "ml_w_in", "ml_b_gate", "ml_g_head", "ml_w_out"]:
        shared[k] = np.ascontiguousarray(inp[k])
    names = ["x"] + list(shared.keys())
    if "nc" not in _NC_CACHE:
        _NC_CACHE["nc"] = build(stages=stages, ntok=inp["x"].shape[1], in_names=names)
    nc = _NC_CACHE["nc"]
    in_maps = []
    for b in range(B):
        m = dict(shared)
        m["x"] = np.ascontiguousarray(inp["x"][b])
        in_maps.append(m)
    res = run_bass_kernel_spmd(nc, in_maps, core_ids=list(range(B)))
    return np.stack([np.asarray(r["out"]) for r in res.results], axis=0).astype(np.float32)
```
